# Optimizing a Trainium2 kernel written in Bass

```python
import jax, jax.numpy as jnp
from jax import lax
import numpy as np

D_MODEL = 1024
BATCH = 8
SEQ = 4096
DEPTH = 4

MLA_HEADS = 8
MLA_NOPE_DIM = 64
MLA_ROPE_DIM = 32
MLA_V_DIM = 64
MLA_Q_RANK = 384
MLA_KV_RANK = 256
ROPE_THETA = 10000.0
MOBA_HEADS = 8
MOBA_HEAD_DIM = 64
MOBA_BLOCK = 256
MOBA_TOPK = 3
MOBA_Q_CHUNK = 32
RET_HEADS = 4
RET_QK_DIM = 256
RET_V_DIM = 512
RET_CHUNK = 128
D_FF = 4 * D_MODEL
ATTN_Q_BLOCK = 128
NORM_EPS = 1e-6
NEG_INF = -1e30

MLA_OUT = MLA_HEADS * MLA_V_DIM
MOBA_W = MOBA_HEADS * MOBA_HEAD_DIM
AB_IN_SIZES = (MLA_Q_RANK, MLA_KV_RANK, MLA_ROPE_DIM, MOBA_W, MOBA_W, MOBA_W)
AB_IN = sum(AB_IN_SIZES)
AB_OUT = MLA_OUT + MOBA_W
RET_QK_W = RET_HEADS * RET_QK_DIM
RET_V_W = RET_HEADS * RET_V_DIM
C_IN_SIZES = (RET_QK_W, RET_QK_W, RET_V_W, RET_V_W)
C_IN = sum(C_IN_SIZES)
N_EVEN = (DEPTH + 1) // 2
N_ODD = DEPTH // 2

kernel_name = 'hybrid_mla_moba_retention_trunk'


def split_cols(h, sizes):
    idx = [int(i) for i in np.cumsum(sizes)[:-1]]
    return jnp.split(h, idx, axis=-1)


def rms_norm(x, g):
    xf = x.astype(jnp.float32)
    y = xf * lax.rsqrt(jnp.mean(xf * xf, axis=-1, keepdims=True) + NORM_EPS)
    return (y * g.astype(jnp.float32)).astype(x.dtype)


def rotate(x, cos, sin):
    half = x.shape[-1] // 2
    xf = x.astype(jnp.float32)
    x1, x2 = xf[..., :half], xf[..., half:]
    return jnp.concatenate([x1 * cos - x2 * sin, x2 * cos + x1 * sin], axis=-1).astype(x.dtype)


def angle_tables(seq, inv_freq):
    ang = jnp.arange(seq, dtype=jnp.float32)[:, None] * inv_freq[None, :]
    return jnp.cos(ang), jnp.sin(ang)


def alibi_slopes(n_heads):
    return jnp.asarray(np.array([2.0 ** (-8.0 * (h + 1) / n_heads) for h in range(n_heads)], dtype=np.float32))


def mla_attention(q_lat, kv_lat, k_rope, q_norm_g, w_q_up, kv_norm_g, w_kv_up):
    B, S, _ = q_lat.shape
    H, dn, dr, dv = MLA_HEADS, MLA_NOPE_DIM, MLA_ROPE_DIM, MLA_V_DIM
    q = (rms_norm(q_lat, q_norm_g) @ w_q_up).reshape(B, S, H, dn + dr)
    kv = (rms_norm(kv_lat, kv_norm_g) @ w_kv_up).reshape(B, S, H, dn + dv)
    q_nope, q_rope = q[..., :dn], q[..., dn:]
    k_nope, v = kv[..., :dn], kv[..., dn:]
    inv_freq = 1.0 / (ROPE_THETA ** (jnp.arange(0, dr, 2, dtype=jnp.float32) / dr))
    cos, sin = angle_tables(S, inv_freq)
    q_rope = rotate(q_rope, cos[:, None, :], sin[:, None, :])
    k_rope = rotate(k_rope, cos, sin)
    scale = (dn + dr) ** -0.5
    k_pos = jnp.arange(S)

    def query_block(i):
        start = i * ATTN_Q_BLOCK
        qn = lax.dynamic_slice_in_dim(q_nope, start, ATTN_Q_BLOCK, axis=1)
        qr = lax.dynamic_slice_in_dim(q_rope, start, ATTN_Q_BLOCK, axis=1)
        s = (jnp.einsum('bqhd,bkhd->bhqk', qn, k_nope, preferred_element_type=jnp.float32)
             + jnp.einsum('bqhd,bkd->bhqk', qr, k_rope, preferred_element_type=jnp.float32)) * scale
        q_pos = start + jnp.arange(ATTN_Q_BLOCK)
        s = jnp.where(k_pos[None, :] <= q_pos[:, None], s, NEG_INF)
        p = jax.nn.softmax(s, axis=-1).astype(v.dtype)
        return jnp.einsum('bhqk,bkhd->bqhd', p, v)

    out = lax.map(query_block, jnp.arange(S // ATTN_Q_BLOCK))
    return jnp.moveaxis(out, 0, 1).reshape(B, S, H * dv)


def moba_attention(q, k, v):
    B, S, H, d = q.shape
    blk = MOBA_BLOCK
    nb = -(-S // blk)
    n_sel = min(MOBA_TOPK, nb - 1)
    pad = nb * blk - S

    def to_blocks(t):
        t = jnp.pad(t, ((0, 0), (0, pad), (0, 0), (0, 0)))
        return t.reshape(B, nb, blk, H, d).transpose(0, 3, 1, 2, 4)

    kb, vb = to_blocks(k), to_blocks(v)
    k_mean = jnp.mean(kb.astype(jnp.float32), axis=3)
    slopes = alibi_slopes(H)
    scale = d ** -0.5
    b_idx = jnp.arange(B)[:, None, None, None]
    h_idx = jnp.arange(H)[None, :, None, None]
    blk_ids = jnp.arange(nb)
    in_blk = jnp.arange(blk)

    def query_chunk(i):
        start = i * MOBA_Q_CHUNK
        qc = jnp.swapaxes(lax.dynamic_slice_in_dim(q, start, MOBA_Q_CHUNK, axis=1), 1, 2)
        q_pos = start + jnp.arange(MOBA_Q_CHUNK)
        own = start // blk
        k_own = lax.dynamic_index_in_dim(kb, own, axis=2, keepdims=False)
        v_own = lax.dynamic_index_in_dim(vb, own, axis=2, keepdims=False)
        dist_own = (q_pos[:, None] - (own * blk + in_blk)[None, :]).astype(jnp.float32)
        s_own = (jnp.einsum('bhqd,bhkd->bhqk', qc, k_own, preferred_element_type=jnp.float32) * scale
                 - slopes[:, None, None] * dist_own)
        s_own = jnp.where(dist_own >= 0, s_own, NEG_INF)
        if n_sel == 0:
            p = jax.nn.softmax(s_own, axis=-1).astype(v.dtype)
            o = jnp.einsum('bhqk,bhkd->bhqd', p, v_own)
        else:
            gate = jnp.einsum('bhqd,bhnd->bhqn', qc.astype(jnp.float32), k_mean)
            gate = jnp.where(blk_ids < own, gate, NEG_INF)
            _, sel = lax.top_k(gate, n_sel)
            valid = jnp.arange(n_sel) < own
            k_sel = kb[b_idx, h_idx, sel]
            v_sel = vb[b_idx, h_idx, sel]
            dist_sel = (q_pos[None, None, :, None, None] - (sel[..., None] * blk + in_blk)).astype(jnp.float32)
            s_sel = (jnp.einsum('bhqd,bhqnkd->bhqnk', qc, k_sel, preferred_element_type=jnp.float32) * scale
                     - slopes[None, :, None, None, None] * dist_sel)
            s_sel = jnp.where(valid[:, None], s_sel, NEG_INF)
            s_all = jnp.concatenate([s_sel.reshape(B, H, MOBA_Q_CHUNK, n_sel * blk), s_own], axis=-1)
            p = jax.nn.softmax(s_all, axis=-1).astype(v.dtype)
            p_sel = p[..., :n_sel * blk].reshape(B, H, MOBA_Q_CHUNK, n_sel, blk)
            o = (jnp.einsum('bhqnk,bhqnkd->bhqd', p_sel, v_sel)
                 + jnp.einsum('bhqk,bhkd->bhqd', p[..., n_sel * blk:], v_own))
        return jnp.swapaxes(o, 1, 2)

    out = lax.map(query_chunk, jnp.arange(S // MOBA_Q_CHUNK))
    return jnp.moveaxis(out, 0, 1).reshape(B, S, H * d)


def multiscale_retention(q, k, v):
    B, S, H, dk = q.shape
    dv = v.shape[-1]
    C = RET_CHUNK
    nc = S // C
    log_gamma = jnp.asarray(np.log(1.0 - 2.0 ** (-5.0 - np.arange(H))).astype(np.float32))
    inv_freq = 1.0 / (10000.0 ** jnp.linspace(0.0, 1.0, dk // 2, dtype=jnp.float32))
    cos, sin = angle_tables(S, inv_freq)
    q = rotate(q.astype(jnp.float32), cos[:, None, :], sin[:, None, :])
    k = rotate(k.astype(jnp.float32), cos[:, None, :], sin[:, None, :]) * (dk ** -0.5)
    v = v.astype(jnp.float32)

    def chunks(t):
        return t.reshape(B, nc, C, H, t.shape[-1]).transpose(1, 0, 3, 2, 4)

    qc, kc, vc = chunks(q), chunks(k), chunks(v)
    pos = jnp.arange(C, dtype=jnp.float32)
    diff = pos[:, None] - pos[None, :]
    decay = jnp.where(diff >= 0, jnp.exp(log_gamma[:, None, None] * jnp.maximum(diff, 0.0)), 0.0)
    scores = jnp.einsum('nbhqd,nbhkd->nbhqk', qc, kc) * decay
    intra = jnp.einsum('nbhqk,nbhke->nbhqe', scores, vc)
    q_decay = jnp.exp(log_gamma[:, None] * (pos + 1.0))[:, :, None]
    k_decay = jnp.exp(log_gamma[:, None] * (C - 1.0 - pos))[:, :, None]
    chunk_decay = jnp.exp(log_gamma * C)[:, None, None]

    def step(state, inp):
        qi, ki, vi = inp
        cross = jnp.einsum('bhqd,bhde->bhqe', qi * q_decay, state)
        state = chunk_decay * state + jnp.einsum('bhkd,bhke->bhde', ki * k_decay, vi)
        return state, cross

    state0 = jnp.zeros((B, H, dk, dv), jnp.float32)
    _, cross = lax.scan(step, state0, (qc, kc, vc))
    out = intra + cross
    return out.transpose(1, 0, 3, 2, 4).reshape(B, S, H, dv)


def mla_moba_mixer(h, w_in, q_norm_g, w_q_up, kv_norm_g, w_kv_up, w_out):
    B, S, _ = h.shape
    q_lat, kv_lat, k_rope, mq, mk, mv = split_cols(h @ w_in, AB_IN_SIZES)
    a = mla_attention(q_lat, kv_lat, k_rope, q_norm_g, w_q_up, kv_norm_g, w_kv_up)
    shp = (B, S, MOBA_HEADS, MOBA_HEAD_DIM)
    b = moba_attention(mq.reshape(shp), mk.reshape(shp), mv.reshape(shp))
    return jnp.concatenate([a, b], axis=-1) @ w_out


def retention_mixer(h, w_in, norm_g, w_out):
    B, S, _ = h.shape
    q, k, v, g = split_cols(h @ w_in, C_IN_SIZES)
    o = multiscale_retention(q.reshape(B, S, RET_HEADS, RET_QK_DIM),
                             k.reshape(B, S, RET_HEADS, RET_QK_DIM),
                             v.reshape(B, S, RET_HEADS, RET_V_DIM))
    mu = jnp.mean(o, axis=-1, keepdims=True)
    var = jnp.mean(jnp.square(o - mu), axis=-1, keepdims=True)
    o = ((o - mu) * lax.rsqrt(var + NORM_EPS)).reshape(B, S, RET_V_W) * norm_g.astype(jnp.float32)
    o = (jax.nn.silu(g.astype(jnp.float32)) * o).astype(h.dtype)
    return o @ w_out


def squared_relu_mlp(h, w_up, w_down):
    return jnp.square(jax.nn.relu(h @ w_up)) @ w_down


def setup_inputs(seed: int = 0) -> dict:
    key = jax.random.key(seed)
    ks = jax.random.split(key, 16)
    f32 = jnp.float32

    def w(k, shape, fan_in):
        return jax.random.normal(k, shape, f32) * (fan_in ** -0.5)

    def gain(k, shape):
        return 1.0 + 0.05 * jax.random.normal(k, shape, f32)

    return {
        'x': jax.random.normal(ks[0], (BATCH, SEQ, D_MODEL), f32),
        'norm_mix': gain(ks[1], (DEPTH, D_MODEL)),
        'norm_mlp': gain(ks[2], (DEPTH, D_MODEL)),
        'w_mlp_up': w(ks[3], (DEPTH, D_MODEL, D_FF), D_MODEL),
        'w_mlp_down': w(ks[4], (DEPTH, D_FF, D_MODEL), D_FF),
        'w_in_ab': w(ks[5], (N_EVEN, D_MODEL, AB_IN), D_MODEL),
        'mla_q_norm': gain(ks[6], (N_EVEN, MLA_Q_RANK)),
        'mla_w_q_up': w(ks[7], (N_EVEN, MLA_Q_RANK, MLA_HEADS * (MLA_NOPE_DIM + MLA_ROPE_DIM)), MLA_Q_RANK),
        'mla_kv_norm': gain(ks[8], (N_EVEN, MLA_KV_RANK)),
        'mla_w_kv_up': w(ks[9], (N_EVEN, MLA_KV_RANK, MLA_HEADS * (MLA_NOPE_DIM + MLA_V_DIM)), MLA_KV_RANK),
        'w_out_ab': w(ks[10], (N_EVEN, AB_OUT, D_MODEL), AB_OUT),
        'w_in_c': w(ks[11], (N_ODD, D_MODEL, C_IN), D_MODEL),
        'ret_norm': gain(ks[12], (N_ODD, RET_V_W)),
        'w_out_c': w(ks[13], (N_ODD, RET_V_W, D_MODEL), RET_V_W),
        'norm_final': gain(ks[14], (D_MODEL,)),
    }


def reference(x, norm_mix, norm_mlp, w_mlp_up, w_mlp_down, w_in_ab, mla_q_norm, mla_w_q_up,
              mla_kv_norm, mla_w_kv_up, w_out_ab, w_in_c, ret_norm, w_out_c, norm_final):
    for layer in range(DEPTH):
        j = layer // 2
        h = rms_norm(x, norm_mix[layer])
        if layer % 2 == 0:
            x = x + mla_moba_mixer(h, w_in_ab[j], mla_q_norm[j], mla_w_q_up[j],
                                   mla_kv_norm[j], mla_w_kv_up[j], w_out_ab[j])
        else:
            x = x + retention_mixer(h, w_in_c[j], ret_norm[j], w_out_c[j])
        h = rms_norm(x, norm_mlp[layer])
        x = x + squared_relu_mlp(h, w_mlp_up[layer], w_mlp_down[layer])
    return rms_norm(x, norm_final)
```

```python
import numpy as np
import concourse.bass as bass
import concourse.mybir as mybir

F32 = mybir.dt.float32
BF16 = mybir.dt.bfloat16
ALU = mybir.AluOpType
AF = mybir.ActivationFunctionType
AX = mybir.AxisListType

ENGS = ("pe", "act", "dve", "pool", "sp")
ROT = 24000


class Tile:
    __slots__ = ("h", "name", "w", "rs", "sem", "dcnt", "psum", "last_dma")

    def __init__(self, h, name, psum=False):
        self.h = h
        self.psum = psum
        self.name = name
        self.w = None
        self.rs = {}
        self.sem = None
        self.dcnt = 0
        self.last_dma = None

    def __getitem__(self, k):
        return self.h[k]


class Op:
    __slots__ = ("eng", "fn", "waits", "needs_inc", "gcnt", "is_dma", "tile", "dma_val", "sem")

    def __init__(self, eng, fn):
        self.eng = eng
        self.fn = fn
        self.waits = []
        self.needs_inc = False
        self.gcnt = None
        self.is_dma = False
        self.tile = None
        self.dma_val = 0


class Prog:
    def __init__(self, nc):
        self.nc = nc
        self.ops = {e: [] for e in ENGS}
        self.nops = 0
        self.dma_tiles = []
        self.banks = None
        self.sem_pool = []
        self.mark = None
        import os
        self.limit = int(os.environ.get('OPLIMIT', '100000000'))

    def make_banks(self):
        self.banks = [self.ps(f"bank{i}", [128, 512], F32) for i in range(8)]

    def sb(self, name, shape, dtype):
        return Tile(self.nc.alloc_sbuf_tensor("sb_" + name, list(shape), dtype), name)

    def ps(self, name, shape, dtype=F32):
        return Tile(self.nc.alloc_psum_tensor("ps_" + name, list(shape), dtype), name, True)

    def _dep(self, o, d):
        if d is o or d is None:
            return
        if d.eng == "pe" and o.eng == "pe" and not d.is_dma and not o.is_dma:
            return
        if not d.is_dma:
            d.needs_inc = True
        o.waits.append(d)

    def op(self, eng, fn, reads=(), writes=()):
        if self.nops >= self.limit:
            return None
        o = Op(eng, fn)
        writes = list(writes) + [t for t in reads if t.psum and t not in writes]
        for t in reads:
            self._dep(o, t.w)
        for t in writes:
            self._dep(o, t.w)
            for r in t.rs.values():
                self._dep(o, r)
        for t in reads:
            t.rs[eng] = o
        for t in writes:
            t.w = o
            t.rs = {}
        self.ops[eng].append(o)
        self.nops += 1
        return o

    def dma(self, eng, out, in_, tile, write, extra_reads=(), extra_writes=()):
        if self.nops >= self.limit:
            return None
        o = Op(eng, lambda e: e.dma_start(out=out, in_=in_))
        o.is_dma = True
        o.tile = tile
        if tile.sem is None:
            if self.sem_pool:
                tile.sem, tile.dcnt = self.sem_pool.pop(0)
            else:
                tile.sem = self.nc.alloc_semaphore("d_" + tile.name)
            self.dma_tiles.append(tile)
        if not (write and tile.w is not None and tile.w.is_dma):
            self._dep(o, tile.w)
        if write:
            for r in tile.rs.values():
                self._dep(o, r)
        tile.dcnt += 1
        o.dma_val = 16 * tile.dcnt
        o.sem = tile.sem
        tile.last_dma = o
        if write:
            tile.w = o
            tile.rs = {}
        else:
            tile.rs["dma"] = o
        self.ops[eng].append(o)
        self.nops += 1
        return o

    def barrier(self):
        lasts = []
        for e in ENGS:
            for o in reversed(self.ops[e]):
                if not o.is_dma:
                    lasts.append(o)
                    break
        dl = [t.last_dma for t in self.dma_tiles if t.last_dma is not None]
        news = []
        for e in ENGS:
            o = Op(e, lambda eng: eng.nop())
            for d in lasts:
                if not (d.eng == "pe" and e == "pe"):
                    d.needs_inc = True
                    o.waits.append(d)
            for d in dl:
                o.waits.append(d)
            news.append(o)
        for o in news:
            self.ops[o.eng].append(o)
            self.nops += 1

    def stage_begin(self):
        self.mark = self.nc.sbuf_base
        self.dma_tiles = []

    def stage_end(self):
        self.barrier()
        for t in self.dma_tiles:
            self.sem_pool.append((t.sem, t.dcnt))
            t.sem = None
        self.dma_tiles = []
        self.nc.sbuf_base = self.mark

    def finalize(self, final_wait_tiles=()):
        nc = self.nc
        esems = {}
        for e in ENGS:
            c = 0
            for o in self.ops[e]:
                if o.needs_inc and not o.is_dma:
                    c += 1
                    o.gcnt = c
            nsem = (c + ROT - 1) // ROT
            esems[e] = [nc.alloc_semaphore(f"s_{e}{i}") for i in range(max(nsem, 1))]
        self.esems = esems
        final_waits = [(t.sem, 16 * t.dcnt) for t in final_wait_tiles if t.sem is not None]

        def emit(engobj, e):
            seen_eng = {}
            seen_sem = {}
            for o in self.ops[e]:
                for d in o.waits:
                    if d.is_dma:
                        s = d.sem
                        if seen_sem.get(s.num, 0) >= d.dma_val:
                            continue
                        seen_sem[s.num] = d.dma_val
                        engobj.wait_ge(s, d.dma_val)
                    else:
                        if seen_eng.get(d.eng, 0) >= d.gcnt:
                            continue
                        seen_eng[d.eng] = d.gcnt
                        k = (d.gcnt - 1) // ROT
                        engobj.wait_ge(esems[d.eng][k], d.gcnt - k * ROT)
                ins = o.fn(engobj)
                if o.is_dma:
                    ins.then_inc(o.sem, 16)
                elif o.needs_inc:
                    k = (o.gcnt - 1) // ROT
                    ins.then_inc(esems[e][k], 1)
            if e == "sp":
                for s, v in final_waits:
                    engobj.wait_ge(s, v)

        with nc.Block() as block:
            @block.tensor
            def _(eng):
                emit(eng, "pe")

            @block.scalar
            def _(eng):
                emit(eng, "act")

            @block.vector
            def _(eng):
                emit(eng, "dve")

            @block.gpsimd
            def _(eng):
                emit(eng, "pool")

            @block.sync
            def _(eng):
                emit(eng, "sp")

D = 1024
FF = 4096
EPS = 1e-6


class Common:
    def __init__(self, P):
        self.P = P
        nc = P.nc
        self.ident = P.sb("ident", [128, 128], BF16)
        self.eps = P.sb("eps", [128, 1], F32)
        self.identf = P.sb("identf32", [128, 128], F32)
        self._rr = 0

    def setup(self, ident_dram):
        P = self.P
        P.dma("pool", self.ident[:, :], ident_dram, self.ident, True)
        P.dma("sp", self.identf[:, :], ident_dram, self.identf, True)
        P.op("dve", lambda e: e.memset(self.eps[:, :], EPS), writes=[self.eps])


def rms_rstd(P, C, xt, ncols, ss, sq, rstd, junk):
    P.op("act", lambda e: e.activation(out=junk[:, :ncols], in_=xt[:, :ncols], func=AF.Square,
                                       accum_out=ss[:, :]),
         reads=[xt], writes=[junk, ss])
    P.op("act", lambda e: e.activation(out=sq[:, :], in_=ss[:, :], func=AF.Sqrt,
                                       bias=C.eps[:, :], scale=1.0 / ncols),
         reads=[ss, C.eps], writes=[sq])
    P.op("dve", lambda e: e.reciprocal(rstd[:, :], sq[:, :]), reads=[sq], writes=[rstd])


def mlp_stage(P, C, S, x_in, x_out, wup_d, wdn_d, g_d, gfin_d=None, pfx="m"):
    nc = P.nc
    TB = 256
    NT = TB // 128
    nblk = S // TB
    KC = D // 128
    FC = FF // 128

    wup = P.sb(f"{pfx}wup", [128, KC, FF], BF16)
    wdn = P.sb(f"{pfx}wdn", [128, FC, D], BF16)
    gb = P.sb(f"{pfx}gb", [128, D], F32)
    gfb = P.sb(f"{pfx}gfb", [128, D], F32) if gfin_d is not None else None
    NX = 4
    xt = [P.sb(f"{pfx}xt{i}", [128, D], F32) for i in range(NX)]
    xn = [P.sb(f"{pfx}xn{i}", [128, D], BF16) for i in range(2)]
    junk = P.sb(f"{pfx}junk", [128, D], BF16)
    st = [[P.sb(f"{pfx}st{i}_{j}", [128, 1], F32) for j in range(3)] for i in range(2)]
    hT = [P.sb(f"{pfx}hT{i}", [128, KC, TB], BF16) for i in range(2)]
    uT = [P.sb(f"{pfx}uT{f}", [128, TB], BF16) for f in range(FC)]
    rl = [P.sb(f"{pfx}rl{i}", [128, TB], F32) for i in range(4)]
    Bk = P.banks
    tp = [Bk[0], Bk[1]]
    up = [Bk[2], Bk[3], Bk[4]]
    dn = [Bk[5], Bk[6]]

    for k in range(KC):
        for c in range(0, FF, 2048):
            P.dma("pool", wup[:, k, c:c + 2048], wup_d[k * 128:(k + 1) * 128, c:c + 2048], wup, True)
    for f in range(FC):
        P.dma("pool", wdn[:, f, :], wdn_d[f * 128:(f + 1) * 128, :], wdn, True)
    P.dma("sp", gb[:, :], g_d.partition_broadcast(128), gb, True)
    if gfb is not None:
        P.dma("sp", gfb[:, :], gfin_d.partition_broadcast(128), gfb, True)

    def load_x(b, t):
        i = (b * NT + t) % NX
        r0 = b * TB + t * 128
        P.dma("sp", xt[i][:, :], x_in[r0:r0 + 128, :], xt[i], True)

    for t in range(NT):
        load_x(0, t)
    cnt = 0
    for b in range(nblk):
        h = hT[b % 2]
        for t in range(NT):
            i = (b * NT + t) % NX
            x = xt[i]
            s = st[t % 2]
            rms_rstd(P, C, x, D, s[0], s[1], s[2], junk)
            n = xn[t % 2]
            P.op("dve", lambda e, n=n, x=x, s=s: e.scalar_tensor_tensor(
                out=n[:, :], in0=x[:, :], scalar=s[2][:, :], in1=gb[:, :], op0=ALU.mult, op1=ALU.mult),
                reads=[x, s[2], gb], writes=[n])
            p = tp[t % 2]
            pv = p.h[:, :].bitcast(BF16).rearrange("p (a b) -> p a b", b=128)
            for k in range(KC):
                P.op("pe", lambda e, pv=pv, n=n, k=k: e.transpose(pv[:, k, :], n[:, k * 128:(k + 1) * 128],
                                                                   C.ident[:, :]),
                     reads=[n, C.ident], writes=[p])
            P.op("act", lambda e, h=h, pv=pv, t=t: e.copy(out=h[:, :, t * 128:(t + 1) * 128], in_=pv[:, 0:8, :]),
                 reads=[p], writes=[h])
        if b + 1 < nblk:
            for t in range(NT):
                load_x(b + 1, t)
        for f in range(FC):
            u = up[f % 3]
            for k in range(KC):
                P.op("pe", lambda e, u=u, k=k, f=f, h=h: e.matmul(
                    u[:, :TB], lhsT=wup[:, k, f * 128:(f + 1) * 128], rhs=h[:, k, :],
                    start=(k == 0), stop=(k == KC - 1)),
                    reads=[wup, h], writes=[u])
            r = rl[f % 4]
            if f % 2 == 0:
                P.op("act", lambda e, r=r, u=u: e.activation(out=r[:, :], in_=u[:, :TB], func=AF.Relu),
                     reads=[u], writes=[r])
            else:
                P.op("dve", lambda e, r=r, u=u: e.tensor_scalar(out=r[:, :], in0=u[:, :TB], scalar1=0.0,
                                                                 scalar2=None, op0=ALU.max),
                     reads=[u], writes=[r])
            P.op("pool", lambda e, r=r, f=f: e.tensor_tensor(out=uT[f][:, :], in0=r[:, :], in1=r[:, :],
                                                              op=ALU.mult),
                 reads=[r], writes=[uT[f]])
        for t in range(NT):
            i = (b * NT + t) % NX
            x = xt[i]
            for dh in range(2):
                d = dn[cnt % 2]
                cnt += 1
                for f in range(FC):
                    P.op("pe", lambda e, d=d, f=f, t=t, dh=dh: e.matmul(
                        d[:, :], lhsT=uT[f][:, t * 128:(t + 1) * 128], rhs=wdn[:, f, dh * 512:(dh + 1) * 512],
                        start=(f == 0), stop=(f == FC - 1)),
                        reads=[uT[f], wdn], writes=[d])
                P.op("dve", lambda e, d=d, x=x, dh=dh: e.tensor_tensor(
                    out=x[:, dh * 512:(dh + 1) * 512], in0=x[:, dh * 512:(dh + 1) * 512], in1=d[:, :],
                    op=ALU.add), reads=[x, d], writes=[x])
            if gfb is not None:
                s = st[t % 2]
                rms_rstd(P, C, x, D, s[0], s[1], s[2], junk)
                P.op("dve", lambda e, x=x, s=s: e.scalar_tensor_tensor(
                    out=x[:, :], in0=x[:, :], scalar=s[2][:, :], in1=gfb[:, :], op0=ALU.mult, op1=ALU.mult),
                    reads=[x, s[2], gfb], writes=[x])
            r0 = b * TB + t * 128
            P.dma("sp", x_out[r0:r0 + 128, :], x[:, :], x, False)
    return xt


RH = 4
RDK = 256
RDV = 512
CH = 128
LOG_GAMMA = np.log(1.0 - 2.0 ** (-5.0 - np.arange(RH))).astype(np.float32).astype(np.float64)


def ret_consts(S):
    pos = np.arange(S, dtype=np.float32)
    inv_freq = (1.0 / (10000.0 ** np.linspace(0.0, 1.0, RDK // 2, dtype=np.float32))).astype(np.float32)
    ang = pos[:, None] * inv_freq[None, :]
    cos, sin = np.cos(ang).astype(np.float64), np.sin(ang).astype(np.float64)
    p = (np.arange(S) % CH).astype(np.float64)
    qdec = np.exp(LOG_GAMMA[None, :] * (p[:, None] + 1.0))
    tq = np.zeros((S, 2, RH, 128), np.float32)
    tq[:, 0] = cos[:, None, :] * qdec[:, :, None]
    tq[:, 1] = sin[:, None, :] * qdec[:, :, None]
    tk = np.zeros((S, 2, 128), np.float32)
    tk[:, 0] = cos * RDK ** -0.5
    tk[:, 1] = sin * RDK ** -0.5
    pc = np.arange(CH, dtype=np.float64)
    kdec = np.exp(LOG_GAMMA[None, :] * (CH - 1.0 - pc[:, None])).astype(np.float32)
    dt = np.zeros((CH, RH, CH), np.float32)
    for h in range(RH):
        m = np.exp(-LOG_GAMMA[h] * (pc[:, None] + 1.0)) * (pc[:, None] <= pc[None, :])
        dt[:, h, :] = m
    cdec = [float(np.exp(LOG_GAMMA[h] * CH)) for h in range(RH)]
    return dict(tq=tq.reshape(S, 1024), tk=tk.reshape(S, 256), kdec=kdec, dt=dt.reshape(CH, RH * CH)), cdec


def norm_T(P, C, x, gb, xn, st, junk, bank, hT):
    rms_rstd(P, C, x, D, st[0], st[1], st[2], junk)
    P.op("dve", lambda e: e.scalar_tensor_tensor(out=xn[:, :], in0=x[:, :], scalar=st[2][:, :], in1=gb[:, :],
                                                 op0=ALU.mult, op1=ALU.mult),
         reads=[x, st[2], gb], writes=[xn])
    bv = bank.h[:, :].bitcast(BF16).rearrange("p (a b) -> p a b", b=128)
    for k in range(8):
        P.op("pe", lambda e, k=k: e.transpose(bv[:, k, :], xn[:, k * 128:(k + 1) * 128], C.ident[:, :]),
             reads=[xn, C.ident], writes=[bank])
    P.op("act", lambda e: e.copy(out=hT[:, :, :], in_=bv[:, 0:8, :]), reads=[bank], writes=[hT])


def retA_stage(P, C, S, x_in, on_out, win_d, g_d, cst, cdec, pfx="ra"):
    nch = S // CH
    B = P.banks
    w = P.sb(f"{pfx}w", [128, 8, 4096], BF16)
    gb = P.sb(f"{pfx}gb", [128, D], F32)
    dtt = P.sb(f"{pfx}dt", [128, RH, CH], F32)
    kdec = P.sb(f"{pfx}kdec", [128, RH], F32)
    xt = [P.sb(f"{pfx}xt{i}", [128, D], F32) for i in range(2)]
    tq = [P.sb(f"{pfx}tq{i}", [128, 2, RH, 128], F32) for i in range(2)]
    tk = [P.sb(f"{pfx}tk{i}", [128, 2, 128], F32) for i in range(2)]
    xn = P.sb(f"{pfx}xn", [128, D], BF16)
    junk = P.sb(f"{pfx}junk", [128, D], BF16)
    st = [P.sb(f"{pfx}st{j}", [128, 1], F32) for j in range(3)]
    hT = P.sb(f"{pfx}hT", [128, 8, 128], BF16)
    qs = P.sb(f"{pfx}qs", [128, RH, 2, 128], F32)
    ks = P.sb(f"{pfx}ks", [128, RH, 2, 128], F32)
    tmp = [P.sb(f"{pfx}tmp{i}", [128, RH, 128], F32) for i in range(4)]
    qd = P.sb(f"{pfx}qd", [128, RH, 2, 128], BF16)
    kr = P.sb(f"{pfx}kr", [128, RH, 2, 128], BF16)
    v = [P.sb(f"{pfx}v{h}", [128, RDV], BF16) for h in range(RH)]
    vd = [P.sb(f"{pfx}vd{h}", [128, RDV], BF16) for h in range(RH)]
    qdT = P.sb(f"{pfx}qdT", [128, 8, 128], BF16)
    kT = P.sb(f"{pfx}kT", [128, 8, 128], BF16)
    Sf = [P.sb(f"{pfx}S{h}", [128, 2, RDV], F32) for h in range(RH)]
    Sb = [P.sb(f"{pfx}Sb{h}", [128, 2, RDV], BF16) for h in range(RH)]
    PT = [P.sb(f"{pfx}PT{i}", [128, 128], BF16) for i in range(2)]
    gst = [[P.sb(f"{pfx}gs{i}_{j}", [128, 8], F32) for j in range(5)] for i in range(2)]
    on = [P.sb(f"{pfx}on{i}", [128, RH * RDV], F32) for i in range(2)]

    for k in range(8):
        for c0 in range(0, 4096, 2048):
            P.dma("pool", w[:, k, c0:c0 + 2048], win_d[k * 128:(k + 1) * 128, c0:c0 + 2048], w, True)
    P.dma("sp", gb[:, :], g_d.partition_broadcast(128), gb, True)
    P.dma("sp", dtt[:, :, :], cst["dt"].rearrange("p (h q) -> p h q", h=RH), dtt, True)
    P.dma("sp", kdec[:, :], cst["kdec"], kdec, True)

    def load(c):
        i = c % 2
        P.dma("sp", xt[i][:, :], x_in[c * CH:(c + 1) * CH, :], xt[i], True)
        P.dma("sp", tq[i][:, :, :, :], cst["tq"][c * CH:(c + 1) * CH, :].rearrange("p (a h f) -> p a h f", a=2, h=RH),
              tq[i], True)
        P.dma("sp", tk[i][:, :, :], cst["tk"][c * CH:(c + 1) * CH, :].rearrange("p (a f) -> p a f", a=2), tk[i], True)

    def proj(col0, ncols, bank):
        for k in range(8):
            P.op("pe", lambda e, k=k: e.matmul(bank[:, :ncols], lhsT=hT[:, k, :], rhs=w[:, k, col0:col0 + ncols],
                                               start=(k == 0), stop=(k == 7)),
                 reads=[hT, w], writes=[bank])

    def rotate(src, dst, cs, sn, tab):
        x1, x2 = src[:, :, 0, :], src[:, :, 1, :]
        P.op("pool", lambda e: e.tensor_tensor(out=tmp[0][:, :, :], in0=x1, in1=cs, op=ALU.mult), reads=[src, tab], writes=[tmp[0]])
        P.op("pool", lambda e: e.tensor_tensor(out=tmp[1][:, :, :], in0=x2, in1=sn, op=ALU.mult), reads=[src, tab], writes=[tmp[1]])
        P.op("dve", lambda e: e.tensor_tensor(out=dst[:, :, 0, :], in0=tmp[0][:, :, :], in1=tmp[1][:, :, :], op=ALU.subtract),
             reads=[tmp[0], tmp[1]], writes=[dst])
        P.op("pool", lambda e: e.tensor_tensor(out=tmp[2][:, :, :], in0=x2, in1=cs, op=ALU.mult), reads=[src, tab], writes=[tmp[2]])
        P.op("pool", lambda e: e.tensor_tensor(out=tmp[3][:, :, :], in0=x1, in1=sn, op=ALU.mult), reads=[src, tab], writes=[tmp[3]])
        P.op("dve", lambda e: e.tensor_tensor(out=dst[:, :, 1, :], in0=tmp[2][:, :, :], in1=tmp[3][:, :, :], op=ALU.add),
             reads=[tmp[2], tmp[3]], writes=[dst])

    load(0)
    for c in range(nch):
        i = c % 2
        x = xt[i]
        norm_T(P, C, x, gb, xn, st, junk, B[0], hT)
        if c + 1 < nch:
            load(c + 1)
        for g in range(2):
            b = B[2 + g]
            proj(g * 512, 512, b)
            P.op("act", lambda e, b=b, g=g: e.copy(out=qs[:, 2 * g:2 * g + 2, :, :],
                                                   in_=b[:, :].rearrange("p (h a f) -> p h a f", h=2, a=2)),
                 reads=[b], writes=[qs])
        rotate(qs, qd, tq[i][:, 0, :, :], tq[i][:, 1, :, :], tq[i])
        for g in range(2):
            b = B[2 + g]
            proj(1024 + g * 512, 512, b)
            P.op("act", lambda e, b=b, g=g: e.copy(out=ks[:, 2 * g:2 * g + 2, :, :],
                                                   in_=b[:, :].rearrange("p (h a f) -> p h a f", h=2, a=2)),
                 reads=[b], writes=[ks])
        rotate(ks, kr, tk[i][:, 0, :].unsqueeze(1).to_broadcast([128, RH, 128]),
               tk[i][:, 1, :].unsqueeze(1).to_broadcast([128, RH, 128]), tk[i])
        for h in range(RH):
            b = B[2 + h % 2]
            proj(2048 + h * 512, 512, b)
            P.op("act", lambda e, b=b, h=h: e.copy(out=v[h][:, :], in_=b[:, :]), reads=[b], writes=[v[h]])
            P.op("dve", lambda e, b=b, h=h: e.tensor_scalar(out=vd[h][:, :], in0=b[:, :], scalar1=kdec[:, h:h + 1],
                                                            scalar2=None, op0=ALU.mult),
                 reads=[b, kdec], writes=[vd[h]])
        for (src, dstT, bank) in ((qd, qdT, B[0]), (kr, kT, B[1])):
            bv = bank.h[:, :].bitcast(BF16).rearrange("p (a b) -> p a b", b=128)
            for h in range(RH):
                for a in range(2):
                    P.op("pe", lambda e, src=src, bv=bv, h=h, a=a: e.transpose(bv[:, 2 * h + a, :], src[:, h, a, :],
                                                                              C.ident[:, :]),
                         reads=[src, C.ident], writes=[bank])
            P.op("dve" if src is qd else "act",
                 (lambda e, dstT=dstT, bv=bv: e.tensor_copy(out=dstT[:, :, :], in_=bv[:, 0:8, :])) if src is qd else
                 (lambda e, dstT=dstT, bv=bv: e.copy(out=dstT[:, :, :], in_=bv[:, 0:8, :])),
                 reads=[bank], writes=[dstT])
        o_t = on[c % 2]
        for h in range(RH):
            sc = B[4]
            for a in range(2):
                P.op("pe", lambda e, h=h, a=a: e.matmul(sc[:, 0:128], lhsT=kT[:, 2 * h + a, :], rhs=qdT[:, 2 * h + a, :],
                                                        start=(a == 0), stop=(a == 1)),
                     reads=[kT, qdT], writes=[sc])
            pt = PT[h % 2]
            P.op("dve", lambda e, pt=pt, h=h: e.tensor_tensor(out=pt[:, :], in0=sc[:, 0:128], in1=dtt[:, h, :], op=ALU.mult),
                 reads=[sc, dtt], writes=[pt])
            ob = B[5]
            P.op("pe", lambda e, pt=pt, h=h: e.matmul(ob[:, :], lhsT=pt[:, :], rhs=v[h][:, :], start=True, stop=(c == 0)),
                 reads=[pt, v[h]], writes=[ob])
            if c > 0:
                for a in range(2):
                    P.op("pe", lambda e, h=h, a=a: e.matmul(ob[:, :], lhsT=qdT[:, 2 * h + a, :], rhs=Sb[h][:, a, :],
                                                            start=False, stop=(a == 1)),
                         reads=[qdT, Sb[h]], writes=[ob])
            for a in range(2):
                su = B[6 + a]
                P.op("pe", lambda e, su=su, h=h, a=a: e.matmul(su[:, :], lhsT=kr[:, h, a, :], rhs=vd[h][:, :],
                                                               start=True, stop=True),
                     reads=[kr, vd[h]], writes=[su])
                if c == 0:
                    P.op("dve", lambda e, su=su, h=h, a=a: e.tensor_copy(out=Sf[h][:, a, :], in_=su[:, :]),
                         reads=[su], writes=[Sf[h]])
                else:
                    P.op("dve", lambda e, su=su, h=h, a=a: e.scalar_tensor_tensor(
                        out=Sf[h][:, a, :], in0=Sf[h][:, a, :], scalar=cdec[h], in1=su[:, :], op0=ALU.mult, op1=ALU.add),
                        reads=[su, Sf[h]], writes=[Sf[h]])
            if c + 1 < nch:
                P.op("pool", lambda e, h=h: e.tensor_copy(out=Sb[h][:, :, :], in_=Sf[h][:, :, :]),
                     reads=[Sf[h]], writes=[Sb[h]])
            g5 = gst[h % 2]
            P.op("dve", lambda e, g5=g5: e.bn_stats(out=g5[0][:, 0:6], in_=ob[:, :]), reads=[ob], writes=[g5[0]])
            P.op("dve", lambda e, g5=g5: e.bn_aggr(out=g5[1][:, 0:2], in_=g5[0][:, 0:6]), reads=[g5[0]], writes=[g5[1]])
            P.op("act", lambda e, g5=g5: e.activation(out=g5[2][:, 0:1], in_=g5[1][:, 1:2], func=AF.Sqrt,
                                                      bias=C.eps[:, :], scale=1.0),
                 reads=[g5[1], C.eps], writes=[g5[2]])
            P.op("dve", lambda e, g5=g5: e.reciprocal(g5[3][:, 0:1], g5[2][:, 0:1]), reads=[g5[2]], writes=[g5[3]])
            P.op("dve", lambda e, g5=g5: e.tensor_scalar(out=g5[4][:, 0:1], in0=g5[1][:, 0:1], scalar1=g5[3][:, 0:1],
                                                         scalar2=-1.0, op0=ALU.mult, op1=ALU.mult),
                 reads=[g5[1], g5[3]], writes=[g5[4]])
            P.op("act", lambda e, g5=g5, h=h, o_t=o_t: e.activation(
                out=o_t[:, h * RDV:(h + 1) * RDV], in_=ob[:, :], func=AF.Identity, bias=g5[4][:, 0:1], scale=g5[3][:, 0:1]),
                reads=[ob, g5[3], g5[4]], writes=[o_t])
        P.dma("sp", on_out[c * CH:(c + 1) * CH, :], o_t[:, :], o_t, False)
    return on


def retB_stage(P, C, S, x_in, on_in, x_out, win_d, wout_d, g_d, rng_d, pfx="rb"):
    nt = S // 128
    B = P.banks
    wg = P.sb(f"{pfx}wg", [128, 8, 2048], BF16)
    wo = P.sb(f"{pfx}wo", [128, 16, D], BF16)
    stg = [P.sb(f"{pfx}stg{i}", [128, D], F32) for i in range(2)]
    rng = P.sb(f"{pfx}rng", [128, 16], F32)
    gb = P.sb(f"{pfx}gb", [128, D], F32)
    xt = [P.sb(f"{pfx}xt{i}", [128, D], F32) for i in range(3)]
    ont = [P.sb(f"{pfx}on{i}", [128, 2048], F32) for i in range(2)]
    xn = P.sb(f"{pfx}xn", [128, D], BF16)
    junk = P.sb(f"{pfx}junk", [128, D], BF16)
    st = [P.sb(f"{pfx}st{j}", [128, 1], F32) for j in range(3)]
    hT = P.sb(f"{pfx}hT", [128, 8, 128], BF16)
    sg = [P.sb(f"{pfx}sg{i}", [128, 512], F32) for i in range(2)]
    og = P.sb(f"{pfx}og", [128, 2048], BF16)
    ogT = P.sb(f"{pfx}ogT", [128, 16, 128], BF16)

    for k in range(8):
        P.dma("pool", wg[:, k, :], win_d[k * 128:(k + 1) * 128, 4096:6144], wg, True)
    P.dma("sp", gb[:, :], g_d.partition_broadcast(128), gb, True)
    P.dma("sp", rng[:, :], rng_d, rng, True)
    for kc in range(16):
        s_ = stg[kc % 2]
        P.dma("sp", s_[:, :], wout_d[kc * 128:(kc + 1) * 128, :], s_, True)
        P.op("pool", lambda e, s_=s_, kc=kc: e.tensor_scalar(out=wo[:, kc, :], in0=s_[:, :], scalar1=rng[:, kc:kc + 1],
                                                             scalar2=None, op0=ALU.mult),
             reads=[s_, rng], writes=[wo])

    def load(t):
        P.dma("sp", xt[t % 3][:, :], x_in[t * 128:(t + 1) * 128, :], xt[t % 3], True)
        P.dma("sp", ont[t % 2][:, :], on_in[t * 128:(t + 1) * 128, :], ont[t % 2], True)

    load(0)
    for t in range(nt):
        x = xt[t % 3]
        o_ = ont[t % 2]
        norm_T(P, C, x, gb, xn, st, junk, B[0], hT)
        if t + 1 < nt:
            load(t + 1)
        for g in range(4):
            b = B[2 + g % 2]
            for k in range(8):
                P.op("pe", lambda e, k=k, g=g, b=b: e.matmul(b[:, :], lhsT=hT[:, k, :], rhs=wg[:, k, g * 512:(g + 1) * 512],
                                                             start=(k == 0), stop=(k == 7)),
                     reads=[hT, wg], writes=[b])
            s_ = sg[g % 2]
            P.op("act", lambda e, s_=s_, b=b: e.activation(out=s_[:, :], in_=b[:, :], func=AF.Silu), reads=[b], writes=[s_])
            P.op("dve", lambda e, s_=s_, g=g, o_=o_: e.tensor_tensor(out=og[:, g * 512:(g + 1) * 512], in0=s_[:, :],
                                                                     in1=o_[:, g * 512:(g + 1) * 512], op=ALU.mult),
                 reads=[s_, o_], writes=[og])
        for half in range(2):
            bank = B[half]
            bv = bank.h[:, :].bitcast(BF16).rearrange("p (a b) -> p a b", b=128)
            for k in range(8):
                kc = half * 8 + k
                P.op("pe", lambda e, bv=bv, k=k, kc=kc: e.transpose(bv[:, k, :], og[:, kc * 128:(kc + 1) * 128], C.ident[:, :]),
                     reads=[og, C.ident], writes=[bank])
            P.op("act" if half == 0 else "dve",
                 (lambda e, bv=bv, half=half: e.copy(out=ogT[:, half * 8:half * 8 + 8, :], in_=bv[:, 0:8, :])) if half == 0 else
                 (lambda e, bv=bv, half=half: e.tensor_copy(out=ogT[:, half * 8:half * 8 + 8, :], in_=bv[:, 0:8, :])),
                 reads=[bank], writes=[ogT])
        for dh in range(2):
            b = B[4 + dh]
            for kc in range(16):
                P.op("pe", lambda e, b=b, kc=kc, dh=dh: e.matmul(b[:, :], lhsT=ogT[:, kc, :], rhs=wo[:, kc, dh * 512:(dh + 1) * 512],
                                                                 start=(kc == 0), stop=(kc == 15)),
                     reads=[ogT, wo], writes=[b])
            P.op("dve", lambda e, b=b, dh=dh, x=x: e.tensor_tensor(out=x[:, dh * 512:(dh + 1) * 512],
                                                                   in0=x[:, dh * 512:(dh + 1) * 512], in1=b[:, :], op=ALU.add),
                 reads=[x, b], writes=[x])
        P.dma("sp", x_out[t * 128:(t + 1) * 128, :], x[:, :], x, False)
    return xt


NH = 8
MLA_SCALE = 96 ** -0.5
MOBA_SCALE = 0.125
BLK = 256
NEGV = 30000.0


def ab_consts(S):
    pos = np.arange(S, dtype=np.float32)
    inv_freq = (1.0 / (10000.0 ** (np.arange(0, 32, 2, dtype=np.float32) / 32))).astype(np.float32)
    ang = pos[:, None] * inv_freq[None, :]
    cos, sin = np.cos(ang), np.sin(ang)
    rq = np.zeros((S, 2, NH, 16), np.float32)
    rq[:, 0] = (cos * MLA_SCALE)[:, None, :]
    rq[:, 1] = (sin * MLA_SCALE)[:, None, :]
    rk = np.zeros((S, 2, 16), np.float32)
    rk[:, 0] = cos
    rk[:, 1] = sin
    pc = np.arange(128)
    negm = np.where(pc[:, None] > pc[None, :], -NEGV, 0.0).astype(np.float32)
    slopes = np.array([2.0 ** (-(h + 1)) for h in range(NH)], np.float64)
    p = np.arange(S)
    blk = p // BLK
    r = p % BLK
    kc = np.zeros((S, NH, 32), np.float32)
    kc[p, :, blk] = NEGV
    kc[:, :, 16] = 1.0
    kc[:, :, 17] = 1.0
    kc[:, :, 18] = slopes[None, :] * 256.0 * blk[:, None]
    kc[:, :, 19] = slopes[None, :] * r[:, None]
    kc[:, :, 20] = 1.0
    qc = np.zeros((S, NH, 4), np.float32)
    qc[:, :, 0] = -slopes[None, :] * 256.0 * blk[:, None]
    qc[:, :, 1] = -slopes[None, :] * r[:, None]
    qc[:, :, 2] = 1.0
    qc[:, :, 3] = 1.0
    return dict(rq=rq.reshape(S, 256), rk=rk.reshape(S, 32), negm=negm, kc=kc.reshape(S, 256), qc=qc.reshape(S, 32))


def attn_stage(P, C, S, kind, x_in, out_d, win_d, g_d, cst, wq_d=None, qg_d=None, wkv_d=None, kvg_d=None, pfx="at"):
    mla = kind == "mla"
    nt = S // 128
    nqb = S // 512
    nblk = S // BLK
    n_sel = min(3, nblk - 1)
    B = P.banks
    A = "a" if mla else "b"
    pfx = pfx + A

    if mla:
        win = P.sb(f"{pfx}win", [128, 8, 672], BF16)
        wq = P.sb(f"{pfx}wq", [128, 3, 768], BF16)
        wkv = P.sb(f"{pfx}wkv", [128, 2, 1024], BF16)
        qgb = P.sb(f"{pfx}qgb", [128, 384], F32)
        kvgb = P.sb(f"{pfx}kvgb", [128, 256], F32)
        rqt = [P.sb(f"{pfx}rq{i}", [128, 2, NH, 16], F32) for i in range(2)]
        rkt = [P.sb(f"{pfx}rk{i}", [128, 2, 16], F32) for i in range(2)]
        latf = P.sb(f"{pfx}latf", [128, 384], F32)
        latn = P.sb(f"{pfx}latn", [128, 384], BF16)
        latT = P.sb(f"{pfx}latT", [128, 3, 128], BF16)
        qf = P.sb(f"{pfx}qf", [128, NH, 96], F32)
        krr = P.sb(f"{pfx}krr", [128, 32], F32)
        sq = P.sb(f"{pfx}sq", [128, NH, 96], F32)
    else:
        win = P.sb(f"{pfx}win", [128, 8, 1536], BF16)
        kct = [P.sb(f"{pfx}kc{i}", [128, NH, 32], F32) for i in range(2)]
        qct = [P.sb(f"{pfx}qc{i}", [128, NH, 4], F32) for i in range(2)]
        mf = P.sb(f"{pfx}mf", [128, 512], F32)
        mqT = P.sb(f"{pfx}mqT", [128, 4, 128], F32)
        KM = P.sb(f"{pfx}KM", [128, 4, 32], F32)
        onesc = P.sb(f"{pfx}ones", [128, 2], F32)
        gs = P.sb(f"{pfx}gs", [128, NH, 16], F32)
        m8 = P.sb(f"{pfx}m8", [128, NH, 8], F32)
        selm = P.sb(f"{pfx}selm", [128, NH, 16], F32)
        sq = P.sb(f"{pfx}sq", [128, NH, 64], F32)
    gb = P.sb(f"{pfx}gb", [128, D], F32)
    negm = P.sb(f"{pfx}negm", [128, 128], BF16)
    xt = [P.sb(f"{pfx}xt{i}", [128, D], F32) for i in range(2)]
    xn = P.sb(f"{pfx}xn", [128, D], BF16)
    junk = P.sb(f"{pfx}junk", [128, D], BF16)
    st = [P.sb(f"{pfx}st{j}", [128, 1], F32) for j in range(3)]
    st2 = [P.sb(f"{pfx}st2{j}", [128, 1], F32) for j in range(3)]
    hT = P.sb(f"{pfx}hT", [128, 8, 128], BF16)
    tmp = [P.sb(f"{pfx}tmp{i}", [128, NH, 16], F32) for i in range(4)]
    ssq = P.sb(f"{pfx}ssq", [128, NH], F32)
    aug = [P.sb(f"{pfx}aug{i}", [128, NH, 128], BF16) for i in range(2)]
    KT = [P.sb(f"{pfx}KT{t}", [128, NH, 128], BF16) for t in range(nt)]
    VP = [P.sb(f"{pfx}VP{t}", [128, NH, 65], BF16) for t in range(nt)]
    QT = P.sb(f"{pfx}QT", [128, NH, 512], BF16)
    PT = [P.sb(f"{pfx}PT{i}", [128, 512], BF16) for i in range(4)]
    o4 = [P.sb(f"{pfx}o{i}", [128, 512], F32) for i in range(4)]
    rc = [P.sb(f"{pfx}rc{i}", [128, 4], F32) for i in range(2)]

    if mla:
        for k in range(8):
            P.dma("pool", win[:, k, :], win_d[k * 128:(k + 1) * 128, 0:672], win, True)
        for j in range(3):
            P.dma("pool", wq[:, j, :], wq_d[j * 128:(j + 1) * 128, :], wq, True)
        for j in range(2):
            P.dma("pool", wkv[:, j, :], wkv_d[j * 128:(j + 1) * 128, :], wkv, True)
        P.dma("sp", qgb[:, :], qg_d.partition_broadcast(128), qgb, True)
        P.dma("sp", kvgb[:, :], kvg_d.partition_broadcast(128), kvgb, True)
    else:
        for k in range(8):
            P.dma("pool", win[:, k, :], win_d[k * 128:(k + 1) * 128, 672:2208], win, True)
        P.op("pool", lambda e: e.memset(KM[:, :, :], 0.0), writes=[KM])
        P.op("pool", lambda e: e.memset(onesc[:, :], 1.0 / BLK), writes=[onesc])
    P.dma("sp", gb[:, :], g_d.partition_broadcast(128), gb, True)
    P.dma("pool", negm[:, :], cst["negm"], negm, True)
    for a_ in aug:
        P.op("pool", lambda e, a_=a_: e.memset(a_[:, :, :], 0.0), writes=[a_])
    for t in range(nt):
        P.op("pool", lambda e, t=t: e.memset(VP[t][:, :, :], 1.0), writes=[VP[t]])

    def load(t, qpass):
        i = t % 2
        P.dma("sp", xt[i][:, :], x_in[t * 128:(t + 1) * 128, :], xt[i], True)
        if mla:
            if qpass:
                P.dma("sp", rqt[i][:, :, :, :], cst["rq"][t * 128:(t + 1) * 128, :].rearrange("p (a h f) -> p a h f", a=2, h=NH),
                      rqt[i], True)
            else:
                P.dma("sp", rkt[i][:, :, :], cst["rk"][t * 128:(t + 1) * 128, :].rearrange("p (a f) -> p a f", a=2), rkt[i], True)
        else:
            if qpass:
                P.dma("sp", qct[i][:, :, :], cst["qc"][t * 128:(t + 1) * 128, :].rearrange("p (h f) -> p h f", h=NH), qct[i], True)
            else:
                P.dma("sp", kct[i][:, :, :], cst["kc"][t * 128:(t + 1) * 128, :].rearrange("p (h f) -> p h f", h=NH), kct[i], True)

    def proj(w, col0, ncols, bank, nk=8, lhs=None):
        lhs = hT if lhs is None else lhs
        for k in range(nk):
            P.op("pe", lambda e, k=k: e.matmul(bank[:, :ncols], lhsT=lhs[:, k, :], rhs=w[:, k, col0:col0 + ncols],
                                               start=(k == 0), stop=(k == nk - 1)),
                 reads=[lhs, w], writes=[bank])

    def latent_norm(ncols, gbt, nchunk, bank):
        rms_rstd(P, C, latf, ncols, st2[0], st2[1], st2[2], junk)
        P.op("dve", lambda e: e.scalar_tensor_tensor(out=latn[:, :ncols], in0=latf[:, :ncols], scalar=st2[2][:, :],
                                                     in1=gbt[:, :ncols], op0=ALU.mult, op1=ALU.mult),
             reads=[latf, st2[2], gbt], writes=[latn])
        bv = bank.h[:, :].bitcast(BF16).rearrange("p (a b) -> p a b", b=128)
        for j in range(nchunk):
            P.op("pe", lambda e, j=j: e.transpose(bv[:, j, :], latn[:, j * 128:(j + 1) * 128], C.ident[:, :]),
                 reads=[latn, C.ident], writes=[bank])
        P.op("act", lambda e: e.copy(out=latT[:, 0:nchunk, :], in_=bv[:, 0:nchunk, :]), reads=[bank], writes=[latT])

    def rope(x1, x2, cs, sn, o1, o2, srct, tabt, dstt, small):
        t = [tt_[:, 0, :] if small else tt_[:, :, :] for tt_ in tmp]
        P.op("pool", lambda e: e.tensor_tensor(out=t[0], in0=x1, in1=cs, op=ALU.mult), reads=[srct, tabt], writes=[tmp[0]])
        P.op("pool", lambda e: e.tensor_tensor(out=t[1], in0=x2, in1=sn, op=ALU.mult), reads=[srct, tabt], writes=[tmp[1]])
        P.op("dve", lambda e: e.tensor_tensor(out=o1, in0=t[0], in1=t[1], op=ALU.subtract), reads=[tmp[0], tmp[1]], writes=[dstt])
        P.op("pool", lambda e: e.tensor_tensor(out=t[2], in0=x2, in1=cs, op=ALU.mult), reads=[srct, tabt], writes=[tmp[2]])
        P.op("pool", lambda e: e.tensor_tensor(out=t[3], in0=x1, in1=sn, op=ALU.mult), reads=[srct, tabt], writes=[tmp[3]])
        P.op("dve", lambda e: e.tensor_tensor(out=o2, in0=t[2], in1=t[3], op=ALU.add), reads=[tmp[2], tmp[3]], writes=[dstt])

    def pe_fence(bank, col, reads, covers=()):
        P.op("pe", lambda e: e.matmul(bank[:, col:col + 2], lhsT=C.ident[:, :], rhs=C.ident[:, 0:2], start=False, stop=True,
                                      skip_group_check=True),
             reads=[C.ident] + list(reads), writes=[bank] + list(covers))

    def transpose_aug(a_, dst_ap, dst_tile):
        bank = B[1]
        bv = bank.h[:, :].bitcast(BF16).rearrange("p (a b) -> p a b", b=128)
        for h in range(NH):
            P.op("pe", lambda e, h=h: e.transpose(bv[:, h, :], a_[:, h, :], C.ident[:, :]), reads=[a_, C.ident], writes=[bank])
        P.op("act", lambda e: e.copy(out=dst_ap, in_=bv[:, 0:8, :]), reads=[bank], writes=[dst_tile])

    load(0, False)
    for t in range(nt):
        i = t % 2
        x = xt[i]
        a_ = aug[i]
        norm_T(P, C, x, gb, xn, st, junk, B[0], hT)
        if t + 1 < nt:
            load(t + 1, False)
        if mla:
            b = B[2]
            proj(win, 384, 288, b)
            P.op("act", lambda e, b=b: e.copy(out=latf[:, 0:288], in_=b[:, 0:288]), reads=[b], writes=[latf])
            rope(latf[:, 256:272], latf[:, 272:288], rkt[i][:, 0, :], rkt[i][:, 1, :], krr[:, 0:16], krr[:, 16:32],
                 latf, rkt[i], krr, True)
            latent_norm(256, kvgb, 2, B[0])
            for hg in range(2):
                b = B[2 + hg]
                proj(wkv, hg * 512, 512, b, nk=2, lhs=latT)
                bv = b[:, :].rearrange("p (h f) -> p h f", h=4)
                P.op("act", lambda e, bv=bv, hg=hg, a_=a_: e.copy(out=a_[:, hg * 4:hg * 4 + 4, 0:64], in_=bv[:, :, 0:64]),
                     reads=[b], writes=[a_])
                P.op("dve", lambda e, bv=bv, hg=hg, t=t: e.tensor_copy(out=VP[t][:, hg * 4:hg * 4 + 4, 0:64], in_=bv[:, :, 64:128]),
                     reads=[b], writes=[VP[t]])
            P.op("dve", lambda e, a_=a_: e.tensor_copy(out=a_[:, :, 64:96], in_=krr[:, :].unsqueeze(1).to_broadcast([128, NH, 32])),
                 reads=[krr], writes=[a_])
            P.op("pool", lambda e, a_=a_: e.memset(a_[:, :, 96:97], 1.0), writes=[a_])
        else:
            b = B[2]
            proj(win, 512, 512, b)
            P.op("act", lambda e, b=b, a_=a_: e.copy(out=a_[:, :, 0:64], in_=b[:, :].rearrange("p (h f) -> p h f", h=NH)),
                 reads=[b], writes=[a_])
            P.op("dve", lambda e, b=b: e.tensor_copy(out=mf[:, :], in_=b[:, :]), reads=[b], writes=[mf])
            b = B[3]
            proj(win, 1024, 512, b)
            P.op("act", lambda e, b=b, t=t: e.copy(out=VP[t][:, :, 0:64], in_=b[:, :].rearrange("p (h f) -> p h f", h=NH)),
                 reads=[b], writes=[VP[t]])
            P.op("pool", lambda e, a_=a_, i=i: e.tensor_copy(out=a_[:, :, 64:96], in_=kct[i][:, :, :]), reads=[kct[i]], writes=[a_])
            kb = B[7]
            for p_ in range(4):
                P.op("pe", lambda e, p_=p_, t=t: e.matmul(kb[:, 2 * p_:2 * p_ + 2], lhsT=mf[:, p_ * 128:(p_ + 1) * 128], rhs=onesc[:, :],
                                                          start=(p_ == 0), stop=(p_ == 3),
                                                          skip_group_check=True),
                     reads=[mf, onesc], writes=[kb])
            pe_fence(kb, 256, [mf, onesc])
            j = t // 2
            kv_ = kb[:, 0:8].rearrange("p (a b) -> p a b", b=2)
            if t % 2 == 0:
                P.op("dve", lambda e, j=j, kv_=kv_: e.tensor_copy(out=KM[0:64, :, j:j + 1], in_=kv_[0:64, :, 0:1]),
                     reads=[kb], writes=[KM])
                P.op("dve", lambda e, j=j, kv_=kv_: e.tensor_copy(out=KM[64:128, :, 16 + j:17 + j], in_=kv_[64:128, :, 0:1]),
                     reads=[kb], writes=[KM])
            else:
                P.op("dve", lambda e, j=j, kv_=kv_: e.tensor_tensor(out=KM[0:64, :, j:j + 1], in0=KM[0:64, :, j:j + 1],
                                                                    in1=kv_[0:64, :, 0:1], op=ALU.add),
                     reads=[kb, KM], writes=[KM])
                P.op("dve", lambda e, j=j, kv_=kv_: e.tensor_tensor(out=KM[64:128, :, 16 + j:17 + j], in0=KM[64:128, :, 16 + j:17 + j],
                                                                    in1=kv_[64:128, :, 0:1], op=ALU.add),
                     reads=[kb, KM], writes=[KM])
        transpose_aug(a_, KT[t][:, :, :], KT[t])

    load(0, True)
    for qb in range(nqb):
        for tt in range(4):
            t = qb * 4 + tt
            i = t % 2
            x = xt[i]
            a_ = aug[i]
            norm_T(P, C, x, gb, xn, st, junk, B[0], hT)
            if t + 1 < nt:
                load(t + 1, True)
            if mla:
                b = B[2]
                proj(win, 0, 384, b)
                P.op("act", lambda e, b=b: e.copy(out=latf[:, 0:384], in_=b[:, 0:384]), reads=[b], writes=[latf])
                latent_norm(384, qgb, 3, B[0])
                for hg in range(2):
                    b = B[2 + hg]
                    proj(wq, hg * 384, 384, b, nk=3, lhs=latT)
                    P.op("act", lambda e, b=b, hg=hg: e.copy(out=qf[:, hg * 4:hg * 4 + 4, :],
                                                             in_=b[:, 0:384].rearrange("p (h f) -> p h f", h=4)),
                         reads=[b], writes=[qf])
                P.op("dve", lambda e, a_=a_: e.tensor_scalar(out=a_[:, :, 0:64], in0=qf[:, :, 0:64], scalar1=MLA_SCALE, scalar2=None,
                                                             op0=ALU.mult), reads=[qf], writes=[a_])
                rope(qf[:, :, 64:80], qf[:, :, 80:96], rqt[i][:, 0, :, :], rqt[i][:, 1, :, :], a_[:, :, 64:80], a_[:, :, 80:96],
                     qf, rqt[i], a_, False)
                P.op("pool", lambda e: e.tensor_tensor(out=sq[:, :, :], in0=qf[:, :, :], in1=qf[:, :, :], op=ALU.mult),
                     reads=[qf], writes=[sq])
                P.op("dve", lambda e: e.tensor_reduce(out=ssq[:, :], in_=sq[:, :, :], axis=AX.X, op=ALU.add), reads=[sq], writes=[ssq])
                P.op("dve", lambda e, a_=a_: e.tensor_scalar(out=a_[:, :, 96:97], in0=ssq[:, :].unsqueeze(2), scalar1=-MLA_SCALE / 2,
                                                             scalar2=None, op0=ALU.mult), reads=[ssq], writes=[a_])
            else:
                own = t // 2
                b = B[2]
                proj(win, 0, 512, b)
                P.op("act", lambda e, b=b: e.copy(out=mf[:, :], in_=b[:, :]), reads=[b], writes=[mf])
                mv3 = mf[:, :].rearrange("p (h f) -> p h f", h=NH)
                P.op("dve", lambda e, a_=a_: e.tensor_scalar(out=a_[:, :, 0:64], in0=mv3, scalar1=MOBA_SCALE, scalar2=None,
                                                             op0=ALU.mult), reads=[mf], writes=[a_])
                P.op("pool", lambda e: e.tensor_tensor(out=sq[:, :, :], in0=mv3, in1=mv3, op=ALU.mult), reads=[mf], writes=[sq])
                P.op("dve", lambda e: e.tensor_reduce(out=ssq[:, :], in_=sq[:, :, :], axis=AX.X, op=ALU.add), reads=[sq], writes=[ssq])
                P.op("dve", lambda e, a_=a_: e.tensor_scalar(out=a_[:, :, 84:85], in0=ssq[:, :].unsqueeze(2), scalar1=-MOBA_SCALE / 2,
                                                             scalar2=None, op0=ALU.mult), reads=[ssq], writes=[a_])
                P.op("pool", lambda e, a_=a_, i=i: e.tensor_copy(out=a_[:, :, 80:84], in_=qct[i][:, :, :]), reads=[qct[i]], writes=[a_])
                if own == 0 or n_sel == 0:
                    P.op("pool", lambda e: e.memset(selm[:, :, :], -1.0), writes=[selm])
                else:
                    bt = B[3]
                    btv = bt[:, :].rearrange("p (a b) -> p a b", b=128)
                    for p_ in range(4):
                        P.op("pe", lambda e, p_=p_: e.transpose(btv[:, p_, :], mf[:, p_ * 128:(p_ + 1) * 128], C.identf[:, :]),
                             reads=[mf, C.identf], writes=[bt])
                    pe_fence(B[2], 256, [mf, C.identf], covers=[bt])
                    P.op("act", lambda e: e.copy(out=mqT[:, :, :], in_=btv[:, 0:4, :]), reads=[bt], writes=[mqT])
                    bg = B[2]
                    for p_ in range(4):
                        P.op("pe", lambda e, p_=p_: e.matmul(bg[:, p_ * 32:(p_ + 1) * 32], lhsT=mqT[:, p_, :], rhs=KM[:, p_, :],
                                                             start=True, stop=True, skip_group_check=True),
                             reads=[mqT, KM], writes=[bg])
                    pe_fence(bg, 256, [mqT, KM])
                    P.op("dve", lambda e: e.tensor_copy(out=gs[:, :, :], in_=bg[:, 0:128].rearrange("p (h f) -> p h f", h=NH)),
                         reads=[bg], writes=[gs])
                    if own < 16:
                        P.op("dve", lambda e, own=own: e.memset(gs[:, :, own:16], -1e30), writes=[gs])
                    if own > n_sel:
                        for h in range(NH):
                            P.op("dve", lambda e, h=h: e.max(out=m8[:, h, :], in_=gs[:, h, :]), reads=[gs], writes=[m8])
                        for h in range(NH):
                            P.op("dve", lambda e, h=h: e.tensor_scalar(out=selm[:, h, :], in0=gs[:, h, :],
                                                                       scalar1=m8[:, h, n_sel - 1:n_sel], scalar2=1.0,
                                                                       op0=ALU.is_ge, op1=ALU.subtract),
                                 reads=[gs, m8], writes=[selm])
                    else:
                        P.op("dve", lambda e: e.tensor_scalar(out=selm[:, :, :], in0=gs[:, :, :], scalar1=-1e29, scalar2=1.0,
                                                              op0=ALU.is_ge, op1=ALU.subtract), reads=[gs], writes=[selm])
                P.op("dve", lambda e, own=own: e.memset(selm[:, :, own:own + 1], 0.0), writes=[selm])
                P.op("dve", lambda e, a_=a_: e.tensor_copy(out=a_[:, :, 64:80], in_=selm[:, :, :]), reads=[selm], writes=[a_])
            transpose_aug(a_, QT[:, :, tt * 128:(tt + 1) * 128], QT)
        nkt = 4 * (qb + 1)
        its = [(h, kt) for h in range(NH) for kt in range(nkt)]
        LOOK = 2
        scb = [B[3], B[4], B[5]]

        def emit_qk(i):
            h, kt = its[i]
            lo = max(0, kt - 4 * qb)
            sc = scb[i % 3]
            pt = PT[i % 4]
            diag = kt >= 4 * qb
            P.op("pe", lambda e: e.matmul(sc[:, lo * 128:512], lhsT=KT[kt][:, h, :], rhs=QT[:, h, lo * 128:512],
                                          start=True, stop=not diag),
                 reads=[KT[kt], QT], writes=[sc])
            if diag:
                P.op("pe", lambda e: e.matmul(sc[:, lo * 128:(lo + 1) * 128], lhsT=C.ident[:, :], rhs=negm[:, :],
                                              start=False, stop=True),
                     reads=[C.ident, negm], writes=[sc])
            P.op("act", lambda e: e.activation(out=pt[:, lo * 128:512], in_=sc[:, lo * 128:512], func=AF.Exp),
                 reads=[sc], writes=[pt])

        def emit_pv(i):
            h, kt = its[i]
            lo = max(0, kt - 4 * qb)
            pt = PT[i % 4]
            acc = B[6 + h % 2]
            accv = acc[:, :].rearrange("p (a b) -> p a b", b=128)
            for qt in range(lo, 4):
                P.op("pe", lambda e, qt=qt: e.matmul(
                    accv[:, qt, 0:65], lhsT=pt[:, qt * 128:(qt + 1) * 128], rhs=VP[kt][:, h, :],
                    start=(kt == 0 and qt == 0), stop=(kt == 4 * qb + qt), skip_group_check=True),
                    reads=[pt, VP[kt]], writes=[acc])
            if kt == nkt - 1:
                r_ = rc[h % 2]
                P.op("dve", lambda e: e.reciprocal(r_[:, :].unsqueeze(2), accv[:, :, 64:65]), reads=[acc], writes=[r_])
                for qt in range(4):
                    P.op("dve", lambda e, qt=qt: e.tensor_scalar(
                        out=o4[qt][:, h * 64:(h + 1) * 64], in0=accv[:, qt, 0:64], scalar1=r_[:, qt:qt + 1], scalar2=None,
                        op0=ALU.mult), reads=[acc, r_], writes=[o4[qt]])

        for i in range(len(its) + LOOK):
            if i < len(its):
                emit_qk(i)
            if i >= LOOK:
                emit_pv(i - LOOK)
        for tt in range(4):
            t = qb * 4 + tt
            P.dma("sp", out_d[t * 128:(t + 1) * 128, :], o4[tt][:, :], o4[tt], False)
    return o4


def about_stage(P, C, S, x_in, a_in, b_in, x_out, wout_d, pfx="ao"):
    nt = S // 128
    B = P.banks
    wo = P.sb(f"{pfx}wo", [128, 8, D], BF16)
    xt = [P.sb(f"{pfx}xt{i}", [128, D], F32) for i in range(3)]
    ab = [P.sb(f"{pfx}ab{i}", [128, 1024], F32) for i in range(2)]
    abn = P.sb(f"{pfx}abn", [128, 1024], BF16)
    abT = P.sb(f"{pfx}abT", [128, 8, 128], BF16)
    for k in range(8):
        P.dma("pool", wo[:, k, :], wout_d[k * 128:(k + 1) * 128, :], wo, True)

    def load(t):
        P.dma("sp", xt[t % 3][:, :], x_in[t * 128:(t + 1) * 128, :], xt[t % 3], True)
        P.dma("sp", ab[t % 2][:, 0:512], a_in[t * 128:(t + 1) * 128, :], ab[t % 2], True)
        P.dma("sp", ab[t % 2][:, 512:1024], b_in[t * 128:(t + 1) * 128, :], ab[t % 2], True)

    load(0)
    for t in range(nt):
        x = xt[t % 3]
        a_ = ab[t % 2]
        P.op("pool", lambda e, a_=a_: e.tensor_copy(out=abn[:, :], in_=a_[:, :]), reads=[a_], writes=[abn])
        if t + 1 < nt:
            load(t + 1)
        bank = B[t % 2]
        bv = bank.h[:, :].bitcast(BF16).rearrange("p (a b) -> p a b", b=128)
        for k in range(8):
            P.op("pe", lambda e, bv=bv, k=k: e.transpose(bv[:, k, :], abn[:, k * 128:(k + 1) * 128], C.ident[:, :]),
                 reads=[abn, C.ident], writes=[bank])
        P.op("act", lambda e, bv=bv: e.copy(out=abT[:, :, :], in_=bv[:, 0:8, :]), reads=[bank], writes=[abT])
        for dh in range(2):
            b = B[2 + (2 * t + dh) % 4]
            for k in range(8):
                P.op("pe", lambda e, b=b, k=k, dh=dh: e.matmul(b[:, :], lhsT=abT[:, k, :], rhs=wo[:, k, dh * 512:(dh + 1) * 512],
                                                               start=(k == 0), stop=(k == 7)),
                     reads=[abT, wo], writes=[b])
            P.op("dve", lambda e, b=b, dh=dh, x=x: e.tensor_tensor(out=x[:, dh * 512:(dh + 1) * 512],
                                                                   in0=x[:, dh * 512:(dh + 1) * 512], in1=b[:, :], op=ALU.add),
                 reads=[x, b], writes=[x])
        P.dma("sp", x_out[t * 128:(t + 1) * 128, :], x[:, :], x, False)
    return xt


from concourse.bass_utils import run_bass_kernel_spmd

SEQ_FULL = 4096
NCORES = 8
MODE = "fused"


def _consts(S):
    c1, cdec = ret_consts(S)
    c2 = ab_consts(S)
    c = {}
    c.update(c1)
    c.update(c2)
    c["identf"] = np.eye(128, dtype=np.float32)
    return c, cdec


class _Builder:
    def __init__(self):
        self.nc = bass.Bass("TRN2", target_bir_lowering=False)
        self.P = Prog(self.nc)
        self.P.make_banks()
        self.C = Common(self.P)
        self.ins = {}

    def din(self, name, shape):
        if name not in self.ins:
            self.ins[name] = self.nc.dram_tensor(name, list(shape), F32, kind="ExternalInput").ap()
        return self.ins[name]

    def dout(self, name, shape):
        return self.nc.dram_tensor(name, list(shape), F32, kind="ExternalOutput").ap()

    def scratch(self, name, shape):
        return self.nc.dram_tensor(name, list(shape), F32).ap()

    def cst(self, cnp):
        b = self

        class _Lazy(dict):
            def __missing__(self, k):
                v = b.din("c_" + k, cnp[k].shape)
                self[k] = v
                return v
        return _Lazy()

    def start(self, cst):
        self.C.setup(cst["identf"])
        self.P.dma_tiles_keep = list(self.P.dma_tiles)

    def finish(self):
        self.P.barrier()
        self.P.finalize()
        return self.nc


_CONST_CACHE = {}


def _get_consts(S):
    if S not in _CONST_CACHE:
        _CONST_CACHE[S] = _consts(S)
    return _CONST_CACHE[S]


def build_stage_prog(kind, S):
    cnp, cdec = _get_consts(S)
    b = _Builder()
    cst = b.cst(cnp)
    b.start(cst)
    P, C = b.P, b.C
    P.stage_begin()
    x = b.din("x", (S, D))
    if kind in ("mla", "moba"):
        o = b.dout("o", (S, 512))
        if kind == "mla":
            attn_stage(P, C, S, kind, x, o, b.din("win", (D, 2208)), b.din("g", (D,)), cst,
                       b.din("wq", (384, 768)), b.din("qg", (384,)), b.din("wkv", (256, 1024)), b.din("kvg", (256,)))
        else:
            attn_stage(P, C, S, kind, x, o, b.din("win", (D, 2208)), b.din("g", (D,)), cst)
    elif kind == "about":
        y = b.dout("y", (S, D))
        about_stage(P, C, S, x, b.din("a", (S, 512)), b.din("b", (S, 512)), y, b.din("wout", (1024, D)))
    elif kind == "retA":
        o = b.dout("on", (S, 2048))
        retA_stage(P, C, S, x, o, b.din("win", (D, 6144)), b.din("g", (D,)), cst, cdec)
    elif kind == "retB":
        y = b.dout("y", (S, D))
        retB_stage(P, C, S, x, b.din("onin", (S, 2048)), y, b.din("win", (D, 6144)), b.din("wout", (2048, D)),
                   b.din("g", (D,)), b.din("rn", (128, 16)))
    elif kind in ("mlp", "mlpf"):
        y = b.dout("y", (S, D))
        mlp_stage(P, C, S, x, y, b.din("wup", (D, FF)), b.din("wdn", (FF, D)), b.din("g", (D,)),
                  b.din("gf", (D,)) if kind == "mlpf" else None)
    P.stage_end()
    nc = b.finish()
    return nc, list(b.ins.keys())


def build_fused_prog(S, depth=4):
    cnp, cdec = _get_consts(S)
    b = _Builder()
    cst = b.cst(cnp)
    b.start(cst)
    P, C = b.P, b.C
    x = b.din("x", (S, D))
    y = b.dout("y", (S, D))
    n_even = (depth + 1) // 2
    n_odd = depth // 2
    norm_mix = b.din("norm_mix", (depth, D))
    norm_mlp = b.din("norm_mlp", (depth, D))
    wup = b.din("w_mlp_up", (depth, D, FF))
    wdn = b.din("w_mlp_down", (depth, FF, D))
    win_ab = b.din("w_in_ab", (n_even, D, 2208))
    qg = b.din("mla_q_norm", (n_even, 384))
    wq = b.din("mla_w_q_up", (n_even, 384, 768))
    kvg = b.din("mla_kv_norm", (n_even, 256))
    wkv = b.din("mla_w_kv_up", (n_even, 256, 1024))
    wout_ab = b.din("w_out_ab", (n_even, 1024, D))
    if n_odd:
        win_c = b.din("w_in_c", (n_odd, D, 6144))
        rn = b.din("ret_norm_l", (n_odd, 128, 16))
        wout_c = b.din("w_out_c", (n_odd, 2048, D))
    gf = b.din("norm_final", (D,))
    xs = [b.scratch("xs0", (S, D)), b.scratch("xs1", (S, D))]
    a_s = b.scratch("a_s", (S, 512))
    b_s = b.scratch("b_s", (S, 512))
    on_s = b.scratch("on_s", (S, 2048))
    cur = x
    for l in range(depth):
        j = l // 2
        mid = xs[0]
        if l % 2 == 0:
            P.stage_begin()
            attn_stage(P, C, S, "mla", cur, a_s, win_ab[j], norm_mix[l], cst, wq[j], qg[j], wkv[j], kvg[j], pfx=f"L{l}")
            P.stage_end()
            P.stage_begin()
            attn_stage(P, C, S, "moba", cur, b_s, win_ab[j], norm_mix[l], cst, pfx=f"L{l}")
            P.stage_end()
            P.stage_begin()
            about_stage(P, C, S, cur, a_s, b_s, mid, wout_ab[j], pfx=f"L{l}ao")
            P.stage_end()
        else:
            P.stage_begin()
            retA_stage(P, C, S, cur, on_s, win_c[j], norm_mix[l], cst, cdec, pfx=f"L{l}ra")
            P.stage_end()
            P.stage_begin()
            retB_stage(P, C, S, cur, on_s, mid, win_c[j], wout_c[j], norm_mix[l], rn[j], pfx=f"L{l}rb")
            P.stage_end()
        last = l == depth - 1
        dst = y if last else xs[1]
        P.stage_begin()
        mlp_stage(P, C, S, mid, dst, wup[l], wdn[l], norm_mlp[l], gf if last else None, pfx=f"L{l}m")
        P.stage_end()
        cur = dst
    nc = b.finish()
    return nc, list(b.ins.keys())


_PROGS = {}


def _prog(kind, S, depth=4):
    key = (kind, S, depth)
    if key not in _PROGS:
        _PROGS[key] = build_stage_prog(kind, S) if kind != "fused" else build_fused_prog(S, depth)
    return _PROGS[key]


def _launch(kind, S, per_core_inputs, shared, depth=4):
    nc, names = _prog(kind, S, depth)
    cnp, _ = _get_consts(S)
    in_maps = []
    for pc in per_core_inputs:
        m = {}
        for n in names:
            if n in pc:
                m[n] = pc[n]
            elif n in shared:
                m[n] = shared[n]
            elif n.startswith("c_"):
                m[n] = cnp[n[2:]]
            else:
                raise KeyError(n)
        in_maps.append(m)
    res = run_bass_kernel_spmd(nc, in_maps, core_ids=list(range(len(in_maps))))
    return res.results


def _f32(a):
    return np.ascontiguousarray(np.asarray(a, dtype=np.float32))


def forward(inputs, S, ncores, mode):
    x = _f32(inputs["x"])
    depth = inputs["norm_mix"].shape[0]
    W = {k: _f32(v) for k, v in inputs.items() if k != "x"}
    rnl = np.ascontiguousarray(W["ret_norm"].reshape(-1, 16, 128).transpose(0, 2, 1)) if "ret_norm" in W else None
    if mode == "fused":
        shared = dict(W)
        shared.pop("ret_norm", None)
        if rnl is not None:
            shared["ret_norm_l"] = rnl
        out = _launch("fused", S, [{"x": x[c]} for c in range(ncores)], shared, depth)
        return np.stack([o["y"] for o in out], axis=0)
    cur = [x[c] for c in range(ncores)]
    for l in range(depth):
        j = l // 2
        g = W["norm_mix"][l]
        if l % 2 == 0:
            sh = dict(win=W["w_in_ab"][j], g=g, wq=W["mla_w_q_up"][j], qg=W["mla_q_norm"][j],
                      wkv=W["mla_w_kv_up"][j], kvg=W["mla_kv_norm"][j])
            a = _launch("mla", S, [{"x": c} for c in cur], sh)
            bb = _launch("moba", S, [{"x": c} for c in cur], sh)
            r = _launch("about", S, [{"x": c, "a": a[i]["o"], "b": bb[i]["o"]} for i, c in enumerate(cur)],
                        dict(wout=W["w_out_ab"][j]))
        else:
            sh = dict(win=W["w_in_c"][j], g=g, wout=W["w_out_c"][j], rn=rnl[j])
            on = _launch("retA", S, [{"x": c} for c in cur], sh)
            r = _launch("retB", S, [{"x": c, "onin": on[i]["on"]} for i, c in enumerate(cur)], sh)
        cur = [o["y"] for o in r]
        last = l == depth - 1
        sh = dict(wup=W["w_mlp_up"][l], wdn=W["w_mlp_down"][l], g=W["norm_mlp"][l], gf=W["norm_final"])
        r = _launch("mlpf" if last else "mlp", S, [{"x": c} for c in cur], sh)
        cur = [o["y"] for o in r]
    return np.stack(cur, axis=0)


def kernel(**inputs):
    out = forward(inputs, SEQ_FULL, NCORES, MODE)
    return out.astype(np.float32)
```

```python
import numpy as np
import concourse.bass as bass
import concourse.mybir as mybir

F32 = mybir.dt.float32
BF16 = mybir.dt.bfloat16
ALU = mybir.AluOpType
AF = mybir.ActivationFunctionType
AX = mybir.AxisListType

ENGS = ("pe", "act", "dve", "pool", "sp")
ROT = 24000


class Tile:
    __slots__ = ("h", "name", "w", "rs", "sem", "dcnt", "psum", "last_dma")

    def __init__(self, h, name, psum=False):
        self.h = h
        self.psum = psum
        self.name = name
        self.w = None
        self.rs = {}
        self.sem = None
        self.dcnt = 0
        self.last_dma = None

    def __getitem__(self, k):
        return self.h[k]


class Op:
    __slots__ = ("eng", "fn", "waits", "needs_inc", "gcnt", "is_dma", "tile", "dma_val", "sem")

    def __init__(self, eng, fn):
        self.eng = eng
        self.fn = fn
        self.waits = []
        self.needs_inc = False
        self.gcnt = None
        self.is_dma = False
        self.tile = None
        self.dma_val = 0


class Prog:
    def __init__(self, nc):
        self.nc = nc
        self.ops = {e: [] for e in ENGS}
        self.nops = 0
        self.dma_tiles = []
        self.banks = None
        self.sem_pool = []
        self.mark = None
        import os
        self.limit = int(os.environ.get('OPLIMIT', '100000000'))

    def make_banks(self):
        self.banks = [self.ps(f"bank{i}", [128, 512], F32) for i in range(8)]

    def sb(self, name, shape, dtype):
        return Tile(self.nc.alloc_sbuf_tensor("sb_" + name, list(shape), dtype), name)

    def ps(self, name, shape, dtype=F32):
        return Tile(self.nc.alloc_psum_tensor("ps_" + name, list(shape), dtype), name, True)

    def _dep(self, o, d):
        if d is o or d is None:
            return
        if d.eng == "pe" and o.eng == "pe" and not d.is_dma and not o.is_dma:
            return
        if not d.is_dma:
            d.needs_inc = True
        o.waits.append(d)

    def op(self, eng, fn, reads=(), writes=()):
        if self.nops >= self.limit:
            return None
        o = Op(eng, fn)
        writes = list(writes) + [t for t in reads if t.psum and t not in writes]
        for t in reads:
            self._dep(o, t.w)
        for t in writes:
            self._dep(o, t.w)
            for r in t.rs.values():
                self._dep(o, r)
        for t in reads:
            t.rs[eng] = o
        for t in writes:
            t.w = o
            t.rs = {}
        self.ops[eng].append(o)
        self.nops += 1
        return o

    def dma(self, eng, out, in_, tile, write, extra_reads=(), extra_writes=()):
        if self.nops >= self.limit:
            return None
        o = Op(eng, lambda e: e.dma_start(out=out, in_=in_))
        o.is_dma = True
        o.tile = tile
        if tile.sem is None:
            if self.sem_pool:
                tile.sem, tile.dcnt = self.sem_pool.pop(0)
            else:
                tile.sem = self.nc.alloc_semaphore("d_" + tile.name)
            self.dma_tiles.append(tile)
        if not (write and tile.w is not None and tile.w.is_dma):
            self._dep(o, tile.w)
        if write:
            for r in tile.rs.values():
                self._dep(o, r)
        tile.dcnt += 1
        o.dma_val = 16 * tile.dcnt
        o.sem = tile.sem
        tile.last_dma = o
        if write:
            tile.w = o
            tile.rs = {}
        else:
            tile.rs["dma"] = o
        self.ops[eng].append(o)
        self.nops += 1
        return o

    def barrier(self):
        lasts = []
        for e in ENGS:
            for o in reversed(self.ops[e]):
                if not o.is_dma:
                    lasts.append(o)
                    break
        dl = [t.last_dma for t in self.dma_tiles if t.last_dma is not None]
        news = []
        for e in ENGS:
            o = Op(e, lambda eng: eng.nop())
            for d in lasts:
                if not (d.eng == "pe" and e == "pe"):
                    d.needs_inc = True
                    o.waits.append(d)
            for d in dl:
                o.waits.append(d)
            news.append(o)
        for o in news:
            self.ops[o.eng].append(o)
            self.nops += 1

    def stage_begin(self):
        self.mark = self.nc.sbuf_base
        self.dma_tiles = []

    def stage_end(self):
        self.barrier()
        for t in self.dma_tiles:
            self.sem_pool.append((t.sem, t.dcnt))
            t.sem = None
        self.dma_tiles = []
        self.nc.sbuf_base = self.mark

    def finalize(self, final_wait_tiles=()):
        nc = self.nc
        esems = {}
        for e in ENGS:
            c = 0
            for o in self.ops[e]:
                if o.needs_inc and not o.is_dma:
                    c += 1
                    o.gcnt = c
            nsem = (c + ROT - 1) // ROT
            esems[e] = [nc.alloc_semaphore(f"s_{e}{i}") for i in range(max(nsem, 1))]
        self.esems = esems
        final_waits = [(t.sem, 16 * t.dcnt) for t in final_wait_tiles if t.sem is not None]

        def emit(engobj, e):
            seen_eng = {}
            seen_sem = {}
            for o in self.ops[e]:
                for d in o.waits:
                    if d.is_dma:
                        s = d.sem
                        if seen_sem.get(s.num, 0) >= d.dma_val:
                            continue
                        seen_sem[s.num] = d.dma_val
                        engobj.wait_ge(s, d.dma_val)
                    else:
                        if seen_eng.get(d.eng, 0) >= d.gcnt:
                            continue
                        seen_eng[d.eng] = d.gcnt
                        k = (d.gcnt - 1) // ROT
                        engobj.wait_ge(esems[d.eng][k], d.gcnt - k * ROT)
                ins = o.fn(engobj)
                if o.is_dma:
                    ins.then_inc(o.sem, 16)
                elif o.needs_inc:
                    k = (o.gcnt - 1) // ROT
                    ins.then_inc(esems[e][k], 1)
            if e == "sp":
                for s, v in final_waits:
                    engobj.wait_ge(s, v)

        with nc.Block() as block:
            @block.tensor
            def _(eng):
                emit(eng, "pe")

            @block.scalar
            def _(eng):
                emit(eng, "act")

            @block.vector
            def _(eng):
                emit(eng, "dve")

            @block.gpsimd
            def _(eng):
                emit(eng, "pool")

            @block.sync
            def _(eng):
                emit(eng, "sp")

D = 1024
FF = 4096
EPS = 1e-6


class Common:
    def __init__(self, P):
        self.P = P
        nc = P.nc
        self.ident = P.sb("ident", [128, 128], BF16)
        self.eps = P.sb("eps", [128, 1], F32)
        self.identf = P.sb("identf32", [128, 128], F32)
        self._rr = 0

    def setup(self, ident_dram):
        P = self.P
        P.dma("pool", self.ident[:, :], ident_dram, self.ident, True)
        P.dma("sp", self.identf[:, :], ident_dram, self.identf, True)
        P.op("dve", lambda e: e.memset(self.eps[:, :], EPS), writes=[self.eps])


def rms_rstd(P, C, xt, ncols, ss, sq, rstd, junk):
    P.op("act", lambda e: e.activation(out=junk[:, :ncols], in_=xt[:, :ncols], func=AF.Square,
                                       accum_out=ss[:, :]),
         reads=[xt], writes=[junk, ss])
    P.op("act", lambda e: e.activation(out=sq[:, :], in_=ss[:, :], func=AF.Sqrt,
                                       bias=C.eps[:, :], scale=1.0 / ncols),
         reads=[ss, C.eps], writes=[sq])
    P.op("dve", lambda e: e.reciprocal(rstd[:, :], sq[:, :]), reads=[sq], writes=[rstd])


def mlp_stage(P, C, S, x_in, x_out, wup_d, wdn_d, g_d, gfin_d=None, pfx="m"):
    nc = P.nc
    TB = 256
    NT = TB // 128
    nblk = S // TB
    KC = D // 128
    FC = FF // 128

    wup = P.sb(f"{pfx}wup", [128, KC, FF], BF16)
    wdn = P.sb(f"{pfx}wdn", [128, FC, D], BF16)
    gb = P.sb(f"{pfx}gb", [128, D], F32)
    gfb = P.sb(f"{pfx}gfb", [128, D], F32) if gfin_d is not None else None
    NX = 4
    xt = [P.sb(f"{pfx}xt{i}", [128, D], F32) for i in range(NX)]
    xn = [P.sb(f"{pfx}xn{i}", [128, D], BF16) for i in range(2)]
    junk = P.sb(f"{pfx}junk", [128, D], BF16)
    st = [[P.sb(f"{pfx}st{i}_{j}", [128, 1], F32) for j in range(3)] for i in range(2)]
    hT = [P.sb(f"{pfx}hT{i}", [128, KC, TB], BF16) for i in range(2)]
    uT = [P.sb(f"{pfx}uT{f}", [128, TB], BF16) for f in range(FC)]
    rl = [P.sb(f"{pfx}rl{i}", [128, TB], F32) for i in range(4)]
    Bk = P.banks
    tp = [Bk[0], Bk[1]]
    up = [Bk[2], Bk[3], Bk[4]]
    dn = [Bk[5], Bk[6]]

    for k in range(KC):
        for c in range(0, FF, 2048):
            P.dma("pool", wup[:, k, c:c + 2048], wup_d[k * 128:(k + 1) * 128, c:c + 2048], wup, True)
    for f in range(FC):
        P.dma("pool", wdn[:, f, :], wdn_d[f * 128:(f + 1) * 128, :], wdn, True)
    P.dma("sp", gb[:, :], g_d.partition_broadcast(128), gb, True)
    if gfb is not None:
        P.dma("sp", gfb[:, :], gfin_d.partition_broadcast(128), gfb, True)

    def load_x(b, t):
        i = (b * NT + t) % NX
        r0 = b * TB + t * 128
        P.dma("sp", xt[i][:, :], x_in[r0:r0 + 128, :], xt[i], True)

    for t in range(NT):
        load_x(0, t)
    cnt = 0
    for b in range(nblk):
        h = hT[b % 2]
        for t in range(NT):
            i = (b * NT + t) % NX
            x = xt[i]
            s = st[t % 2]
            rms_rstd(P, C, x, D, s[0], s[1], s[2], junk)
            n = xn[t % 2]
            P.op("dve", lambda e, n=n, x=x, s=s: e.scalar_tensor_tensor(
                out=n[:, :], in0=x[:, :], scalar=s[2][:, :], in1=gb[:, :], op0=ALU.mult, op1=ALU.mult),
                reads=[x, s[2], gb], writes=[n])
            p = tp[t % 2]
            pv = p.h[:, :].bitcast(BF16).rearrange("p (a b) -> p a b", b=128)
            for k in range(KC):
                P.op("pe", lambda e, pv=pv, n=n, k=k: e.transpose(pv[:, k, :], n[:, k * 128:(k + 1) * 128],
                                                                   C.ident[:, :]),
                     reads=[n, C.ident], writes=[p])
            P.op("act", lambda e, h=h, pv=pv, t=t: e.copy(out=h[:, :, t * 128:(t + 1) * 128], in_=pv[:, 0:8, :]),
                 reads=[p], writes=[h])
        if b + 1 < nblk:
            for t in range(NT):
                load_x(b + 1, t)
        for f in range(FC):
            u = up[f % 3]
            for k in range(KC):
                P.op("pe", lambda e, u=u, k=k, f=f, h=h: e.matmul(
                    u[:, :TB], lhsT=wup[:, k, f * 128:(f + 1) * 128], rhs=h[:, k, :],
                    start=(k == 0), stop=(k == KC - 1)),
                    reads=[wup, h], writes=[u])
            r = rl[f % 4]
            if f % 2 == 0:
                P.op("act", lambda e, r=r, u=u: e.activation(out=r[:, :], in_=u[:, :TB], func=AF.Relu),
                     reads=[u], writes=[r])
            else:
                P.op("dve", lambda e, r=r, u=u: e.tensor_scalar(out=r[:, :], in0=u[:, :TB], scalar1=0.0,
                                                                 scalar2=None, op0=ALU.max),
                     reads=[u], writes=[r])
            P.op("pool", lambda e, r=r, f=f: e.tensor_tensor(out=uT[f][:, :], in0=r[:, :], in1=r[:, :],
                                                              op=ALU.mult),
                 reads=[r], writes=[uT[f]])
        for t in range(NT):
            i = (b * NT + t) % NX
            x = xt[i]
            for dh in range(2):
                d = dn[cnt % 2]
                cnt += 1
                for f in range(FC):
                    P.op("pe", lambda e, d=d, f=f, t=t, dh=dh: e.matmul(
                        d[:, :], lhsT=uT[f][:, t * 128:(t + 1) * 128], rhs=wdn[:, f, dh * 512:(dh + 1) * 512],
                        start=(f == 0), stop=(f == FC - 1)),
                        reads=[uT[f], wdn], writes=[d])
                P.op("dve", lambda e, d=d, x=x, dh=dh: e.tensor_tensor(
                    out=x[:, dh * 512:(dh + 1) * 512], in0=x[:, dh * 512:(dh + 1) * 512], in1=d[:, :],
                    op=ALU.add), reads=[x, d], writes=[x])
            if gfb is not None:
                s = st[t % 2]
                rms_rstd(P, C, x, D, s[0], s[1], s[2], junk)
                P.op("dve", lambda e, x=x, s=s: e.scalar_tensor_tensor(
                    out=x[:, :], in0=x[:, :], scalar=s[2][:, :], in1=gfb[:, :], op0=ALU.mult, op1=ALU.mult),
                    reads=[x, s[2], gfb], writes=[x])
            r0 = b * TB + t * 128
            P.dma("sp", x_out[r0:r0 + 128, :], x[:, :], x, False)
    return xt


RH = 4
RDK = 256
RDV = 512
CH = 128
LOG_GAMMA = np.log(1.0 - 2.0 ** (-5.0 - np.arange(RH))).astype(np.float32).astype(np.float64)


def ret_consts(S):
    pos = np.arange(S, dtype=np.float32)
    inv_freq = (1.0 / (10000.0 ** np.linspace(0.0, 1.0, RDK // 2, dtype=np.float32))).astype(np.float32)
    ang = pos[:, None] * inv_freq[None, :]
    cos, sin = np.cos(ang).astype(np.float64), np.sin(ang).astype(np.float64)
    p = (np.arange(S) % CH).astype(np.float64)
    qdec = np.exp(LOG_GAMMA[None, :] * (p[:, None] + 1.0))
    tq = np.zeros((S, 2, RH, 128), np.float32)
    tq[:, 0] = cos[:, None, :] * qdec[:, :, None]
    tq[:, 1] = sin[:, None, :] * qdec[:, :, None]
    tk = np.zeros((S, 2, 128), np.float32)
    tk[:, 0] = cos * RDK ** -0.5
    tk[:, 1] = sin * RDK ** -0.5
    pc = np.arange(CH, dtype=np.float64)
    kdec = np.exp(LOG_GAMMA[None, :] * (CH - 1.0 - pc[:, None])).astype(np.float32)
    dt = np.zeros((CH, RH, CH), np.float32)
    for h in range(RH):
        m = np.exp(-LOG_GAMMA[h] * (pc[:, None] + 1.0)) * (pc[:, None] <= pc[None, :])
        dt[:, h, :] = m
    cdec = [float(np.exp(LOG_GAMMA[h] * CH)) for h in range(RH)]
    return dict(tq=tq.reshape(S, 1024), tk=tk.reshape(S, 256), kdec=kdec, dt=dt.reshape(CH, RH * CH)), cdec


def norm_T(P, C, x, gb, xn, st, junk, bank, hT):
    rms_rstd(P, C, x, D, st[0], st[1], st[2], junk)
    P.op("dve", lambda e: e.scalar_tensor_tensor(out=xn[:, :], in0=x[:, :], scalar=st[2][:, :], in1=gb[:, :],
                                                 op0=ALU.mult, op1=ALU.mult),
         reads=[x, st[2], gb], writes=[xn])
    bv = bank.h[:, :].bitcast(BF16).rearrange("p (a b) -> p a b", b=128)
    for k in range(8):
        P.op("pe", lambda e, k=k: e.transpose(bv[:, k, :], xn[:, k * 128:(k + 1) * 128], C.ident[:, :]),
             reads=[xn, C.ident], writes=[bank])
    P.op("act", lambda e: e.copy(out=hT[:, :, :], in_=bv[:, 0:8, :]), reads=[bank], writes=[hT])


def retA_stage(P, C, S, x_in, on_out, win_d, g_d, cst, cdec, pfx="ra"):
    nch = S // CH
    B = P.banks
    w = P.sb(f"{pfx}w", [128, 8, 4096], BF16)
    gb = P.sb(f"{pfx}gb", [128, D], F32)
    dtt = P.sb(f"{pfx}dt", [128, RH, CH], F32)
    kdec = P.sb(f"{pfx}kdec", [128, RH], F32)
    xt = [P.sb(f"{pfx}xt{i}", [128, D], F32) for i in range(2)]
    tq = [P.sb(f"{pfx}tq{i}", [128, 2, RH, 128], F32) for i in range(2)]
    tk = [P.sb(f"{pfx}tk{i}", [128, 2, 128], F32) for i in range(2)]
    xn = P.sb(f"{pfx}xn", [128, D], BF16)
    junk = P.sb(f"{pfx}junk", [128, D], BF16)
    st = [P.sb(f"{pfx}st{j}", [128, 1], F32) for j in range(3)]
    hT = P.sb(f"{pfx}hT", [128, 8, 128], BF16)
    qs = P.sb(f"{pfx}qs", [128, RH, 2, 128], F32)
    ks = P.sb(f"{pfx}ks", [128, RH, 2, 128], F32)
    tmp = [P.sb(f"{pfx}tmp{i}", [128, RH, 128], F32) for i in range(4)]
    qd = P.sb(f"{pfx}qd", [128, RH, 2, 128], BF16)
    kr = P.sb(f"{pfx}kr", [128, RH, 2, 128], BF16)
    v = [P.sb(f"{pfx}v{h}", [128, RDV], BF16) for h in range(RH)]
    vd = [P.sb(f"{pfx}vd{h}", [128, RDV], BF16) for h in range(RH)]
    qdT = P.sb(f"{pfx}qdT", [128, 8, 128], BF16)
    kT = P.sb(f"{pfx}kT", [128, 8, 128], BF16)
    Sf = [P.sb(f"{pfx}S{h}", [128, 2, RDV], F32) for h in range(RH)]
    Sb = [P.sb(f"{pfx}Sb{h}", [128, 2, RDV], BF16) for h in range(RH)]
    PT = [P.sb(f"{pfx}PT{i}", [128, 128], BF16) for i in range(2)]
    gst = [[P.sb(f"{pfx}gs{i}_{j}", [128, 8], F32) for j in range(5)] for i in range(2)]
    on = [P.sb(f"{pfx}on{i}", [128, RH * RDV], F32) for i in range(2)]

    for k in range(8):
        for c0 in range(0, 4096, 2048):
            P.dma("pool", w[:, k, c0:c0 + 2048], win_d[k * 128:(k + 1) * 128, c0:c0 + 2048], w, True)
    P.dma("sp", gb[:, :], g_d.partition_broadcast(128), gb, True)
    P.dma("sp", dtt[:, :, :], cst["dt"].rearrange("p (h q) -> p h q", h=RH), dtt, True)
    P.dma("sp", kdec[:, :], cst["kdec"], kdec, True)

    def load(c):
        i = c % 2
        P.dma("sp", xt[i][:, :], x_in[c * CH:(c + 1) * CH, :], xt[i], True)
        P.dma("sp", tq[i][:, :, :, :], cst["tq"][c * CH:(c + 1) * CH, :].rearrange("p (a h f) -> p a h f", a=2, h=RH),
              tq[i], True)
        P.dma("sp", tk[i][:, :, :], cst["tk"][c * CH:(c + 1) * CH, :].rearrange("p (a f) -> p a f", a=2), tk[i], True)

    def proj(col0, ncols, bank):
        for k in range(8):
            P.op("pe", lambda e, k=k: e.matmul(bank[:, :ncols], lhsT=hT[:, k, :], rhs=w[:, k, col0:col0 + ncols],
                                               start=(k == 0), stop=(k == 7)),
                 reads=[hT, w], writes=[bank])

    def rotate(src, dst, cs, sn, tab):
        x1, x2 = src[:, :, 0, :], src[:, :, 1, :]
        P.op("pool", lambda e: e.tensor_tensor(out=tmp[0][:, :, :], in0=x1, in1=cs, op=ALU.mult), reads=[src, tab], writes=[tmp[0]])
        P.op("pool", lambda e: e.tensor_tensor(out=tmp[1][:, :, :], in0=x2, in1=sn, op=ALU.mult), reads=[src, tab], writes=[tmp[1]])
        P.op("dve", lambda e: e.tensor_tensor(out=dst[:, :, 0, :], in0=tmp[0][:, :, :], in1=tmp[1][:, :, :], op=ALU.subtract),
             reads=[tmp[0], tmp[1]], writes=[dst])
        P.op("pool", lambda e: e.tensor_tensor(out=tmp[2][:, :, :], in0=x2, in1=cs, op=ALU.mult), reads=[src, tab], writes=[tmp[2]])
        P.op("pool", lambda e: e.tensor_tensor(out=tmp[3][:, :, :], in0=x1, in1=sn, op=ALU.mult), reads=[src, tab], writes=[tmp[3]])
        P.op("dve", lambda e: e.tensor_tensor(out=dst[:, :, 1, :], in0=tmp[2][:, :, :], in1=tmp[3][:, :, :], op=ALU.add),
             reads=[tmp[2], tmp[3]], writes=[dst])

    load(0)
    for c in range(nch):
        i = c % 2
        x = xt[i]
        norm_T(P, C, x, gb, xn, st, junk, B[0], hT)
        if c + 1 < nch:
            load(c + 1)
        for g in range(2):
            b = B[2 + g]
            proj(g * 512, 512, b)
            P.op("act", lambda e, b=b, g=g: e.copy(out=qs[:, 2 * g:2 * g + 2, :, :],
                                                   in_=b[:, :].rearrange("p (h a f) -> p h a f", h=2, a=2)),
                 reads=[b], writes=[qs])
        rotate(qs, qd, tq[i][:, 0, :, :], tq[i][:, 1, :, :], tq[i])
        for g in range(2):
            b = B[2 + g]
            proj(1024 + g * 512, 512, b)
            P.op("act", lambda e, b=b, g=g: e.copy(out=ks[:, 2 * g:2 * g + 2, :, :],
                                                   in_=b[:, :].rearrange("p (h a f) -> p h a f", h=2, a=2)),
                 reads=[b], writes=[ks])
        rotate(ks, kr, tk[i][:, 0, :].unsqueeze(1).to_broadcast([128, RH, 128]),
               tk[i][:, 1, :].unsqueeze(1).to_broadcast([128, RH, 128]), tk[i])
        for h in range(RH):
            b = B[2 + h % 2]
            proj(2048 + h * 512, 512, b)
            P.op("act", lambda e, b=b, h=h: e.copy(out=v[h][:, :], in_=b[:, :]), reads=[b], writes=[v[h]])
            P.op("dve", lambda e, b=b, h=h: e.tensor_scalar(out=vd[h][:, :], in0=b[:, :], scalar1=kdec[:, h:h + 1],
                                                            scalar2=None, op0=ALU.mult),
                 reads=[b, kdec], writes=[vd[h]])
        for (src, dstT, bank) in ((qd, qdT, B[0]), (kr, kT, B[1])):
            bv = bank.h[:, :].bitcast(BF16).rearrange("p (a b) -> p a b", b=128)
            for h in range(RH):
                for a in range(2):
                    P.op("pe", lambda e, src=src, bv=bv, h=h, a=a: e.transpose(bv[:, 2 * h + a, :], src[:, h, a, :],
                                                                              C.ident[:, :]),
                         reads=[src, C.ident], writes=[bank])
            P.op("dve" if src is qd else "act",
                 (lambda e, dstT=dstT, bv=bv: e.tensor_copy(out=dstT[:, :, :], in_=bv[:, 0:8, :])) if src is qd else
                 (lambda e, dstT=dstT, bv=bv: e.copy(out=dstT[:, :, :], in_=bv[:, 0:8, :])),
                 reads=[bank], writes=[dstT])
        o_t = on[c % 2]
        for h in range(RH):
            sc = B[4]
            for a in range(2):
                P.op("pe", lambda e, h=h, a=a: e.matmul(sc[:, 0:128], lhsT=kT[:, 2 * h + a, :], rhs=qdT[:, 2 * h + a, :],
                                                        start=(a == 0), stop=(a == 1)),
                     reads=[kT, qdT], writes=[sc])
            pt = PT[h % 2]
            P.op("dve", lambda e, pt=pt, h=h: e.tensor_tensor(out=pt[:, :], in0=sc[:, 0:128], in1=dtt[:, h, :], op=ALU.mult),
                 reads=[sc, dtt], writes=[pt])
            ob = B[5]
            P.op("pe", lambda e, pt=pt, h=h: e.matmul(ob[:, :], lhsT=pt[:, :], rhs=v[h][:, :], start=True, stop=(c == 0)),
                 reads=[pt, v[h]], writes=[ob])
            if c > 0:
                for a in range(2):
                    P.op("pe", lambda e, h=h, a=a: e.matmul(ob[:, :], lhsT=qdT[:, 2 * h + a, :], rhs=Sb[h][:, a, :],
                                                            start=False, stop=(a == 1)),
                         reads=[qdT, Sb[h]], writes=[ob])
            for a in range(2):
                su = B[6 + a]
                P.op("pe", lambda e, su=su, h=h, a=a: e.matmul(su[:, :], lhsT=kr[:, h, a, :], rhs=vd[h][:, :],
                                                               start=True, stop=True),
                     reads=[kr, vd[h]], writes=[su])
                if c == 0:
                    P.op("dve", lambda e, su=su, h=h, a=a: e.tensor_copy(out=Sf[h][:, a, :], in_=su[:, :]),
                         reads=[su], writes=[Sf[h]])
                else:
                    P.op("dve", lambda e, su=su, h=h, a=a: e.scalar_tensor_tensor(
                        out=Sf[h][:, a, :], in0=Sf[h][:, a, :], scalar=cdec[h], in1=su[:, :], op0=ALU.mult, op1=ALU.add),
                        reads=[su, Sf[h]], writes=[Sf[h]])
            if c + 1 < nch:
                P.op("pool", lambda e, h=h: e.tensor_copy(out=Sb[h][:, :, :], in_=Sf[h][:, :, :]),
                     reads=[Sf[h]], writes=[Sb[h]])
            g5 = gst[h % 2]
            P.op("dve", lambda e, g5=g5: e.bn_stats(out=g5[0][:, 0:6], in_=ob[:, :]), reads=[ob], writes=[g5[0]])
            P.op("dve", lambda e, g5=g5: e.bn_aggr(out=g5[1][:, 0:2], in_=g5[0][:, 0:6]), reads=[g5[0]], writes=[g5[1]])
            P.op("act", lambda e, g5=g5: e.activation(out=g5[2][:, 0:1], in_=g5[1][:, 1:2], func=AF.Sqrt,
                                                      bias=C.eps[:, :], scale=1.0),
                 reads=[g5[1], C.eps], writes=[g5[2]])
            P.op("dve", lambda e, g5=g5: e.reciprocal(g5[3][:, 0:1], g5[2][:, 0:1]), reads=[g5[2]], writes=[g5[3]])
            P.op("dve", lambda e, g5=g5: e.tensor_scalar(out=g5[4][:, 0:1], in0=g5[1][:, 0:1], scalar1=g5[3][:, 0:1],
                                                         scalar2=-1.0, op0=ALU.mult, op1=ALU.mult),
                 reads=[g5[1], g5[3]], writes=[g5[4]])
            P.op("act", lambda e, g5=g5, h=h, o_t=o_t: e.activation(
                out=o_t[:, h * RDV:(h + 1) * RDV], in_=ob[:, :], func=AF.Identity, bias=g5[4][:, 0:1], scale=g5[3][:, 0:1]),
                reads=[ob, g5[3], g5[4]], writes=[o_t])
        P.dma("sp", on_out[c * CH:(c + 1) * CH, :], o_t[:, :], o_t, False)
    return on


def retB_stage(P, C, S, x_in, on_in, x_out, win_d, wout_d, g_d, rng_d, pfx="rb"):
    nt = S // 128
    B = P.banks
    wg = P.sb(f"{pfx}wg", [128, 8, 2048], BF16)
    wo = P.sb(f"{pfx}wo", [128, 16, D], BF16)
    stg = [P.sb(f"{pfx}stg{i}", [128, D], F32) for i in range(2)]
    rng = P.sb(f"{pfx}rng", [128, 16], F32)
    gb = P.sb(f"{pfx}gb", [128, D], F32)
    xt = [P.sb(f"{pfx}xt{i}", [128, D], F32) for i in range(3)]
    ont = [P.sb(f"{pfx}on{i}", [128, 2048], F32) for i in range(2)]
    xn = P.sb(f"{pfx}xn", [128, D], BF16)
    junk = P.sb(f"{pfx}junk", [128, D], BF16)
    st = [P.sb(f"{pfx}st{j}", [128, 1], F32) for j in range(3)]
    hT = P.sb(f"{pfx}hT", [128, 8, 128], BF16)
    sg = [P.sb(f"{pfx}sg{i}", [128, 512], F32) for i in range(2)]
    og = P.sb(f"{pfx}og", [128, 2048], BF16)
    ogT = P.sb(f"{pfx}ogT", [128, 16, 128], BF16)

    for k in range(8):
        P.dma("pool", wg[:, k, :], win_d[k * 128:(k + 1) * 128, 4096:6144], wg, True)
    P.dma("sp", gb[:, :], g_d.partition_broadcast(128), gb, True)
    P.dma("sp", rng[:, :], rng_d, rng, True)
    for kc in range(16):
        s_ = stg[kc % 2]
        P.dma("sp", s_[:, :], wout_d[kc * 128:(kc + 1) * 128, :], s_, True)
        P.op("pool", lambda e, s_=s_, kc=kc: e.tensor_scalar(out=wo[:, kc, :], in0=s_[:, :], scalar1=rng[:, kc:kc + 1],
                                                             scalar2=None, op0=ALU.mult),
             reads=[s_, rng], writes=[wo])

    def load(t):
        P.dma("sp", xt[t % 3][:, :], x_in[t * 128:(t + 1) * 128, :], xt[t % 3], True)
        P.dma("sp", ont[t % 2][:, :], on_in[t * 128:(t + 1) * 128, :], ont[t % 2], True)

    load(0)
    for t in range(nt):
        x = xt[t % 3]
        o_ = ont[t % 2]
        norm_T(P, C, x, gb, xn, st, junk, B[0], hT)
        if t + 1 < nt:
            load(t + 1)
        for g in range(4):
            b = B[2 + g % 2]
            for k in range(8):
                P.op("pe", lambda e, k=k, g=g, b=b: e.matmul(b[:, :], lhsT=hT[:, k, :], rhs=wg[:, k, g * 512:(g + 1) * 512],
                                                             start=(k == 0), stop=(k == 7)),
                     reads=[hT, wg], writes=[b])
            s_ = sg[g % 2]
            P.op("act", lambda e, s_=s_, b=b: e.activation(out=s_[:, :], in_=b[:, :], func=AF.Silu), reads=[b], writes=[s_])
            P.op("dve", lambda e, s_=s_, g=g, o_=o_: e.tensor_tensor(out=og[:, g * 512:(g + 1) * 512], in0=s_[:, :],
                                                                     in1=o_[:, g * 512:(g + 1) * 512], op=ALU.mult),
                 reads=[s_, o_], writes=[og])
        for half in range(2):
            bank = B[half]
            bv = bank.h[:, :].bitcast(BF16).rearrange("p (a b) -> p a b", b=128)
            for k in range(8):
                kc = half * 8 + k
                P.op("pe", lambda e, bv=bv, k=k, kc=kc: e.transpose(bv[:, k, :], og[:, kc * 128:(kc + 1) * 128], C.ident[:, :]),
                     reads=[og, C.ident], writes=[bank])
            P.op("act" if half == 0 else "dve",
                 (lambda e, bv=bv, half=half: e.copy(out=ogT[:, half * 8:half * 8 + 8, :], in_=bv[:, 0:8, :])) if half == 0 else
                 (lambda e, bv=bv, half=half: e.tensor_copy(out=ogT[:, half * 8:half * 8 + 8, :], in_=bv[:, 0:8, :])),
                 reads=[bank], writes=[ogT])
        for dh in range(2):
            b = B[4 + dh]
            for kc in range(16):
                P.op("pe", lambda e, b=b, kc=kc, dh=dh: e.matmul(b[:, :], lhsT=ogT[:, kc, :], rhs=wo[:, kc, dh * 512:(dh + 1) * 512],
                                                                 start=(kc == 0), stop=(kc == 15)),
                     reads=[ogT, wo], writes=[b])
            P.op("dve", lambda e, b=b, dh=dh, x=x: e.tensor_tensor(out=x[:, dh * 512:(dh + 1) * 512],
                                                                   in0=x[:, dh * 512:(dh + 1) * 512], in1=b[:, :], op=ALU.add),
                 reads=[x, b], writes=[x])
        P.dma("sp", x_out[t * 128:(t + 1) * 128, :], x[:, :], x, False)
    return xt


NH = 8
MLA_SCALE = 96 ** -0.5
MOBA_SCALE = 0.125
BLK = 256
NEGV = 30000.0


def ab_consts(S):
    pos = np.arange(S, dtype=np.float32)
    inv_freq = (1.0 / (10000.0 ** (np.arange(0, 32, 2, dtype=np.float32) / 32))).astype(np.float32)
    ang = pos[:, None] * inv_freq[None, :]
    cos, sin = np.cos(ang), np.sin(ang)
    rq = np.zeros((S, 2, NH, 16), np.float32)
    rq[:, 0] = (cos * MLA_SCALE)[:, None, :]
    rq[:, 1] = (sin * MLA_SCALE)[:, None, :]
    rk = np.zeros((S, 2, 16), np.float32)
    rk[:, 0] = cos
    rk[:, 1] = sin
    pc = np.arange(128)
    negm = np.where(pc[:, None] > pc[None, :], -NEGV, 0.0).astype(np.float32)
    slopes = np.array([2.0 ** (-(h + 1)) for h in range(NH)], np.float64)
    p = np.arange(S)
    blk = p // BLK
    r = p % BLK
    kc = np.zeros((S, NH, 32), np.float32)
    kc[p, :, blk] = NEGV
    kc[:, :, 16] = 1.0
    kc[:, :, 17] = 1.0
    kc[:, :, 18] = slopes[None, :] * 256.0 * blk[:, None]
    kc[:, :, 19] = slopes[None, :] * r[:, None]
    kc[:, :, 20] = 1.0
    qc = np.zeros((S, NH, 4), np.float32)
    qc[:, :, 0] = -slopes[None, :] * 256.0 * blk[:, None]
    qc[:, :, 1] = -slopes[None, :] * r[:, None]
    qc[:, :, 2] = 1.0
    qc[:, :, 3] = 1.0
    return dict(rq=rq.reshape(S, 256), rk=rk.reshape(S, 32), negm=negm, kc=kc.reshape(S, 256), qc=qc.reshape(S, 32))


def attn_stage(P, C, S, kind, x_in, out_d, win_d, g_d, cst, wq_d=None, qg_d=None, wkv_d=None, kvg_d=None, pfx="at"):
    mla = kind == "mla"
    nt = S // 128
    nqb = S // 512
    nblk = S // BLK
    n_sel = min(3, nblk - 1)
    B = P.banks
    A = "a" if mla else "b"
    pfx = pfx + A

    if mla:
        win = P.sb(f"{pfx}win", [128, 8, 672], BF16)
        wq = P.sb(f"{pfx}wq", [128, 3, 768], BF16)
        wkv = P.sb(f"{pfx}wkv", [128, 2, 1024], BF16)
        qgb = P.sb(f"{pfx}qgb", [128, 384], F32)
        kvgb = P.sb(f"{pfx}kvgb", [128, 256], F32)
        rqt = [P.sb(f"{pfx}rq{i}", [128, 2, NH, 16], F32) for i in range(2)]
        rkt = [P.sb(f"{pfx}rk{i}", [128, 2, 16], F32) for i in range(2)]
        latf = P.sb(f"{pfx}latf", [128, 384], F32)
        latn = P.sb(f"{pfx}latn", [128, 384], BF16)
        latT = P.sb(f"{pfx}latT", [128, 3, 128], BF16)
        qf = P.sb(f"{pfx}qf", [128, NH, 96], F32)
        krr = P.sb(f"{pfx}krr", [128, 32], F32)
        sq = P.sb(f"{pfx}sq", [128, NH, 96], F32)
    else:
        win = P.sb(f"{pfx}win", [128, 8, 1536], BF16)
        kct = [P.sb(f"{pfx}kc{i}", [128, NH, 32], F32) for i in range(2)]
        qct = [P.sb(f"{pfx}qc{i}", [128, NH, 4], F32) for i in range(2)]
        mf = P.sb(f"{pfx}mf", [128, 512], F32)
        mqT = P.sb(f"{pfx}mqT", [128, 4, 128], F32)
        KM = P.sb(f"{pfx}KM", [128, 4, 32], F32)
        onesc = P.sb(f"{pfx}ones", [128, 2], F32)
        gs = P.sb(f"{pfx}gs", [128, NH, 16], F32)
        m8 = P.sb(f"{pfx}m8", [128, NH, 8], F32)
        selm = P.sb(f"{pfx}selm", [128, NH, 16], F32)
        sq = P.sb(f"{pfx}sq", [128, NH, 64], F32)
    gb = P.sb(f"{pfx}gb", [128, D], F32)
    negm = P.sb(f"{pfx}negm", [128, 128], BF16)
    xt = [P.sb(f"{pfx}xt{i}", [128, D], F32) for i in range(2)]
    xn = P.sb(f"{pfx}xn", [128, D], BF16)
    junk = P.sb(f"{pfx}junk", [128, D], BF16)
    st = [P.sb(f"{pfx}st{j}", [128, 1], F32) for j in range(3)]
    st2 = [P.sb(f"{pfx}st2{j}", [128, 1], F32) for j in range(3)]
    hT = P.sb(f"{pfx}hT", [128, 8, 128], BF16)
    tmp = [P.sb(f"{pfx}tmp{i}", [128, NH, 16], F32) for i in range(4)]
    ssq = P.sb(f"{pfx}ssq", [128, NH], F32)
    aug = [P.sb(f"{pfx}aug{i}", [128, NH, 128], BF16) for i in range(2)]
    KT = [P.sb(f"{pfx}KT{t}", [128, NH, 128], BF16) for t in range(nt)]
    VP = [P.sb(f"{pfx}VP{t}", [128, NH, 65], BF16) for t in range(nt)]
    QT = P.sb(f"{pfx}QT", [128, NH, 512], BF16)
    PT = [P.sb(f"{pfx}PT{i}", [128, 512], BF16) for i in range(4)]
    o4 = [P.sb(f"{pfx}o{i}", [128, 512], F32) for i in range(4)]
    rc = [P.sb(f"{pfx}rc{i}", [128, 4], F32) for i in range(2)]

    if mla:
        for k in range(8):
            P.dma("pool", win[:, k, :], win_d[k * 128:(k + 1) * 128, 0:672], win, True)
        for j in range(3):
            P.dma("pool", wq[:, j, :], wq_d[j * 128:(j + 1) * 128, :], wq, True)
        for j in range(2):
            P.dma("pool", wkv[:, j, :], wkv_d[j * 128:(j + 1) * 128, :], wkv, True)
        P.dma("sp", qgb[:, :], qg_d.partition_broadcast(128), qgb, True)
        P.dma("sp", kvgb[:, :], kvg_d.partition_broadcast(128), kvgb, True)
    else:
        for k in range(8):
            P.dma("pool", win[:, k, :], win_d[k * 128:(k + 1) * 128, 672:2208], win, True)
        P.op("pool", lambda e: e.memset(KM[:, :, :], 0.0), writes=[KM])
        P.op("pool", lambda e: e.memset(onesc[:, :], 1.0 / BLK), writes=[onesc])
    P.dma("sp", gb[:, :], g_d.partition_broadcast(128), gb, True)
    P.dma("pool", negm[:, :], cst["negm"], negm, True)
    for a_ in aug:
        P.op("pool", lambda e, a_=a_: e.memset(a_[:, :, :], 0.0), writes=[a_])
    for t in range(nt):
        P.op("pool", lambda e, t=t: e.memset(VP[t][:, :, :], 1.0), writes=[VP[t]])

    def load(t, qpass):
        i = t % 2
        P.dma("sp", xt[i][:, :], x_in[t * 128:(t + 1) * 128, :], xt[i], True)
        if mla:
            if qpass:
                P.dma("sp", rqt[i][:, :, :, :], cst["rq"][t * 128:(t + 1) * 128, :].rearrange("p (a h f) -> p a h f", a=2, h=NH),
                      rqt[i], True)
            else:
                P.dma("sp", rkt[i][:, :, :], cst["rk"][t * 128:(t + 1) * 128, :].rearrange("p (a f) -> p a f", a=2), rkt[i], True)
        else:
            if qpass:
                P.dma("sp", qct[i][:, :, :], cst["qc"][t * 128:(t + 1) * 128, :].rearrange("p (h f) -> p h f", h=NH), qct[i], True)
            else:
                P.dma("sp", kct[i][:, :, :], cst["kc"][t * 128:(t + 1) * 128, :].rearrange("p (h f) -> p h f", h=NH), kct[i], True)

    def proj(w, col0, ncols, bank, nk=8, lhs=None):
        lhs = hT if lhs is None else lhs
        for k in range(nk):
            P.op("pe", lambda e, k=k: e.matmul(bank[:, :ncols], lhsT=lhs[:, k, :], rhs=w[:, k, col0:col0 + ncols],
                                               start=(k == 0), stop=(k == nk - 1)),
                 reads=[lhs, w], writes=[bank])

    def latent_norm(ncols, gbt, nchunk, bank):
        rms_rstd(P, C, latf, ncols, st2[0], st2[1], st2[2], junk)
        P.op("dve", lambda e: e.scalar_tensor_tensor(out=latn[:, :ncols], in0=latf[:, :ncols], scalar=st2[2][:, :],
                                                     in1=gbt[:, :ncols], op0=ALU.mult, op1=ALU.mult),
             reads=[latf, st2[2], gbt], writes=[latn])
        bv = bank.h[:, :].bitcast(BF16).rearrange("p (a b) -> p a b", b=128)
        for j in range(nchunk):
            P.op("pe", lambda e, j=j: e.transpose(bv[:, j, :], latn[:, j * 128:(j + 1) * 128], C.ident[:, :]),
                 reads=[latn, C.ident], writes=[bank])
        P.op("act", lambda e: e.copy(out=latT[:, 0:nchunk, :], in_=bv[:, 0:nchunk, :]), reads=[bank], writes=[latT])

    def rope(x1, x2, cs, sn, o1, o2, srct, tabt, dstt, small):
        t = [tt_[:, 0, :] if small else tt_[:, :, :] for tt_ in tmp]
        P.op("pool", lambda e: e.tensor_tensor(out=t[0], in0=x1, in1=cs, op=ALU.mult), reads=[srct, tabt], writes=[tmp[0]])
        P.op("pool", lambda e: e.tensor_tensor(out=t[1], in0=x2, in1=sn, op=ALU.mult), reads=[srct, tabt], writes=[tmp[1]])
        P.op("dve", lambda e: e.tensor_tensor(out=o1, in0=t[0], in1=t[1], op=ALU.subtract), reads=[tmp[0], tmp[1]], writes=[dstt])
        P.op("pool", lambda e: e.tensor_tensor(out=t[2], in0=x2, in1=cs, op=ALU.mult), reads=[srct, tabt], writes=[tmp[2]])
        P.op("pool", lambda e: e.tensor_tensor(out=t[3], in0=x1, in1=sn, op=ALU.mult), reads=[srct, tabt], writes=[tmp[3]])
        P.op("dve", lambda e: e.tensor_tensor(out=o2, in0=t[2], in1=t[3], op=ALU.add), reads=[tmp[2], tmp[3]], writes=[dstt])

    def pe_fence(bank, col, reads, covers=()):
        P.op("pe", lambda e: e.matmul(bank[:, col:col + 2], lhsT=C.ident[:, :], rhs=C.ident[:, 0:2], start=False, stop=True,
                                      skip_group_check=True),
             reads=[C.ident] + list(reads), writes=[bank] + list(covers))

    def transpose_aug(a_, dst_ap, dst_tile):
        bank = B[1]
        bv = bank.h[:, :].bitcast(BF16).rearrange("p (a b) -> p a b", b=128)
        for h in range(NH):
            P.op("pe", lambda e, h=h: e.transpose(bv[:, h, :], a_[:, h, :], C.ident[:, :]), reads=[a_, C.ident], writes=[bank])
        P.op("act", lambda e: e.copy(out=dst_ap, in_=bv[:, 0:8, :]), reads=[bank], writes=[dst_tile])

    load(0, False)
    for t in range(nt):
        i = t % 2
        x = xt[i]
        a_ = aug[i]
        norm_T(P, C, x, gb, xn, st, junk, B[0], hT)
        if t + 1 < nt:
            load(t + 1, False)
        if mla:
            b = B[2]
            proj(win, 384, 288, b)
            P.op("act", lambda e, b=b: e.copy(out=latf[:, 0:288], in_=b[:, 0:288]), reads=[b], writes=[latf])
            rope(latf[:, 256:272], latf[:, 272:288], rkt[i][:, 0, :], rkt[i][:, 1, :], krr[:, 0:16], krr[:, 16:32],
                 latf, rkt[i], krr, True)
            latent_norm(256, kvgb, 2, B[0])
            for hg in range(2):
                b = B[2 + hg]
                proj(wkv, hg * 512, 512, b, nk=2, lhs=latT)
                bv = b[:, :].rearrange("p (h f) -> p h f", h=4)
                P.op("act", lambda e, bv=bv, hg=hg, a_=a_: e.copy(out=a_[:, hg * 4:hg * 4 + 4, 0:64], in_=bv[:, :, 0:64]),
                     reads=[b], writes=[a_])
                P.op("dve", lambda e, bv=bv, hg=hg, t=t: e.tensor_copy(out=VP[t][:, hg * 4:hg * 4 + 4, 0:64], in_=bv[:, :, 64:128]),
                     reads=[b], writes=[VP[t]])
            P.op("dve", lambda e, a_=a_: e.tensor_copy(out=a_[:, :, 64:96], in_=krr[:, :].unsqueeze(1).to_broadcast([128, NH, 32])),
                 reads=[krr], writes=[a_])
            P.op("pool", lambda e, a_=a_: e.memset(a_[:, :, 96:97], 1.0), writes=[a_])
        else:
            b = B[2]
            proj(win, 512, 512, b)
            P.op("act", lambda e, b=b, a_=a_: e.copy(out=a_[:, :, 0:64], in_=b[:, :].rearrange("p (h f) -> p h f", h=NH)),
                 reads=[b], writes=[a_])
            P.op("dve", lambda e, b=b: e.tensor_copy(out=mf[:, :], in_=b[:, :]), reads=[b], writes=[mf])
            b = B[3]
            proj(win, 1024, 512, b)
            P.op("act", lambda e, b=b, t=t: e.copy(out=VP[t][:, :, 0:64], in_=b[:, :].rearrange("p (h f) -> p h f", h=NH)),
                 reads=[b], writes=[VP[t]])
            P.op("pool", lambda e, a_=a_, i=i: e.tensor_copy(out=a_[:, :, 64:96], in_=kct[i][:, :, :]), reads=[kct[i]], writes=[a_])
            kb = B[7]
            for p_ in range(4):
                P.op("pe", lambda e, p_=p_, t=t: e.matmul(kb[:, 2 * p_:2 * p_ + 2], lhsT=mf[:, p_ * 128:(p_ + 1) * 128], rhs=onesc[:, :],
                                                          start=(p_ == 0), stop=(p_ == 3),
                                                          skip_group_check=True),
                     reads=[mf, onesc], writes=[kb])
            pe_fence(kb, 256, [mf, onesc])
            j = t // 2
            kv_ = kb[:, 0:8].rearrange("p (a b) -> p a b", b=2)
            if t % 2 == 0:
                P.op("dve", lambda e, j=j, kv_=kv_: e.tensor_copy(out=KM[0:64, :, j:j + 1], in_=kv_[0:64, :, 0:1]),
                     reads=[kb], writes=[KM])
                P.op("dve", lambda e, j=j, kv_=kv_: e.tensor_copy(out=KM[64:128, :, 16 + j:17 + j], in_=kv_[64:128, :, 0:1]),
                     reads=[kb], writes=[KM])
            else:
                P.op("dve", lambda e, j=j, kv_=kv_: e.tensor_tensor(out=KM[0:64, :, j:j + 1], in0=KM[0:64, :, j:j + 1],
                                                                    in1=kv_[0:64, :, 0:1], op=ALU.add),
                     reads=[kb, KM], writes=[KM])
                P.op("dve", lambda e, j=j, kv_=kv_: e.tensor_tensor(out=KM[64:128, :, 16 + j:17 + j], in0=KM[64:128, :, 16 + j:17 + j],
                                                                    in1=kv_[64:128, :, 0:1], op=ALU.add),
                     reads=[kb, KM], writes=[KM])
        transpose_aug(a_, KT[t][:, :, :], KT[t])

    load(0, True)
    for qb in range(nqb):
        for tt in range(4):
            t = qb * 4 + tt
            i = t % 2
            x = xt[i]
            a_ = aug[i]
            norm_T(P, C, x, gb, xn, st, junk, B[0], hT)
            if t + 1 < nt:
                load(t + 1, True)
            if mla:
                b = B[2]
                proj(win, 0, 384, b)
                P.op("act", lambda e, b=b: e.copy(out=latf[:, 0:384], in_=b[:, 0:384]), reads=[b], writes=[latf])
                latent_norm(384, qgb, 3, B[0])
                for hg in range(2):
                    b = B[2 + hg]
                    proj(wq, hg * 384, 384, b, nk=3, lhs=latT)
                    P.op("act", lambda e, b=b, hg=hg: e.copy(out=qf[:, hg * 4:hg * 4 + 4, :],
                                                             in_=b[:, 0:384].rearrange("p (h f) -> p h f", h=4)),
                         reads=[b], writes=[qf])
                P.op("dve", lambda e, a_=a_: e.tensor_scalar(out=a_[:, :, 0:64], in0=qf[:, :, 0:64], scalar1=MLA_SCALE, scalar2=None,
                                                             op0=ALU.mult), reads=[qf], writes=[a_])
                rope(qf[:, :, 64:80], qf[:, :, 80:96], rqt[i][:, 0, :, :], rqt[i][:, 1, :, :], a_[:, :, 64:80], a_[:, :, 80:96],
                     qf, rqt[i], a_, False)
                P.op("pool", lambda e: e.tensor_tensor(out=sq[:, :, :], in0=qf[:, :, :], in1=qf[:, :, :], op=ALU.mult),
                     reads=[qf], writes=[sq])
                P.op("dve", lambda e: e.tensor_reduce(out=ssq[:, :], in_=sq[:, :, :], axis=AX.X, op=ALU.add), reads=[sq], writes=[ssq])
                P.op("dve", lambda e, a_=a_: e.tensor_scalar(out=a_[:, :, 96:97], in0=ssq[:, :].unsqueeze(2), scalar1=-MLA_SCALE / 2,
                                                             scalar2=None, op0=ALU.mult), reads=[ssq], writes=[a_])
            else:
                own = t // 2
                b = B[2]
                proj(win, 0, 512, b)
                P.op("act", lambda e, b=b: e.copy(out=mf[:, :], in_=b[:, :]), reads=[b], writes=[mf])
                mv3 = mf[:, :].rearrange("p (h f) -> p h f", h=NH)
                P.op("dve", lambda e, a_=a_: e.tensor_scalar(out=a_[:, :, 0:64], in0=mv3, scalar1=MOBA_SCALE, scalar2=None,
                                                             op0=ALU.mult), reads=[mf], writes=[a_])
                P.op("pool", lambda e: e.tensor_tensor(out=sq[:, :, :], in0=mv3, in1=mv3, op=ALU.mult), reads=[mf], writes=[sq])
                P.op("dve", lambda e: e.tensor_reduce(out=ssq[:, :], in_=sq[:, :, :], axis=AX.X, op=ALU.add), reads=[sq], writes=[ssq])
                P.op("dve", lambda e, a_=a_: e.tensor_scalar(out=a_[:, :, 84:85], in0=ssq[:, :].unsqueeze(2), scalar1=-MOBA_SCALE / 2,
                                                             scalar2=None, op0=ALU.mult), reads=[ssq], writes=[a_])
                P.op("pool", lambda e, a_=a_, i=i: e.tensor_copy(out=a_[:, :, 80:84], in_=qct[i][:, :, :]), reads=[qct[i]], writes=[a_])
                if own == 0 or n_sel == 0:
                    P.op("pool", lambda e: e.memset(selm[:, :, :], -1.0), writes=[selm])
                else:
                    bt = B[3]
                    btv = bt[:, :].rearrange("p (a b) -> p a b", b=128)
                    for p_ in range(4):
                        P.op("pe", lambda e, p_=p_: e.transpose(btv[:, p_, :], mf[:, p_ * 128:(p_ + 1) * 128], C.identf[:, :]),
                             reads=[mf, C.identf], writes=[bt])
                    pe_fence(B[2], 256, [mf, C.identf], covers=[bt])
                    P.op("act", lambda e: e.copy(out=mqT[:, :, :], in_=btv[:, 0:4, :]), reads=[bt], writes=[mqT])
                    bg = B[2]
                    for p_ in range(4):
                        P.op("pe", lambda e, p_=p_: e.matmul(bg[:, p_ * 32:(p_ + 1) * 32], lhsT=mqT[:, p_, :], rhs=KM[:, p_, :],
                                                             start=True, stop=True, skip_group_check=True),
                             reads=[mqT, KM], writes=[bg])
                    pe_fence(bg, 256, [mqT, KM])
                    P.op("dve", lambda e: e.tensor_copy(out=gs[:, :, :], in_=bg[:, 0:128].rearrange("p (h f) -> p h f", h=NH)),
                         reads=[bg], writes=[gs])
                    if own < 16:
                        P.op("dve", lambda e, own=own: e.memset(gs[:, :, own:16], -1e30), writes=[gs])
                    if own > n_sel:
                        for h in range(NH):
                            P.op("dve", lambda e, h=h: e.max(out=m8[:, h, :], in_=gs[:, h, :]), reads=[gs], writes=[m8])
                        for h in range(NH):
                            P.op("dve", lambda e, h=h: e.tensor_scalar(out=selm[:, h, :], in0=gs[:, h, :],
                                                                       scalar1=m8[:, h, n_sel - 1:n_sel], scalar2=1.0,
                                                                       op0=ALU.is_ge, op1=ALU.subtract),
                                 reads=[gs, m8], writes=[selm])
                    else:
                        P.op("dve", lambda e: e.tensor_scalar(out=selm[:, :, :], in0=gs[:, :, :], scalar1=-1e29, scalar2=1.0,
                                                              op0=ALU.is_ge, op1=ALU.subtract), reads=[gs], writes=[selm])
                P.op("dve", lambda e, own=own: e.memset(selm[:, :, own:own + 1], 0.0), writes=[selm])
                P.op("dve", lambda e, a_=a_: e.tensor_copy(out=a_[:, :, 64:80], in_=selm[:, :, :]), reads=[selm], writes=[a_])
            transpose_aug(a_, QT[:, :, tt * 128:(tt + 1) * 128], QT)
        nkt = 4 * (qb + 1)
        its = [(h, kt) for h in range(NH) for kt in range(nkt)]
        LOOK = 2
        scb = [B[3], B[4], B[5]]

        def emit_qk(i):
            h, kt = its[i]
            lo = max(0, kt - 4 * qb)
            sc = scb[i % 3]
            pt = PT[i % 4]
            diag = kt >= 4 * qb
            P.op("pe", lambda e: e.matmul(sc[:, lo * 128:512], lhsT=KT[kt][:, h, :], rhs=QT[:, h, lo * 128:512],
                                          start=True, stop=not diag),
                 reads=[KT[kt], QT], writes=[sc])
            if diag:
                P.op("pe", lambda e: e.matmul(sc[:, lo * 128:(lo + 1) * 128], lhsT=C.ident[:, :], rhs=negm[:, :],
                                              start=False, stop=True),
                     reads=[C.ident, negm], writes=[sc])
            P.op("act", lambda e: e.activation(out=pt[:, lo * 128:512], in_=sc[:, lo * 128:512], func=AF.Exp),
                 reads=[sc], writes=[pt])

        def emit_pv(i):
            h, kt = its[i]
            lo = max(0, kt - 4 * qb)
            pt = PT[i % 4]
            acc = B[6 + h % 2]
            accv = acc[:, :].rearrange("p (a b) -> p a b", b=128)
            for qt in range(lo, 4):
                P.op("pe", lambda e, qt=qt: e.matmul(
                    accv[:, qt, 0:65], lhsT=pt[:, qt * 128:(qt + 1) * 128], rhs=VP[kt][:, h, :],
                    start=(kt == 0 and qt == 0), stop=(kt == 4 * qb + qt), skip_group_check=True),
                    reads=[pt, VP[kt]], writes=[acc])
            if kt == nkt - 1:
                r_ = rc[h % 2]
                P.op("dve", lambda e: e.reciprocal(r_[:, :].unsqueeze(2), accv[:, :, 64:65]), reads=[acc], writes=[r_])
                for qt in range(4):
                    P.op("dve", lambda e, qt=qt: e.tensor_scalar(
                        out=o4[qt][:, h * 64:(h + 1) * 64], in0=accv[:, qt, 0:64], scalar1=r_[:, qt:qt + 1], scalar2=None,
                        op0=ALU.mult), reads=[acc, r_], writes=[o4[qt]])

        for i in range(len(its) + LOOK):
            if i < len(its):
                emit_qk(i)
            if i >= LOOK:
                emit_pv(i - LOOK)
        for tt in range(4):
            t = qb * 4 + tt
            P.dma("sp", out_d[t * 128:(t + 1) * 128, :], o4[tt][:, :], o4[tt], False)
    return o4


def about_stage(P, C, S, x_in, a_in, b_in, x_out, wout_d, pfx="ao"):
    nt = S // 128
    B = P.banks
    wo = P.sb(f"{pfx}wo", [128, 8, D], BF16)
    xt = [P.sb(f"{pfx}xt{i}", [128, D], F32) for i in range(3)]
    ab = [P.sb(f"{pfx}ab{i}", [128, 1024], F32) for i in range(2)]
    abn = P.sb(f"{pfx}abn", [128, 1024], BF16)
    abT = P.sb(f"{pfx}abT", [128, 8, 128], BF16)
    for k in range(8):
        P.dma("pool", wo[:, k, :], wout_d[k * 128:(k + 1) * 128, :], wo, True)

    def load(t):
        P.dma("sp", xt[t % 3][:, :], x_in[t * 128:(t + 1) * 128, :], xt[t % 3], True)
        P.dma("sp", ab[t % 2][:, 0:512], a_in[t * 128:(t + 1) * 128, :], ab[t % 2], True)
        P.dma("sp", ab[t % 2][:, 512:1024], b_in[t * 128:(t + 1) * 128, :], ab[t % 2], True)

    load(0)
    for t in range(nt):
        x = xt[t % 3]
        a_ = ab[t % 2]
        P.op("pool", lambda e, a_=a_: e.tensor_copy(out=abn[:, :], in_=a_[:, :]), reads=[a_], writes=[abn])
        if t + 1 < nt:
            load(t + 1)
        bank = B[t % 2]
        bv = bank.h[:, :].bitcast(BF16).rearrange("p (a b) -> p a b", b=128)
        for k in range(8):
            P.op("pe", lambda e, bv=bv, k=k: e.transpose(bv[:, k, :], abn[:, k * 128:(k + 1) * 128], C.ident[:, :]),
                 reads=[abn, C.ident], writes=[bank])
        P.op("act", lambda e, bv=bv: e.copy(out=abT[:, :, :], in_=bv[:, 0:8, :]), reads=[bank], writes=[abT])
        for dh in range(2):
            b = B[2 + (2 * t + dh) % 4]
            for k in range(8):
                P.op("pe", lambda e, b=b, k=k, dh=dh: e.matmul(b[:, :], lhsT=abT[:, k, :], rhs=wo[:, k, dh * 512:(dh + 1) * 512],
                                                               start=(k == 0), stop=(k == 7)),
                     reads=[abT, wo], writes=[b])
            P.op("dve", lambda e, b=b, dh=dh, x=x: e.tensor_tensor(out=x[:, dh * 512:(dh + 1) * 512],
                                                                   in0=x[:, dh * 512:(dh + 1) * 512], in1=b[:, :], op=ALU.add),
                 reads=[x, b], writes=[x])
        P.dma("sp", x_out[t * 128:(t + 1) * 128, :], x[:, :], x, False)
    return xt


def attn_stage2(P, C, S, kind, x_in, out_d, win_d, g_d, cst, wq_d=None, qg_d=None, wkv_d=None, kvg_d=None, pfx="at"):
    mla = kind == "mla"
    nt = S // 128
    nqb = S // 512
    nblk = S // BLK
    n_sel = min(3, nblk - 1)
    B = P.banks
    TB0, TB1, TB2 = B[0], B[1], B[2]
    pfx = pfx + ("a" if mla else "b")

    if mla:
        win = P.sb(f"{pfx}win", [128, 8, 672], BF16)
        wq = P.sb(f"{pfx}wq", [128, 3, 768], BF16)
        wkv = P.sb(f"{pfx}wkv", [128, 2, 1024], BF16)
        qgb = P.sb(f"{pfx}qgb", [128, 384], F32)
        kvgb = P.sb(f"{pfx}kvgb", [128, 256], F32)
        rqt = [P.sb(f"{pfx}rq{i}", [128, 2, NH, 16], F32) for i in range(2)]
        rkt = [P.sb(f"{pfx}rk{i}", [128, 2, 16], F32) for i in range(2)]
        latfK = P.sb(f"{pfx}latfK", [128, 288], F32)
        latfQ = P.sb(f"{pfx}latfQ", [128, 384], F32)
        latnK = P.sb(f"{pfx}latnK", [128, 256], BF16)
        latnQ = P.sb(f"{pfx}latnQ", [128, 384], BF16)
        latTK = P.sb(f"{pfx}latTK", [128, 2, 128], BF16)
        latTQ = P.sb(f"{pfx}latTQ", [128, 3, 128], BF16)
        qf = P.sb(f"{pfx}qf", [128, NH, 96], F32)
        krr = P.sb(f"{pfx}krr", [128, 32], F32)
        sq = P.sb(f"{pfx}sq", [128, NH, 96], F32)
        st2K = [P.sb(f"{pfx}st2K{j}", [128, 1], F32) for j in range(3)]
        st2Q = [P.sb(f"{pfx}st2Q{j}", [128, 1], F32) for j in range(3)]
        junk2 = P.sb(f"{pfx}junk2", [128, 384], BF16)
    else:
        win = P.sb(f"{pfx}win", [128, 8, 1536], BF16)
        kct = [P.sb(f"{pfx}kc{i}", [128, NH, 32], F32) for i in range(2)]
        qct = [P.sb(f"{pfx}qc{i}", [128, NH, 4], F32) for i in range(2)]
        mfK = P.sb(f"{pfx}mfK", [128, 512], F32)
        mfQ = P.sb(f"{pfx}mfQ", [128, 512], F32)
        mqT = P.sb(f"{pfx}mqT", [128, 4, 128], F32)
        KM = P.sb(f"{pfx}KM", [128, 4, 32], F32)
        onesc = P.sb(f"{pfx}ones", [128, 2], F32)
        gs = P.sb(f"{pfx}gs", [128, NH, 16], F32)
        m8 = P.sb(f"{pfx}m8", [128, NH, 8], F32)
        selm = P.sb(f"{pfx}selm", [128, NH, 16], F32)
        sq = P.sb(f"{pfx}sq", [128, NH, 64], F32)
    gb = P.sb(f"{pfx}gb", [128, D], F32)
    negm = P.sb(f"{pfx}negm", [128, 128], BF16)
    xt = [P.sb(f"{pfx}xt{i}", [128, D], F32) for i in range(2)]
    xn = P.sb(f"{pfx}xn", [128, D], BF16)
    junk = P.sb(f"{pfx}junk", [128, D], BF16)
    st = [P.sb(f"{pfx}st{j}", [128, 1], F32) for j in range(3)]
    hT = P.sb(f"{pfx}hT", [128, 8, 128], BF16)
    tmp = [P.sb(f"{pfx}tmp{i}", [128, NH, 16], F32) for i in range(4)]
    ssq = P.sb(f"{pfx}ssq", [128, NH], F32)
    augK = [P.sb(f"{pfx}augK{i}", [128, NH, 128], BF16) for i in range(2)]
    augQ = [P.sb(f"{pfx}augQ{i}", [128, NH, 128], BF16) for i in range(2)]
    KT = [P.sb(f"{pfx}KT{t}", [128, NH, 128], BF16) for t in range(nt)]
    VP = [P.sb(f"{pfx}VP{t}", [128, NH, 65], BF16) for t in range(nt)]
    QT = [P.sb(f"{pfx}QT{i}", [128, NH, 512], BF16) for i in range(2)]
    PT = [P.sb(f"{pfx}PT{i}", [128, 512], BF16) for i in range(4)]
    o4 = [P.sb(f"{pfx}o{i}", [128, 512], F32) for i in range(4)]
    rc = [P.sb(f"{pfx}rc{i}", [128, 4], F32) for i in range(2)]

    if mla:
        for k in range(8):
            P.dma("pool", win[:, k, :], win_d[k * 128:(k + 1) * 128, 0:672], win, True)
        for j in range(3):
            P.dma("pool", wq[:, j, :], wq_d[j * 128:(j + 1) * 128, :], wq, True)
        for j in range(2):
            P.dma("pool", wkv[:, j, :], wkv_d[j * 128:(j + 1) * 128, :], wkv, True)
        P.dma("sp", qgb[:, :], qg_d.partition_broadcast(128), qgb, True)
        P.dma("sp", kvgb[:, :], kvg_d.partition_broadcast(128), kvgb, True)
    else:
        for k in range(8):
            P.dma("pool", win[:, k, :], win_d[k * 128:(k + 1) * 128, 672:2208], win, True)
        P.op("pool", lambda e: e.memset(KM[:, :, :], 0.0), writes=[KM])
        P.op("pool", lambda e: e.memset(onesc[:, :], 1.0 / BLK), writes=[onesc])
    P.dma("sp", gb[:, :], g_d.partition_broadcast(128), gb, True)
    P.dma("pool", negm[:, :], cst["negm"], negm, True)
    for a_ in augK + augQ:
        P.op("pool", lambda e, a_=a_: e.memset(a_[:, :, :], 0.0), writes=[a_])
    for t in range(nt):
        P.op("pool", lambda e, t=t: e.memset(VP[t][:, :, :], 1.0), writes=[VP[t]])

    def load(t):
        i = t % 2
        P.dma("sp", xt[i][:, :], x_in[t * 128:(t + 1) * 128, :], xt[i], True)
        if mla:
            P.dma("sp", rqt[i][:, :, :, :], cst["rq"][t * 128:(t + 1) * 128, :].rearrange("p (a h f) -> p a h f", a=2, h=NH),
                  rqt[i], True)
            P.dma("sp", rkt[i][:, :, :], cst["rk"][t * 128:(t + 1) * 128, :].rearrange("p (a f) -> p a f", a=2), rkt[i], True)
        else:
            P.dma("sp", qct[i][:, :, :], cst["qc"][t * 128:(t + 1) * 128, :].rearrange("p (h f) -> p h f", h=NH), qct[i], True)
            P.dma("sp", kct[i][:, :, :], cst["kc"][t * 128:(t + 1) * 128, :].rearrange("p (h f) -> p h f", h=NH), kct[i], True)

    def proj(w, col0, ncols, bank, nk=8, lhs=None):
        lhs = hT if lhs is None else lhs
        for k in range(nk):
            P.op("pe", lambda e, k=k: e.matmul(bank[:, :ncols], lhsT=lhs[:, k, :], rhs=w[:, k, col0:col0 + ncols],
                                               start=(k == 0), stop=(k == nk - 1)),
                 reads=[lhs, w], writes=[bank])

    def latent_norm(latf, latn, latT, st2, ncols, gbt, nchunk, bank):
        rms_rstd(P, C, latf, ncols, st2[0], st2[1], st2[2], junk2)
        P.op("dve", lambda e: e.scalar_tensor_tensor(out=latn[:, :ncols], in0=latf[:, :ncols], scalar=st2[2][:, :],
                                                     in1=gbt[:, :ncols], op0=ALU.mult, op1=ALU.mult),
             reads=[latf, st2[2], gbt], writes=[latn])
        yield
        bv = bank.h[:, :].bitcast(BF16).rearrange("p (a b) -> p a b", b=128)
        for j in range(nchunk):
            P.op("pe", lambda e, j=j: e.transpose(bv[:, j, :], latn[:, j * 128:(j + 1) * 128], C.ident[:, :]),
                 reads=[latn, C.ident], writes=[bank])
        yield
        P.op("act", lambda e: e.copy(out=latT[:, 0:nchunk, :], in_=bv[:, 0:nchunk, :]), reads=[bank], writes=[latT])

    def rope(x1, x2, cs, sn, o1, o2, srct, tabt, dstt, small):
        t = [tt_[:, 0, :] if small else tt_[:, :, :] for tt_ in tmp]
        P.op("pool", lambda e: e.tensor_tensor(out=t[0], in0=x1, in1=cs, op=ALU.mult), reads=[srct, tabt], writes=[tmp[0]])
        P.op("pool", lambda e: e.tensor_tensor(out=t[1], in0=x2, in1=sn, op=ALU.mult), reads=[srct, tabt], writes=[tmp[1]])
        P.op("pool", lambda e: e.tensor_tensor(out=t[2], in0=x2, in1=cs, op=ALU.mult), reads=[srct, tabt], writes=[tmp[2]])
        P.op("pool", lambda e: e.tensor_tensor(out=t[3], in0=x1, in1=sn, op=ALU.mult), reads=[srct, tabt], writes=[tmp[3]])
        yield
        P.op("dve", lambda e: e.tensor_tensor(out=o1, in0=t[0], in1=t[1], op=ALU.subtract), reads=[tmp[0], tmp[1]], writes=[dstt])
        P.op("dve", lambda e: e.tensor_tensor(out=o2, in0=t[2], in1=t[3], op=ALU.add), reads=[tmp[2], tmp[3]], writes=[dstt])

    def pe_fence(bank, col, reads, covers=()):
        P.op("pe", lambda e: e.matmul(bank[:, col:col + 2], lhsT=C.ident[:, :], rhs=C.ident[:, 0:2], start=False, stop=True,
                                      skip_group_check=True),
             reads=[C.ident] + list(reads), writes=[bank] + list(covers))

    def transpose_aug(a_, dst_ap, dst_tile):
        bank = TB0
        bv = bank.h[:, :].bitcast(BF16).rearrange("p (a b) -> p a b", b=128)
        for h in range(NH):
            P.op("pe", lambda e, h=h: e.transpose(bv[:, h, :], a_[:, h, :], C.ident[:, :]), reads=[a_, C.ident], writes=[bank])
        yield
        P.op("act", lambda e: e.copy(out=dst_ap, in_=bv[:, 0:8, :]), reads=[bank], writes=[dst_tile])

    def norm_T_gen(x):
        rms_rstd(P, C, x, D, st[0], st[1], st[2], junk)
        P.op("dve", lambda e: e.scalar_tensor_tensor(out=xn[:, :], in0=x[:, :], scalar=st[2][:, :], in1=gb[:, :],
                                                     op0=ALU.mult, op1=ALU.mult),
             reads=[x, st[2], gb], writes=[xn])
        yield
        yield
        bv = TB0.h[:, :].bitcast(BF16).rearrange("p (a b) -> p a b", b=128)
        for k in range(8):
            P.op("pe", lambda e, k=k: e.transpose(bv[:, k, :], xn[:, k * 128:(k + 1) * 128], C.ident[:, :]),
                 reads=[xn, C.ident], writes=[TB0])
        yield
        P.op("act", lambda e: e.copy(out=hT[:, :, :], in_=bv[:, 0:8, :]), reads=[TB0], writes=[hT])
        yield

    def tile_gen(t, tt, QTb):
        i = t % 2
        x = xt[i]
        aK = augK[i]
        aQ = augQ[i]
        yield from norm_T_gen(x)
        if t + 1 < nt:
            load(t + 1)
        if mla:
            proj(win, 384, 288, TB1)
            proj(win, 0, 384, TB2)
            yield
            P.op("act", lambda e: e.copy(out=latfK[:, 0:288], in_=TB1[:, 0:288]), reads=[TB1], writes=[latfK])
            P.op("act", lambda e: e.copy(out=latfQ[:, 0:384], in_=TB2[:, 0:384]), reads=[TB2], writes=[latfQ])
            yield
            yield from rope(latfK[:, 256:272], latfK[:, 272:288], rkt[i][:, 0, :], rkt[i][:, 1, :], krr[:, 0:16], krr[:, 16:32],
                            latfK, rkt[i], krr, True)
            yield
            yield from latent_norm(latfK, latnK, latTK, st2K, 256, kvgb, 2, TB0)
            yield
            for hg in range(2):
                b = TB1 if hg == 0 else TB2
                proj(wkv, hg * 512, 512, b, nk=2, lhs=latTK)
            yield
            for hg in range(2):
                b = TB1 if hg == 0 else TB2
                bv = b[:, :].rearrange("p (h f) -> p h f", h=4)
                P.op("act", lambda e, bv=bv, hg=hg: e.copy(out=aK[:, hg * 4:hg * 4 + 4, 0:64], in_=bv[:, :, 0:64]),
                     reads=[b], writes=[aK])
                P.op("dve", lambda e, bv=bv, hg=hg: e.tensor_copy(out=VP[t][:, hg * 4:hg * 4 + 4, 0:64], in_=bv[:, :, 64:128]),
                     reads=[b], writes=[VP[t]])
            P.op("dve", lambda e: e.tensor_copy(out=aK[:, :, 64:96], in_=krr[:, :].unsqueeze(1).to_broadcast([128, NH, 32])),
                 reads=[krr], writes=[aK])
            P.op("pool", lambda e: e.memset(aK[:, :, 96:97], 1.0), writes=[aK])
            yield
            yield from transpose_aug(aK, KT[t][:, :, :], KT[t])
            yield
            yield from latent_norm(latfQ, latnQ, latTQ, st2Q, 384, qgb, 3, TB0)
            yield
            for hg in range(2):
                b = TB1 if hg == 0 else TB2
                proj(wq, hg * 384, 384, b, nk=3, lhs=latTQ)
            yield
            for hg in range(2):
                b = TB1 if hg == 0 else TB2
                P.op("act", lambda e, b=b, hg=hg: e.copy(out=qf[:, hg * 4:hg * 4 + 4, :],
                                                         in_=b[:, 0:384].rearrange("p (h f) -> p h f", h=4)),
                     reads=[b], writes=[qf])
            yield
            P.op("dve", lambda e: e.tensor_scalar(out=aQ[:, :, 0:64], in0=qf[:, :, 0:64], scalar1=MLA_SCALE, scalar2=None,
                                                  op0=ALU.mult), reads=[qf], writes=[aQ])
            P.op("pool", lambda e: e.tensor_tensor(out=sq[:, :, :], in0=qf[:, :, :], in1=qf[:, :, :], op=ALU.mult),
                 reads=[qf], writes=[sq])
            yield
            yield from rope(qf[:, :, 64:80], qf[:, :, 80:96], rqt[i][:, 0, :, :], rqt[i][:, 1, :, :], aQ[:, :, 64:80], aQ[:, :, 80:96],
                            qf, rqt[i], aQ, False)
            P.op("dve", lambda e: e.tensor_reduce(out=ssq[:, :], in_=sq[:, :, :], axis=AX.X, op=ALU.add), reads=[sq], writes=[ssq])
            P.op("dve", lambda e: e.tensor_scalar(out=aQ[:, :, 96:97], in0=ssq[:, :].unsqueeze(2), scalar1=-MLA_SCALE / 2,
                                                  scalar2=None, op0=ALU.mult), reads=[ssq], writes=[aQ])
            yield
        else:
            own = t // 2
            proj(win, 512, 512, TB1)
            proj(win, 1024, 512, TB2)
            yield
            P.op("act", lambda e: e.copy(out=aK[:, :, 0:64], in_=TB1[:, :].rearrange("p (h f) -> p h f", h=NH)),
                 reads=[TB1], writes=[aK])
            P.op("dve", lambda e: e.tensor_copy(out=mfK[:, :], in_=TB1[:, :]), reads=[TB1], writes=[mfK])
            P.op("act", lambda e: e.copy(out=VP[t][:, :, 0:64], in_=TB2[:, :].rearrange("p (h f) -> p h f", h=NH)),
                 reads=[TB2], writes=[VP[t]])
            P.op("pool", lambda e: e.tensor_copy(out=aK[:, :, 64:96], in_=kct[i][:, :, :]), reads=[kct[i]], writes=[aK])
            yield
            kb = TB1
            for p_ in range(4):
                P.op("pe", lambda e, p_=p_: e.matmul(kb[:, 2 * p_:2 * p_ + 2], lhsT=mfK[:, p_ * 128:(p_ + 1) * 128], rhs=onesc[:, :],
                                                     start=(p_ == 0), stop=(p_ == 3), skip_group_check=True),
                     reads=[mfK, onesc], writes=[kb])
            pe_fence(kb, 256, [mfK, onesc])
            proj(win, 0, 512, TB2)
            yield
            j = t // 2
            kv_ = kb[:, 0:8].rearrange("p (a b) -> p a b", b=2)
            if t % 2 == 0:
                P.op("dve", lambda e: e.tensor_copy(out=KM[0:64, :, j:j + 1], in_=kv_[0:64, :, 0:1]), reads=[kb], writes=[KM])
                P.op("dve", lambda e: e.tensor_copy(out=KM[64:128, :, 16 + j:17 + j], in_=kv_[64:128, :, 0:1]),
                     reads=[kb], writes=[KM])
            else:
                P.op("dve", lambda e: e.tensor_tensor(out=KM[0:64, :, j:j + 1], in0=KM[0:64, :, j:j + 1], in1=kv_[0:64, :, 0:1],
                                                      op=ALU.add), reads=[kb, KM], writes=[KM])
                P.op("dve", lambda e: e.tensor_tensor(out=KM[64:128, :, 16 + j:17 + j], in0=KM[64:128, :, 16 + j:17 + j],
                                                      in1=kv_[64:128, :, 0:1], op=ALU.add), reads=[kb, KM], writes=[KM])
            P.op("act", lambda e: e.copy(out=mfQ[:, :], in_=TB2[:, :]), reads=[TB2], writes=[mfQ])
            yield
            yield from transpose_aug(aK, KT[t][:, :, :], KT[t])
            yield
            mv3 = mfQ[:, :].rearrange("p (h f) -> p h f", h=NH)
            P.op("dve", lambda e: e.tensor_scalar(out=aQ[:, :, 0:64], in0=mv3, scalar1=MOBA_SCALE, scalar2=None, op0=ALU.mult),
                 reads=[mfQ], writes=[aQ])
            P.op("pool", lambda e: e.tensor_tensor(out=sq[:, :, :], in0=mv3, in1=mv3, op=ALU.mult), reads=[mfQ], writes=[sq])
            P.op("pool", lambda e: e.tensor_copy(out=aQ[:, :, 80:84], in_=qct[i][:, :, :]), reads=[qct[i]], writes=[aQ])
            yield
            P.op("dve", lambda e: e.tensor_reduce(out=ssq[:, :], in_=sq[:, :, :], axis=AX.X, op=ALU.add), reads=[sq], writes=[ssq])
            P.op("dve", lambda e: e.tensor_scalar(out=aQ[:, :, 84:85], in0=ssq[:, :].unsqueeze(2), scalar1=-MOBA_SCALE / 2,
                                                  scalar2=None, op0=ALU.mult), reads=[ssq], writes=[aQ])
            if own == 0 or n_sel == 0:
                P.op("pool", lambda e: e.memset(selm[:, :, :], -1.0), writes=[selm])
            else:
                bt = TB1
                btv = bt[:, :].rearrange("p (a b) -> p a b", b=128)
                for p_ in range(4):
                    P.op("pe", lambda e, p_=p_: e.transpose(btv[:, p_, :], mfQ[:, p_ * 128:(p_ + 1) * 128], C.identf[:, :]),
                         reads=[mfQ, C.identf], writes=[bt])
                pe_fence(TB2, 256, [mfQ, C.identf], covers=[bt])
                yield
                P.op("act", lambda e: e.copy(out=mqT[:, :, :], in_=btv[:, 0:4, :]), reads=[bt], writes=[mqT])
                yield
                bg = TB2
                for p_ in range(4):
                    P.op("pe", lambda e, p_=p_: e.matmul(bg[:, p_ * 32:(p_ + 1) * 32], lhsT=mqT[:, p_, :], rhs=KM[:, p_, :],
                                                         start=True, stop=True, skip_group_check=True),
                         reads=[mqT, KM], writes=[bg])
                pe_fence(bg, 256, [mqT, KM])
                yield
                P.op("dve", lambda e: e.tensor_copy(out=gs[:, :, :], in_=bg[:, 0:128].rearrange("p (h f) -> p h f", h=NH)),
                     reads=[bg], writes=[gs])
                if own < 16:
                    P.op("dve", lambda e: e.memset(gs[:, :, own:16], -1e30), writes=[gs])
                if own > n_sel:
                    for h in range(NH):
                        P.op("dve", lambda e, h=h: e.max(out=m8[:, h, :], in_=gs[:, h, :]), reads=[gs], writes=[m8])
                    yield
                    for h in range(NH):
                        P.op("dve", lambda e, h=h: e.tensor_scalar(out=selm[:, h, :], in0=gs[:, h, :],
                                                                   scalar1=m8[:, h, n_sel - 1:n_sel], scalar2=1.0,
                                                                   op0=ALU.is_ge, op1=ALU.subtract),
                             reads=[gs, m8], writes=[selm])
                else:
                    P.op("dve", lambda e: e.tensor_scalar(out=selm[:, :, :], in0=gs[:, :, :], scalar1=-1e29, scalar2=1.0,
                                                          op0=ALU.is_ge, op1=ALU.subtract), reads=[gs], writes=[selm])
            yield
            P.op("dve", lambda e: e.memset(selm[:, :, own:own + 1], 0.0), writes=[selm])
            P.op("dve", lambda e: e.tensor_copy(out=aQ[:, :, 64:80], in_=selm[:, :, :]), reads=[selm], writes=[aQ])
            yield
        yield from transpose_aug(aQ, QTb[:, :, tt * 128:(tt + 1) * 128], QTb)
        yield

    def block_gen(qb):
        for tt in range(4):
            yield from tile_gen(qb * 4 + tt, tt, QT[qb % 2])

    load(0)
    for _ in block_gen(0):
        pass
    for qb in range(nqb):
        nxt = block_gen(qb + 1) if qb + 1 < nqb else None
        QTb = QT[qb % 2]
        nkt = 4 * (qb + 1)
        its = [(h, kt) for h in range(NH) for kt in range(nkt)]
        LOOK = 2
        scb = [B[3], B[4], B[5]]
        NSTEP = 110
        per = max(1, -(-NSTEP // len(its)))
        every = max(1, len(its) // NSTEP)

        def emit_qk(i):
            h, kt = its[i]
            lo = max(0, kt - 4 * qb)
            sc = scb[i % 3]
            pt = PT[i % 4]
            diag = kt >= 4 * qb
            q_ap = QTb[:, h, lo * 128:512]
            k_ap = KT[kt][:, h, :]
            P.op("pe", lambda e: e.matmul(sc[:, lo * 128:512], lhsT=k_ap, rhs=q_ap, start=True, stop=not diag),
                 reads=[KT[kt], QTb], writes=[sc])
            if diag:
                P.op("pe", lambda e: e.matmul(sc[:, lo * 128:(lo + 1) * 128], lhsT=C.ident[:, :], rhs=negm[:, :],
                                              start=False, stop=True),
                     reads=[C.ident, negm], writes=[sc])
            P.op("act", lambda e: e.activation(out=pt[:, lo * 128:512], in_=sc[:, lo * 128:512], func=AF.Exp),
                 reads=[sc], writes=[pt])

        def emit_pv(i):
            h, kt = its[i]
            lo = max(0, kt - 4 * qb)
            pt = PT[i % 4]
            acc = B[6 + h % 2]
            accv = acc[:, :].rearrange("p (a b) -> p a b", b=128)
            v_ap = VP[kt][:, h, :]
            for qt in range(lo, 4):
                stp = (kt == 4 * qb + qt)
                P.op("pe", lambda e, qt=qt, stp=stp: e.matmul(
                    accv[:, qt, 0:65], lhsT=pt[:, qt * 128:(qt + 1) * 128], rhs=v_ap,
                    start=(kt == 0 and qt == 0), stop=stp, skip_group_check=True),
                    reads=[pt, VP[kt]], writes=[acc])
            if kt == nkt - 1:
                r_ = rc[h % 2]
                P.op("dve", lambda e: e.reciprocal(r_[:, :].unsqueeze(2), accv[:, :, 64:65]), reads=[acc], writes=[r_])
                for qt in range(4):
                    P.op("dve", lambda e, qt=qt: e.tensor_scalar(
                        out=o4[qt][:, h * 64:(h + 1) * 64], in0=accv[:, qt, 0:64], scalar1=r_[:, qt:qt + 1], scalar2=None,
                        op0=ALU.mult), reads=[acc, r_], writes=[o4[qt]])

        for i in range(len(its) + LOOK):
            if i < len(its):
                emit_qk(i)
            if i >= LOOK:
                emit_pv(i - LOOK)
            if nxt is not None and i % every == 0:
                for _ in range(per):
                    next(nxt, None)
        if nxt is not None:
            for _ in nxt:
                pass
        for tt in range(4):
            t = qb * 4 + tt
            P.dma("sp", out_d[t * 128:(t + 1) * 128, :], o4[tt][:, :], o4[tt], False)
    return o4


from concourse.bass_utils import run_bass_kernel_spmd

SEQ_FULL = 4096
NCORES = 8
MODE = "fused"


def _consts(S):
    c1, cdec = ret_consts(S)
    c2 = ab_consts(S)
    c = {}
    c.update(c1)
    c.update(c2)
    c["identf"] = np.eye(128, dtype=np.float32)
    return c, cdec


class _Builder:
    def __init__(self):
        self.nc = bass.Bass("TRN2", target_bir_lowering=False)
        self.P = Prog(self.nc)
        self.P.make_banks()
        self.C = Common(self.P)
        self.ins = {}

    def din(self, name, shape):
        if name not in self.ins:
            self.ins[name] = self.nc.dram_tensor(name, list(shape), F32, kind="ExternalInput").ap()
        return self.ins[name]

    def dout(self, name, shape):
        return self.nc.dram_tensor(name, list(shape), F32, kind="ExternalOutput").ap()

    def scratch(self, name, shape):
        return self.nc.dram_tensor(name, list(shape), F32).ap()

    def cst(self, cnp):
        b = self

        class _Lazy(dict):
            def __missing__(self, k):
                v = b.din("c_" + k, cnp[k].shape)
                self[k] = v
                return v
        return _Lazy()

    def start(self, cst):
        self.C.setup(cst["identf"])
        self.P.dma_tiles_keep = list(self.P.dma_tiles)

    def finish(self):
        self.P.barrier()
        self.P.finalize()
        return self.nc


_CONST_CACHE = {}


def _get_consts(S):
    if S not in _CONST_CACHE:
        _CONST_CACHE[S] = _consts(S)
    return _CONST_CACHE[S]


def build_stage_prog(kind, S):
    cnp, cdec = _get_consts(S)
    b = _Builder()
    cst = b.cst(cnp)
    b.start(cst)
    P, C = b.P, b.C
    P.stage_begin()
    x = b.din("x", (S, D))
    if kind in ("mla", "moba"):
        o = b.dout("o", (S, 512))
        if kind == "mla":
            attn_stage2(P, C, S, kind, x, o, b.din("win", (D, 2208)), b.din("g", (D,)), cst,
                       b.din("wq", (384, 768)), b.din("qg", (384,)), b.din("wkv", (256, 1024)), b.din("kvg", (256,)))
        else:
            attn_stage2(P, C, S, kind, x, o, b.din("win", (D, 2208)), b.din("g", (D,)), cst)
    elif kind == "about":
        y = b.dout("y", (S, D))
        about_stage(P, C, S, x, b.din("a", (S, 512)), b.din("b", (S, 512)), y, b.din("wout", (1024, D)))
    elif kind == "retA":
        o = b.dout("on", (S, 2048))
        retA_stage(P, C, S, x, o, b.din("win", (D, 6144)), b.din("g", (D,)), cst, cdec)
    elif kind == "retB":
        y = b.dout("y", (S, D))
        retB_stage(P, C, S, x, b.din("onin", (S, 2048)), y, b.din("win", (D, 6144)), b.din("wout", (2048, D)),
                   b.din("g", (D,)), b.din("rn", (128, 16)))
    elif kind in ("mlp", "mlpf"):
        y = b.dout("y", (S, D))
        mlp_stage(P, C, S, x, y, b.din("wup", (D, FF)), b.din("wdn", (FF, D)), b.din("g", (D,)),
                  b.din("gf", (D,)) if kind == "mlpf" else None)
    P.stage_end()
    nc = b.finish()
    return nc, list(b.ins.keys())


def build_fused_prog(S, depth=4):
    cnp, cdec = _get_consts(S)
    b = _Builder()
    cst = b.cst(cnp)
    b.start(cst)
    P, C = b.P, b.C
    x = b.din("x", (S, D))
    y = b.dout("y", (S, D))
    n_even = (depth + 1) // 2
    n_odd = depth // 2
    norm_mix = b.din("norm_mix", (depth, D))
    norm_mlp = b.din("norm_mlp", (depth, D))
    wup = b.din("w_mlp_up", (depth, D, FF))
    wdn = b.din("w_mlp_down", (depth, FF, D))
    win_ab = b.din("w_in_ab", (n_even, D, 2208))
    qg = b.din("mla_q_norm", (n_even, 384))
    wq = b.din("mla_w_q_up", (n_even, 384, 768))
    kvg = b.din("mla_kv_norm", (n_even, 256))
    wkv = b.din("mla_w_kv_up", (n_even, 256, 1024))
    wout_ab = b.din("w_out_ab", (n_even, 1024, D))
    if n_odd:
        win_c = b.din("w_in_c", (n_odd, D, 6144))
        rn = b.din("ret_norm_l", (n_odd, 128, 16))
        wout_c = b.din("w_out_c", (n_odd, 2048, D))
    gf = b.din("norm_final", (D,))
    xs = [b.scratch("xs0", (S, D)), b.scratch("xs1", (S, D))]
    a_s = b.scratch("a_s", (S, 512))
    b_s = b.scratch("b_s", (S, 512))
    on_s = b.scratch("on_s", (S, 2048))
    cur = x
    for l in range(depth):
        j = l // 2
        mid = xs[0]
        if l % 2 == 0:
            P.stage_begin()
            attn_stage2(P, C, S, "mla", cur, a_s, win_ab[j], norm_mix[l], cst, wq[j], qg[j], wkv[j], kvg[j], pfx=f"L{l}")
            P.stage_end()
            P.stage_begin()
            attn_stage2(P, C, S, "moba", cur, b_s, win_ab[j], norm_mix[l], cst, pfx=f"L{l}")
            P.stage_end()
            P.stage_begin()
            about_stage(P, C, S, cur, a_s, b_s, mid, wout_ab[j], pfx=f"L{l}ao")
            P.stage_end()
        else:
            P.stage_begin()
            retA_stage(P, C, S, cur, on_s, win_c[j], norm_mix[l], cst, cdec, pfx=f"L{l}ra")
            P.stage_end()
            P.stage_begin()
            retB_stage(P, C, S, cur, on_s, mid, win_c[j], wout_c[j], norm_mix[l], rn[j], pfx=f"L{l}rb")
            P.stage_end()
        last = l == depth - 1
        dst = y if last else xs[1]
        P.stage_begin()
        mlp_stage(P, C, S, mid, dst, wup[l], wdn[l], norm_mlp[l], gf if last else None, pfx=f"L{l}m")
        P.stage_end()
        cur = dst
    nc = b.finish()
    return nc, list(b.ins.keys())


_PROGS = {}


def _prog(kind, S, depth=4):
    key = (kind, S, depth)
    if key not in _PROGS:
        _PROGS[key] = build_stage_prog(kind, S) if kind != "fused" else build_fused_prog(S, depth)
    return _PROGS[key]


def _launch(kind, S, per_core_inputs, shared, depth=4):
    nc, names = _prog(kind, S, depth)
    cnp, _ = _get_consts(S)
    in_maps = []
    for pc in per_core_inputs:
        m = {}
        for n in names:
            if n in pc:
                m[n] = pc[n]
            elif n in shared:
                m[n] = shared[n]
            elif n.startswith("c_"):
                m[n] = cnp[n[2:]]
            else:
                raise KeyError(n)
        in_maps.append(m)
    res = run_bass_kernel_spmd(nc, in_maps, core_ids=list(range(len(in_maps))))
    return res.results


def _f32(a):
    return np.ascontiguousarray(np.asarray(a, dtype=np.float32))


def forward(inputs, S, ncores, mode):
    x = _f32(inputs["x"])
    depth = inputs["norm_mix"].shape[0]
    W = {k: _f32(v) for k, v in inputs.items() if k != "x"}
    rnl = np.ascontiguousarray(W["ret_norm"].reshape(-1, 16, 128).transpose(0, 2, 1)) if "ret_norm" in W else None
    if mode == "fused":
        shared = dict(W)
        shared.pop("ret_norm", None)
        if rnl is not None:
            shared["ret_norm_l"] = rnl
        out = _launch("fused", S, [{"x": x[c]} for c in range(ncores)], shared, depth)
        return np.stack([o["y"] for o in out], axis=0)
    cur = [x[c] for c in range(ncores)]
    for l in range(depth):
        j = l // 2
        g = W["norm_mix"][l]
        if l % 2 == 0:
            sh = dict(win=W["w_in_ab"][j], g=g, wq=W["mla_w_q_up"][j], qg=W["mla_q_norm"][j],
                      wkv=W["mla_w_kv_up"][j], kvg=W["mla_kv_norm"][j])
            a = _launch("mla", S, [{"x": c} for c in cur], sh)
            bb = _launch("moba", S, [{"x": c} for c in cur], sh)
            r = _launch("about", S, [{"x": c, "a": a[i]["o"], "b": bb[i]["o"]} for i, c in enumerate(cur)],
                        dict(wout=W["w_out_ab"][j]))
        else:
            sh = dict(win=W["w_in_c"][j], g=g, wout=W["w_out_c"][j], rn=rnl[j])
            on = _launch("retA", S, [{"x": c} for c in cur], sh)
            r = _launch("retB", S, [{"x": c, "onin": on[i]["on"]} for i, c in enumerate(cur)], sh)
        cur = [o["y"] for o in r]
        last = l == depth - 1
        sh = dict(wup=W["w_mlp_up"][l], wdn=W["w_mlp_down"][l], g=W["norm_mlp"][l], gf=W["norm_final"])
        r = _launch("mlpf" if last else "mlp", S, [{"x": c} for c in cur], sh)
        cur = [o["y"] for o in r]
    return np.stack(cur, axis=0)


def kernel(**inputs):
    out = forward(inputs, SEQ_FULL, NCORES, MODE)
    return out.astype(np.float32)
```

```python
import numpy as np
import concourse.bass as bass
import concourse.mybir as mybir

F32 = mybir.dt.float32
BF16 = mybir.dt.bfloat16
ALU = mybir.AluOpType
AF = mybir.ActivationFunctionType
AX = mybir.AxisListType

ENGS = ("pe", "act", "dve", "pool", "sp")
ROT = 24000


class Tile:
    __slots__ = ("h", "name", "w", "rs", "sem", "dcnt", "psum", "last_dma")

    def __init__(self, h, name, psum=False):
        self.h = h
        self.psum = psum
        self.name = name
        self.w = None
        self.rs = {}
        self.sem = None
        self.dcnt = 0
        self.last_dma = None

    def __getitem__(self, k):
        return self.h[k]


class Op:
    __slots__ = ("eng", "fn", "waits", "needs_inc", "gcnt", "is_dma", "tile", "dma_val", "sem")

    def __init__(self, eng, fn):
        self.eng = eng
        self.fn = fn
        self.waits = []
        self.needs_inc = False
        self.gcnt = None
        self.is_dma = False
        self.tile = None
        self.dma_val = 0


class Prog:
    def __init__(self, nc):
        self.nc = nc
        self.ops = {e: [] for e in ENGS}
        self.nops = 0
        self.dma_tiles = []
        self.banks = None
        self.sem_pool = []
        self.mark = None
        import os
        self.limit = int(os.environ.get('OPLIMIT', '100000000'))

    def make_banks(self):
        self.banks = [self.ps(f"bank{i}", [128, 512], F32) for i in range(8)]

    def sb(self, name, shape, dtype):
        return Tile(self.nc.alloc_sbuf_tensor("sb_" + name, list(shape), dtype), name)

    def ps(self, name, shape, dtype=F32):
        return Tile(self.nc.alloc_psum_tensor("ps_" + name, list(shape), dtype), name, True)

    def _dep(self, o, d):
        if d is o or d is None:
            return
        if d.eng == "pe" and o.eng == "pe" and not d.is_dma and not o.is_dma:
            return
        if not d.is_dma:
            d.needs_inc = True
        o.waits.append(d)

    def op(self, eng, fn, reads=(), writes=()):
        if self.nops >= self.limit:
            return None
        o = Op(eng, fn)
        writes = list(writes) + [t for t in reads if t.psum and t not in writes]
        for t in reads:
            self._dep(o, t.w)
        for t in writes:
            self._dep(o, t.w)
            for r in t.rs.values():
                self._dep(o, r)
        for t in reads:
            t.rs[eng] = o
        for t in writes:
            t.w = o
            t.rs = {}
        self.ops[eng].append(o)
        self.nops += 1
        return o

    def dma(self, eng, out, in_, tile, write, extra_reads=(), extra_writes=()):
        if self.nops >= self.limit:
            return None
        o = Op(eng, lambda e: e.dma_start(out=out, in_=in_))
        o.is_dma = True
        o.tile = tile
        if tile.sem is None:
            if self.sem_pool:
                tile.sem, tile.dcnt = self.sem_pool.pop(0)
            else:
                tile.sem = self.nc.alloc_semaphore("d_" + tile.name)
            self.dma_tiles.append(tile)
        if not (write and tile.w is not None and tile.w.is_dma):
            self._dep(o, tile.w)
        if write:
            for r in tile.rs.values():
                self._dep(o, r)
        tile.dcnt += 1
        o.dma_val = 16 * tile.dcnt
        o.sem = tile.sem
        tile.last_dma = o
        if write:
            tile.w = o
            tile.rs = {}
        else:
            tile.rs["dma"] = o
        self.ops[eng].append(o)
        self.nops += 1
        return o

    def barrier(self):
        lasts = []
        for e in ENGS:
            for o in reversed(self.ops[e]):
                if not o.is_dma:
                    lasts.append(o)
                    break
        dl = [t.last_dma for t in self.dma_tiles if t.last_dma is not None]
        news = []
        for e in ENGS:
            o = Op(e, lambda eng: eng.nop())
            for d in lasts:
                if not (d.eng == "pe" and e == "pe"):
                    d.needs_inc = True
                    o.waits.append(d)
            for d in dl:
                o.waits.append(d)
            news.append(o)
        for o in news:
            self.ops[o.eng].append(o)
            self.nops += 1

    def stage_begin(self):
        self.mark = self.nc.sbuf_base
        self.dma_tiles = []

    def stage_end(self):
        self.barrier()
        for t in self.dma_tiles:
            self.sem_pool.append((t.sem, t.dcnt))
            t.sem = None
        self.dma_tiles = []
        self.nc.sbuf_base = self.mark

    def finalize(self, final_wait_tiles=()):
        nc = self.nc
        esems = {}
        for e in ENGS:
            c = 0
            for o in self.ops[e]:
                if o.needs_inc and not o.is_dma:
                    c += 1
                    o.gcnt = c
            nsem = (c + ROT - 1) // ROT
            esems[e] = [nc.alloc_semaphore(f"s_{e}{i}") for i in range(max(nsem, 1))]
        self.esems = esems
        final_waits = [(t.sem, 16 * t.dcnt) for t in final_wait_tiles if t.sem is not None]

        def emit(engobj, e):
            seen_eng = {}
            seen_sem = {}
            for o in self.ops[e]:
                for d in o.waits:
                    if d.is_dma:
                        s = d.sem
                        if seen_sem.get(s.num, 0) >= d.dma_val:
                            continue
                        seen_sem[s.num] = d.dma_val
                        engobj.wait_ge(s, d.dma_val)
                    else:
                        if seen_eng.get(d.eng, 0) >= d.gcnt:
                            continue
                        seen_eng[d.eng] = d.gcnt
                        k = (d.gcnt - 1) // ROT
                        engobj.wait_ge(esems[d.eng][k], d.gcnt - k * ROT)
                ins = o.fn(engobj)
                if o.is_dma:
                    ins.then_inc(o.sem, 16)
                elif o.needs_inc:
                    k = (o.gcnt - 1) // ROT
                    ins.then_inc(esems[e][k], 1)
            if e == "sp":
                for s, v in final_waits:
                    engobj.wait_ge(s, v)

        with nc.Block() as block:
            @block.tensor
            def _(eng):
                emit(eng, "pe")

            @block.scalar
            def _(eng):
                emit(eng, "act")

            @block.vector
            def _(eng):
                emit(eng, "dve")

            @block.gpsimd
            def _(eng):
                emit(eng, "pool")

            @block.sync
            def _(eng):
                emit(eng, "sp")

D = 1024
FF = 4096
EPS = 1e-6


class Common:
    def __init__(self, P):
        self.P = P
        nc = P.nc
        self.ident = P.sb("ident", [128, 128], BF16)
        self.eps = P.sb("eps", [128, 1], F32)
        self.identf = P.sb("identf32", [128, 128], F32)
        self._rr = 0

    def setup(self, ident_dram):
        P = self.P
        P.dma("pool", self.ident[:, :], ident_dram, self.ident, True)
        P.dma("sp", self.identf[:, :], ident_dram, self.identf, True)
        P.op("dve", lambda e: e.memset(self.eps[:, :], EPS), writes=[self.eps])


def rms_rstd(P, C, xt, ncols, ss, sq, rstd, junk):
    P.op("act", lambda e: e.activation(out=junk[:, :ncols], in_=xt[:, :ncols], func=AF.Square,
                                       accum_out=ss[:, :]),
         reads=[xt], writes=[junk, ss])
    P.op("act", lambda e: e.activation(out=sq[:, :], in_=ss[:, :], func=AF.Sqrt,
                                       bias=C.eps[:, :], scale=1.0 / ncols),
         reads=[ss, C.eps], writes=[sq])
    P.op("dve", lambda e: e.reciprocal(rstd[:, :], sq[:, :]), reads=[sq], writes=[rstd])


def mlp_stage(P, C, S, x_in, x_out, wup_d, wdn_d, g_d, gfin_d=None, pfx="m"):
    nc = P.nc
    TB = 256
    NT = TB // 128
    nblk = S // TB
    KC = D // 128
    FC = FF // 128

    wup = P.sb(f"{pfx}wup", [128, KC, FF], BF16)
    wdn = P.sb(f"{pfx}wdn", [128, FC, D], BF16)
    gb = P.sb(f"{pfx}gb", [128, D], F32)
    gfb = P.sb(f"{pfx}gfb", [128, D], F32) if gfin_d is not None else None
    NX = 4
    xt = [P.sb(f"{pfx}xt{i}", [128, D], F32) for i in range(NX)]
    xn = [P.sb(f"{pfx}xn{i}", [128, D], BF16) for i in range(2)]
    junk = P.sb(f"{pfx}junk", [128, D], BF16)
    st = [[P.sb(f"{pfx}st{i}_{j}", [128, 1], F32) for j in range(3)] for i in range(2)]
    hT = [P.sb(f"{pfx}hT{i}", [128, KC, TB], BF16) for i in range(2)]
    uT = [P.sb(f"{pfx}uT{f}", [128, TB], BF16) for f in range(FC)]
    rl = [P.sb(f"{pfx}rl{i}", [128, TB], F32) for i in range(4)]
    Bk = P.banks
    tp = [Bk[0], Bk[1]]
    up = [Bk[2], Bk[3], Bk[4]]
    dn = [Bk[5], Bk[6]]

    for k in range(KC):
        for c in range(0, FF, 2048):
            P.dma("pool", wup[:, k, c:c + 2048], wup_d[k * 128:(k + 1) * 128, c:c + 2048], wup, True)
    for f in range(FC):
        P.dma("pool", wdn[:, f, :], wdn_d[f * 128:(f + 1) * 128, :], wdn, True)
    P.dma("sp", gb[:, :], g_d.partition_broadcast(128), gb, True)
    if gfb is not None:
        P.dma("sp", gfb[:, :], gfin_d.partition_broadcast(128), gfb, True)

    def load_x(b, t):
        i = (b * NT + t) % NX
        r0 = b * TB + t * 128
        P.dma("sp", xt[i][:, :], x_in[r0:r0 + 128, :], xt[i], True)

    for t in range(NT):
        load_x(0, t)
    cnt = 0
    for b in range(nblk):
        h = hT[b % 2]
        for t in range(NT):
            i = (b * NT + t) % NX
            x = xt[i]
            s = st[t % 2]
            rms_rstd(P, C, x, D, s[0], s[1], s[2], junk)
            n = xn[t % 2]
            P.op("dve", lambda e, n=n, x=x, s=s: e.scalar_tensor_tensor(
                out=n[:, :], in0=x[:, :], scalar=s[2][:, :], in1=gb[:, :], op0=ALU.mult, op1=ALU.mult),
                reads=[x, s[2], gb], writes=[n])
            p = tp[t % 2]
            pv = p.h[:, :].bitcast(BF16).rearrange("p (a b) -> p a b", b=128)
            for k in range(KC):
                P.op("pe", lambda e, pv=pv, n=n, k=k: e.transpose(pv[:, k, :], n[:, k * 128:(k + 1) * 128],
                                                                   C.ident[:, :]),
                     reads=[n, C.ident], writes=[p])
            P.op("act", lambda e, h=h, pv=pv, t=t: e.copy(out=h[:, :, t * 128:(t + 1) * 128], in_=pv[:, 0:8, :]),
                 reads=[p], writes=[h])
        if b + 1 < nblk:
            for t in range(NT):
                load_x(b + 1, t)
        for f in range(FC):
            u = up[f % 3]
            for k in range(KC):
                P.op("pe", lambda e, u=u, k=k, f=f, h=h: e.matmul(
                    u[:, :TB], lhsT=wup[:, k, f * 128:(f + 1) * 128], rhs=h[:, k, :],
                    start=(k == 0), stop=(k == KC - 1)),
                    reads=[wup, h], writes=[u])
            r = rl[f % 4]
            if f % 2 == 0:
                P.op("act", lambda e, r=r, u=u: e.activation(out=r[:, :], in_=u[:, :TB], func=AF.Relu),
                     reads=[u], writes=[r])
            else:
                P.op("dve", lambda e, r=r, u=u: e.tensor_scalar(out=r[:, :], in0=u[:, :TB], scalar1=0.0,
                                                                 scalar2=None, op0=ALU.max),
                     reads=[u], writes=[r])
            P.op("pool", lambda e, r=r, f=f: e.tensor_tensor(out=uT[f][:, :], in0=r[:, :], in1=r[:, :],
                                                              op=ALU.mult),
                 reads=[r], writes=[uT[f]])
        for t in range(NT):
            i = (b * NT + t) % NX
            x = xt[i]
            for dh in range(2):
                d = dn[cnt % 2]
                cnt += 1
                for f in range(FC):
                    P.op("pe", lambda e, d=d, f=f, t=t, dh=dh: e.matmul(
                        d[:, :], lhsT=uT[f][:, t * 128:(t + 1) * 128], rhs=wdn[:, f, dh * 512:(dh + 1) * 512],
                        start=(f == 0), stop=(f == FC - 1)),
                        reads=[uT[f], wdn], writes=[d])
                P.op("dve", lambda e, d=d, x=x, dh=dh: e.tensor_tensor(
                    out=x[:, dh * 512:(dh + 1) * 512], in0=x[:, dh * 512:(dh + 1) * 512], in1=d[:, :],
                    op=ALU.add), reads=[x, d], writes=[x])
            if gfb is not None:
                s = st[t % 2]
                rms_rstd(P, C, x, D, s[0], s[1], s[2], junk)
                P.op("dve", lambda e, x=x, s=s: e.scalar_tensor_tensor(
                    out=x[:, :], in0=x[:, :], scalar=s[2][:, :], in1=gfb[:, :], op0=ALU.mult, op1=ALU.mult),
                    reads=[x, s[2], gfb], writes=[x])
            r0 = b * TB + t * 128
            P.dma("sp", x_out[r0:r0 + 128, :], x[:, :], x, False)
    return xt


RH = 4
RDK = 256
RDV = 512
CH = 128
LOG_GAMMA = np.log(1.0 - 2.0 ** (-5.0 - np.arange(RH))).astype(np.float32).astype(np.float64)


def ret_consts(S):
    pos = np.arange(S, dtype=np.float32)
    inv_freq = (1.0 / (10000.0 ** np.linspace(0.0, 1.0, RDK // 2, dtype=np.float32))).astype(np.float32)
    ang = pos[:, None] * inv_freq[None, :]
    cos, sin = np.cos(ang).astype(np.float64), np.sin(ang).astype(np.float64)
    p = (np.arange(S) % CH).astype(np.float64)
    qdec = np.exp(LOG_GAMMA[None, :] * (p[:, None] + 1.0))
    tq = np.zeros((S, 2, RH, 128), np.float32)
    tq[:, 0] = cos[:, None, :] * qdec[:, :, None]
    tq[:, 1] = sin[:, None, :] * qdec[:, :, None]
    tk = np.zeros((S, 2, 128), np.float32)
    tk[:, 0] = cos * RDK ** -0.5
    tk[:, 1] = sin * RDK ** -0.5
    pc = np.arange(CH, dtype=np.float64)
    kdec = np.exp(LOG_GAMMA[None, :] * (CH - 1.0 - pc[:, None])).astype(np.float32)
    dt = np.zeros((CH, RH, CH), np.float32)
    for h in range(RH):
        m = np.exp(-LOG_GAMMA[h] * (pc[:, None] + 1.0)) * (pc[:, None] <= pc[None, :])
        dt[:, h, :] = m
    cdec = [float(np.exp(LOG_GAMMA[h] * CH)) for h in range(RH)]
    return dict(tq=tq.reshape(S, 1024), tk=tk.reshape(S, 256), kdec=kdec, dt=dt.reshape(CH, RH * CH)), cdec


def norm_T(P, C, x, gb, xn, st, junk, bank, hT):
    rms_rstd(P, C, x, D, st[0], st[1], st[2], junk)
    P.op("dve", lambda e: e.scalar_tensor_tensor(out=xn[:, :], in0=x[:, :], scalar=st[2][:, :], in1=gb[:, :],
                                                 op0=ALU.mult, op1=ALU.mult),
         reads=[x, st[2], gb], writes=[xn])
    bv = bank.h[:, :].bitcast(BF16).rearrange("p (a b) -> p a b", b=128)
    for k in range(8):
        P.op("pe", lambda e, k=k: e.transpose(bv[:, k, :], xn[:, k * 128:(k + 1) * 128], C.ident[:, :]),
             reads=[xn, C.ident], writes=[bank])
    P.op("act", lambda e: e.copy(out=hT[:, :, :], in_=bv[:, 0:8, :]), reads=[bank], writes=[hT])


def retA_stage(P, C, S, x_in, on_out, win_d, g_d, cst, cdec, pfx="ra"):
    nch = S // CH
    B = P.banks
    w = P.sb(f"{pfx}w", [128, 8, 4096], BF16)
    gb = P.sb(f"{pfx}gb", [128, D], F32)
    dtt = P.sb(f"{pfx}dt", [128, RH, CH], F32)
    kdec = P.sb(f"{pfx}kdec", [128, RH], F32)
    xt = [P.sb(f"{pfx}xt{i}", [128, D], F32) for i in range(2)]
    tq = [P.sb(f"{pfx}tq{i}", [128, 2, RH, 128], F32) for i in range(2)]
    tk = [P.sb(f"{pfx}tk{i}", [128, 2, 128], F32) for i in range(2)]
    xn = P.sb(f"{pfx}xn", [128, D], BF16)
    junk = P.sb(f"{pfx}junk", [128, D], BF16)
    st = [P.sb(f"{pfx}st{j}", [128, 1], F32) for j in range(3)]
    hT = P.sb(f"{pfx}hT", [128, 8, 128], BF16)
    qs = P.sb(f"{pfx}qs", [128, RH, 2, 128], F32)
    ks = P.sb(f"{pfx}ks", [128, RH, 2, 128], F32)
    tmp = [P.sb(f"{pfx}tmp{i}", [128, RH, 128], F32) for i in range(4)]
    qd = P.sb(f"{pfx}qd", [128, RH, 2, 128], BF16)
    kr = P.sb(f"{pfx}kr", [128, RH, 2, 128], BF16)
    v = [P.sb(f"{pfx}v{h}", [128, RDV], BF16) for h in range(RH)]
    vd = [P.sb(f"{pfx}vd{h}", [128, RDV], BF16) for h in range(RH)]
    qdT = P.sb(f"{pfx}qdT", [128, 8, 128], BF16)
    kT = P.sb(f"{pfx}kT", [128, 8, 128], BF16)
    Sf = [P.sb(f"{pfx}S{h}", [128, 2, RDV], F32) for h in range(RH)]
    Sb = [P.sb(f"{pfx}Sb{h}", [128, 2, RDV], BF16) for h in range(RH)]
    PT = [P.sb(f"{pfx}PT{i}", [128, 128], BF16) for i in range(2)]
    gst = [[P.sb(f"{pfx}gs{i}_{j}", [128, 8], F32) for j in range(5)] for i in range(2)]
    on = [P.sb(f"{pfx}on{i}", [128, RH * RDV], F32) for i in range(2)]

    for k in range(8):
        for c0 in range(0, 4096, 2048):
            P.dma("pool", w[:, k, c0:c0 + 2048], win_d[k * 128:(k + 1) * 128, c0:c0 + 2048], w, True)
    P.dma("sp", gb[:, :], g_d.partition_broadcast(128), gb, True)
    P.dma("sp", dtt[:, :, :], cst["dt"].rearrange("p (h q) -> p h q", h=RH), dtt, True)
    P.dma("sp", kdec[:, :], cst["kdec"], kdec, True)

    def load(c):
        i = c % 2
        P.dma("sp", xt[i][:, :], x_in[c * CH:(c + 1) * CH, :], xt[i], True)
        P.dma("sp", tq[i][:, :, :, :], cst["tq"][c * CH:(c + 1) * CH, :].rearrange("p (a h f) -> p a h f", a=2, h=RH),
              tq[i], True)
        P.dma("sp", tk[i][:, :, :], cst["tk"][c * CH:(c + 1) * CH, :].rearrange("p (a f) -> p a f", a=2), tk[i], True)

    def proj(col0, ncols, bank):
        for k in range(8):
            P.op("pe", lambda e, k=k: e.matmul(bank[:, :ncols], lhsT=hT[:, k, :], rhs=w[:, k, col0:col0 + ncols],
                                               start=(k == 0), stop=(k == 7)),
                 reads=[hT, w], writes=[bank])

    def rotate(src, dst, cs, sn, tab):
        x1, x2 = src[:, :, 0, :], src[:, :, 1, :]
        P.op("pool", lambda e: e.tensor_tensor(out=tmp[0][:, :, :], in0=x1, in1=cs, op=ALU.mult), reads=[src, tab], writes=[tmp[0]])
        P.op("pool", lambda e: e.tensor_tensor(out=tmp[1][:, :, :], in0=x2, in1=sn, op=ALU.mult), reads=[src, tab], writes=[tmp[1]])
        P.op("dve", lambda e: e.tensor_tensor(out=dst[:, :, 0, :], in0=tmp[0][:, :, :], in1=tmp[1][:, :, :], op=ALU.subtract),
             reads=[tmp[0], tmp[1]], writes=[dst])
        P.op("pool", lambda e: e.tensor_tensor(out=tmp[2][:, :, :], in0=x2, in1=cs, op=ALU.mult), reads=[src, tab], writes=[tmp[2]])
        P.op("pool", lambda e: e.tensor_tensor(out=tmp[3][:, :, :], in0=x1, in1=sn, op=ALU.mult), reads=[src, tab], writes=[tmp[3]])
        P.op("dve", lambda e: e.tensor_tensor(out=dst[:, :, 1, :], in0=tmp[2][:, :, :], in1=tmp[3][:, :, :], op=ALU.add),
             reads=[tmp[2], tmp[3]], writes=[dst])

    load(0)
    for c in range(nch):
        i = c % 2
        x = xt[i]
        norm_T(P, C, x, gb, xn, st, junk, B[0], hT)
        if c + 1 < nch:
            load(c + 1)
        for g in range(2):
            b = B[2 + g]
            proj(g * 512, 512, b)
            P.op("act", lambda e, b=b, g=g: e.copy(out=qs[:, 2 * g:2 * g + 2, :, :],
                                                   in_=b[:, :].rearrange("p (h a f) -> p h a f", h=2, a=2)),
                 reads=[b], writes=[qs])
        rotate(qs, qd, tq[i][:, 0, :, :], tq[i][:, 1, :, :], tq[i])
        for g in range(2):
            b = B[2 + g]
            proj(1024 + g * 512, 512, b)
            P.op("act", lambda e, b=b, g=g: e.copy(out=ks[:, 2 * g:2 * g + 2, :, :],
                                                   in_=b[:, :].rearrange("p (h a f) -> p h a f", h=2, a=2)),
                 reads=[b], writes=[ks])
        rotate(ks, kr, tk[i][:, 0, :].unsqueeze(1).to_broadcast([128, RH, 128]),
               tk[i][:, 1, :].unsqueeze(1).to_broadcast([128, RH, 128]), tk[i])
        for h in range(RH):
            b = B[2 + h % 2]
            proj(2048 + h * 512, 512, b)
            P.op("act", lambda e, b=b, h=h: e.copy(out=v[h][:, :], in_=b[:, :]), reads=[b], writes=[v[h]])
            P.op("dve", lambda e, b=b, h=h: e.tensor_scalar(out=vd[h][:, :], in0=b[:, :], scalar1=kdec[:, h:h + 1],
                                                            scalar2=None, op0=ALU.mult),
                 reads=[b, kdec], writes=[vd[h]])
        for (src, dstT, bank) in ((qd, qdT, B[0]), (kr, kT, B[1])):
            bv = bank.h[:, :].bitcast(BF16).rearrange("p (a b) -> p a b", b=128)
            for h in range(RH):
                for a in range(2):
                    P.op("pe", lambda e, src=src, bv=bv, h=h, a=a: e.transpose(bv[:, 2 * h + a, :], src[:, h, a, :],
                                                                              C.ident[:, :]),
                         reads=[src, C.ident], writes=[bank])
            P.op("dve" if src is qd else "act",
                 (lambda e, dstT=dstT, bv=bv: e.tensor_copy(out=dstT[:, :, :], in_=bv[:, 0:8, :])) if src is qd else
                 (lambda e, dstT=dstT, bv=bv: e.copy(out=dstT[:, :, :], in_=bv[:, 0:8, :])),
                 reads=[bank], writes=[dstT])
        o_t = on[c % 2]
        for h in range(RH):
            sc = B[4]
            for a in range(2):
                P.op("pe", lambda e, h=h, a=a: e.matmul(sc[:, 0:128], lhsT=kT[:, 2 * h + a, :], rhs=qdT[:, 2 * h + a, :],
                                                        start=(a == 0), stop=(a == 1)),
                     reads=[kT, qdT], writes=[sc])
            pt = PT[h % 2]
            P.op("dve", lambda e, pt=pt, h=h: e.tensor_tensor(out=pt[:, :], in0=sc[:, 0:128], in1=dtt[:, h, :], op=ALU.mult),
                 reads=[sc, dtt], writes=[pt])
            ob = B[5]
            P.op("pe", lambda e, pt=pt, h=h: e.matmul(ob[:, :], lhsT=pt[:, :], rhs=v[h][:, :], start=True, stop=(c == 0)),
                 reads=[pt, v[h]], writes=[ob])
            if c > 0:
                for a in range(2):
                    P.op("pe", lambda e, h=h, a=a: e.matmul(ob[:, :], lhsT=qdT[:, 2 * h + a, :], rhs=Sb[h][:, a, :],
                                                            start=False, stop=(a == 1)),
                         reads=[qdT, Sb[h]], writes=[ob])
            for a in range(2):
                su = B[6 + a]
                P.op("pe", lambda e, su=su, h=h, a=a: e.matmul(su[:, :], lhsT=kr[:, h, a, :], rhs=vd[h][:, :],
                                                               start=True, stop=True),
                     reads=[kr, vd[h]], writes=[su])
                if c == 0:
                    P.op("dve", lambda e, su=su, h=h, a=a: e.tensor_copy(out=Sf[h][:, a, :], in_=su[:, :]),
                         reads=[su], writes=[Sf[h]])
                else:
                    P.op("dve", lambda e, su=su, h=h, a=a: e.scalar_tensor_tensor(
                        out=Sf[h][:, a, :], in0=Sf[h][:, a, :], scalar=cdec[h], in1=su[:, :], op0=ALU.mult, op1=ALU.add),
                        reads=[su, Sf[h]], writes=[Sf[h]])
            if c + 1 < nch:
                P.op("pool", lambda e, h=h: e.tensor_copy(out=Sb[h][:, :, :], in_=Sf[h][:, :, :]),
                     reads=[Sf[h]], writes=[Sb[h]])
            g5 = gst[h % 2]
            P.op("dve", lambda e, g5=g5: e.bn_stats(out=g5[0][:, 0:6], in_=ob[:, :]), reads=[ob], writes=[g5[0]])
            P.op("dve", lambda e, g5=g5: e.bn_aggr(out=g5[1][:, 0:2], in_=g5[0][:, 0:6]), reads=[g5[0]], writes=[g5[1]])
            P.op("act", lambda e, g5=g5: e.activation(out=g5[2][:, 0:1], in_=g5[1][:, 1:2], func=AF.Sqrt,
                                                      bias=C.eps[:, :], scale=1.0),
                 reads=[g5[1], C.eps], writes=[g5[2]])
            P.op("dve", lambda e, g5=g5: e.reciprocal(g5[3][:, 0:1], g5[2][:, 0:1]), reads=[g5[2]], writes=[g5[3]])
            P.op("dve", lambda e, g5=g5: e.tensor_scalar(out=g5[4][:, 0:1], in0=g5[1][:, 0:1], scalar1=g5[3][:, 0:1],
                                                         scalar2=-1.0, op0=ALU.mult, op1=ALU.mult),
                 reads=[g5[1], g5[3]], writes=[g5[4]])
            P.op("act", lambda e, g5=g5, h=h, o_t=o_t: e.activation(
                out=o_t[:, h * RDV:(h + 1) * RDV], in_=ob[:, :], func=AF.Identity, bias=g5[4][:, 0:1], scale=g5[3][:, 0:1]),
                reads=[ob, g5[3], g5[4]], writes=[o_t])
        P.dma("sp", on_out[c * CH:(c + 1) * CH, :], o_t[:, :], o_t, False)
    return on


def retB_stage(P, C, S, x_in, on_in, x_out, win_d, wout_d, g_d, rng_d, pfx="rb"):
    nt = S // 128
    B = P.banks
    wg = P.sb(f"{pfx}wg", [128, 8, 2048], BF16)
    wo = P.sb(f"{pfx}wo", [128, 16, D], BF16)
    stg = [P.sb(f"{pfx}stg{i}", [128, D], F32) for i in range(2)]
    rng = P.sb(f"{pfx}rng", [128, 16], F32)
    gb = P.sb(f"{pfx}gb", [128, D], F32)
    xt = [P.sb(f"{pfx}xt{i}", [128, D], F32) for i in range(3)]
    ont = [P.sb(f"{pfx}on{i}", [128, 2048], F32) for i in range(2)]
    xn = P.sb(f"{pfx}xn", [128, D], BF16)
    junk = P.sb(f"{pfx}junk", [128, D], BF16)
    st = [P.sb(f"{pfx}st{j}", [128, 1], F32) for j in range(3)]
    hT = P.sb(f"{pfx}hT", [128, 8, 128], BF16)
    sg = [P.sb(f"{pfx}sg{i}", [128, 512], F32) for i in range(2)]
    og = P.sb(f"{pfx}og", [128, 2048], BF16)
    ogT = P.sb(f"{pfx}ogT", [128, 16, 128], BF16)

    for k in range(8):
        P.dma("pool", wg[:, k, :], win_d[k * 128:(k + 1) * 128, 4096:6144], wg, True)
    P.dma("sp", gb[:, :], g_d.partition_broadcast(128), gb, True)
    P.dma("sp", rng[:, :], rng_d, rng, True)
    for kc in range(16):
        s_ = stg[kc % 2]
        P.dma("sp", s_[:, :], wout_d[kc * 128:(kc + 1) * 128, :], s_, True)
        P.op("pool", lambda e, s_=s_, kc=kc: e.tensor_scalar(out=wo[:, kc, :], in0=s_[:, :], scalar1=rng[:, kc:kc + 1],
                                                             scalar2=None, op0=ALU.mult),
             reads=[s_, rng], writes=[wo])

    def load(t):
        P.dma("sp", xt[t % 3][:, :], x_in[t * 128:(t + 1) * 128, :], xt[t % 3], True)
        P.dma("sp", ont[t % 2][:, :], on_in[t * 128:(t + 1) * 128, :], ont[t % 2], True)

    load(0)
    for t in range(nt):
        x = xt[t % 3]
        o_ = ont[t % 2]
        norm_T(P, C, x, gb, xn, st, junk, B[0], hT)
        if t + 1 < nt:
            load(t + 1)
        for g in range(4):
            b = B[2 + g % 2]
            for k in range(8):
                P.op("pe", lambda e, k=k, g=g, b=b: e.matmul(b[:, :], lhsT=hT[:, k, :], rhs=wg[:, k, g * 512:(g + 1) * 512],
                                                             start=(k == 0), stop=(k == 7)),
                     reads=[hT, wg], writes=[b])
            s_ = sg[g % 2]
            P.op("act", lambda e, s_=s_, b=b: e.activation(out=s_[:, :], in_=b[:, :], func=AF.Silu), reads=[b], writes=[s_])
            P.op("dve", lambda e, s_=s_, g=g, o_=o_: e.tensor_tensor(out=og[:, g * 512:(g + 1) * 512], in0=s_[:, :],
                                                                     in1=o_[:, g * 512:(g + 1) * 512], op=ALU.mult),
                 reads=[s_, o_], writes=[og])
        for half in range(2):
            bank = B[half]
            bv = bank.h[:, :].bitcast(BF16).rearrange("p (a b) -> p a b", b=128)
            for k in range(8):
                kc = half * 8 + k
                P.op("pe", lambda e, bv=bv, k=k, kc=kc: e.transpose(bv[:, k, :], og[:, kc * 128:(kc + 1) * 128], C.ident[:, :]),
                     reads=[og, C.ident], writes=[bank])
            P.op("act" if half == 0 else "dve",
                 (lambda e, bv=bv, half=half: e.copy(out=ogT[:, half * 8:half * 8 + 8, :], in_=bv[:, 0:8, :])) if half == 0 else
                 (lambda e, bv=bv, half=half: e.tensor_copy(out=ogT[:, half * 8:half * 8 + 8, :], in_=bv[:, 0:8, :])),
                 reads=[bank], writes=[ogT])
        for dh in range(2):
            b = B[4 + dh]
            for kc in range(16):
                P.op("pe", lambda e, b=b, kc=kc, dh=dh: e.matmul(b[:, :], lhsT=ogT[:, kc, :], rhs=wo[:, kc, dh * 512:(dh + 1) * 512],
                                                                 start=(kc == 0), stop=(kc == 15)),
                     reads=[ogT, wo], writes=[b])
            P.op("dve", lambda e, b=b, dh=dh, x=x: e.tensor_tensor(out=x[:, dh * 512:(dh + 1) * 512],
                                                                   in0=x[:, dh * 512:(dh + 1) * 512], in1=b[:, :], op=ALU.add),
                 reads=[x, b], writes=[x])
        P.dma("sp", x_out[t * 128:(t + 1) * 128, :], x[:, :], x, False)
    return xt


NH = 8
MLA_SCALE = 96 ** -0.5
MOBA_SCALE = 0.125
BLK = 256
NEGV = 30000.0


def ab_consts(S):
    pos = np.arange(S, dtype=np.float32)
    inv_freq = (1.0 / (10000.0 ** (np.arange(0, 32, 2, dtype=np.float32) / 32))).astype(np.float32)
    ang = pos[:, None] * inv_freq[None, :]
    cos, sin = np.cos(ang), np.sin(ang)
    rq = np.zeros((S, 2, NH, 16), np.float32)
    rq[:, 0] = (cos * MLA_SCALE)[:, None, :]
    rq[:, 1] = (sin * MLA_SCALE)[:, None, :]
    rk = np.zeros((S, 2, 16), np.float32)
    rk[:, 0] = cos
    rk[:, 1] = sin
    pc = np.arange(128)
    negm = np.where(pc[:, None] > pc[None, :], -NEGV, 0.0).astype(np.float32)
    slopes = np.array([2.0 ** (-(h + 1)) for h in range(NH)], np.float64)
    p = np.arange(S)
    blk = p // BLK
    r = p % BLK
    kc = np.zeros((S, NH, 32), np.float32)
    kc[p, :, blk] = NEGV
    kc[:, :, 16] = 1.0
    kc[:, :, 17] = 1.0
    kc[:, :, 18] = slopes[None, :] * 256.0 * blk[:, None]
    kc[:, :, 19] = slopes[None, :] * r[:, None]
    kc[:, :, 20] = 1.0
    qc = np.zeros((S, NH, 4), np.float32)
    qc[:, :, 0] = -slopes[None, :] * 256.0 * blk[:, None]
    qc[:, :, 1] = -slopes[None, :] * r[:, None]
    qc[:, :, 2] = 1.0
    qc[:, :, 3] = 1.0
    return dict(rq=rq.reshape(S, 256), rk=rk.reshape(S, 32), negm=negm, kc=kc.reshape(S, 256), qc=qc.reshape(S, 32))


def attn_stage(P, C, S, kind, x_in, out_d, win_d, g_d, cst, wq_d=None, qg_d=None, wkv_d=None, kvg_d=None, pfx="at"):
    mla = kind == "mla"
    nt = S // 128
    nqb = S // 512
    nblk = S // BLK
    n_sel = min(3, nblk - 1)
    B = P.banks
    A = "a" if mla else "b"
    pfx = pfx + A

    if mla:
        win = P.sb(f"{pfx}win", [128, 8, 672], BF16)
        wq = P.sb(f"{pfx}wq", [128, 3, 768], BF16)
        wkv = P.sb(f"{pfx}wkv", [128, 2, 1024], BF16)
        qgb = P.sb(f"{pfx}qgb", [128, 384], F32)
        kvgb = P.sb(f"{pfx}kvgb", [128, 256], F32)
        rqt = [P.sb(f"{pfx}rq{i}", [128, 2, NH, 16], F32) for i in range(2)]
        rkt = [P.sb(f"{pfx}rk{i}", [128, 2, 16], F32) for i in range(2)]
        latf = P.sb(f"{pfx}latf", [128, 384], F32)
        latn = P.sb(f"{pfx}latn", [128, 384], BF16)
        latT = P.sb(f"{pfx}latT", [128, 3, 128], BF16)
        qf = P.sb(f"{pfx}qf", [128, NH, 96], F32)
        krr = P.sb(f"{pfx}krr", [128, 32], F32)
        sq = P.sb(f"{pfx}sq", [128, NH, 96], F32)
    else:
        win = P.sb(f"{pfx}win", [128, 8, 1536], BF16)
        kct = [P.sb(f"{pfx}kc{i}", [128, NH, 32], F32) for i in range(2)]
        qct = [P.sb(f"{pfx}qc{i}", [128, NH, 4], F32) for i in range(2)]
        mf = P.sb(f"{pfx}mf", [128, 512], F32)
        mqT = P.sb(f"{pfx}mqT", [128, 4, 128], F32)
        KM = P.sb(f"{pfx}KM", [128, 4, 32], F32)
        onesc = P.sb(f"{pfx}ones", [128, 2], F32)
        gs = P.sb(f"{pfx}gs", [128, NH, 16], F32)
        m8 = P.sb(f"{pfx}m8", [128, NH, 8], F32)
        selm = P.sb(f"{pfx}selm", [128, NH, 16], F32)
        sq = P.sb(f"{pfx}sq", [128, NH, 64], F32)
    gb = P.sb(f"{pfx}gb", [128, D], F32)
    negm = P.sb(f"{pfx}negm", [128, 128], BF16)
    xt = [P.sb(f"{pfx}xt{i}", [128, D], F32) for i in range(2)]
    xn = P.sb(f"{pfx}xn", [128, D], BF16)
    junk = P.sb(f"{pfx}junk", [128, D], BF16)
    st = [P.sb(f"{pfx}st{j}", [128, 1], F32) for j in range(3)]
    st2 = [P.sb(f"{pfx}st2{j}", [128, 1], F32) for j in range(3)]
    hT = P.sb(f"{pfx}hT", [128, 8, 128], BF16)
    tmp = [P.sb(f"{pfx}tmp{i}", [128, NH, 16], F32) for i in range(4)]
    ssq = P.sb(f"{pfx}ssq", [128, NH], F32)
    aug = [P.sb(f"{pfx}aug{i}", [128, NH, 128], BF16) for i in range(2)]
    KT = [P.sb(f"{pfx}KT{t}", [128, NH, 128], BF16) for t in range(nt)]
    VP = [P.sb(f"{pfx}VP{t}", [128, NH, 65], BF16) for t in range(nt)]
    QT = P.sb(f"{pfx}QT", [128, NH, 512], BF16)
    PT = [P.sb(f"{pfx}PT{i}", [128, 512], BF16) for i in range(4)]
    o4 = [P.sb(f"{pfx}o{i}", [128, 512], F32) for i in range(4)]
    rc = [P.sb(f"{pfx}rc{i}", [128, 4], F32) for i in range(2)]

    if mla:
        for k in range(8):
            P.dma("pool", win[:, k, :], win_d[k * 128:(k + 1) * 128, 0:672], win, True)
        for j in range(3):
            P.dma("pool", wq[:, j, :], wq_d[j * 128:(j + 1) * 128, :], wq, True)
        for j in range(2):
            P.dma("pool", wkv[:, j, :], wkv_d[j * 128:(j + 1) * 128, :], wkv, True)
        P.dma("sp", qgb[:, :], qg_d.partition_broadcast(128), qgb, True)
        P.dma("sp", kvgb[:, :], kvg_d.partition_broadcast(128), kvgb, True)
    else:
        for k in range(8):
            P.dma("pool", win[:, k, :], win_d[k * 128:(k + 1) * 128, 672:2208], win, True)
        P.op("pool", lambda e: e.memset(KM[:, :, :], 0.0), writes=[KM])
        P.op("pool", lambda e: e.memset(onesc[:, :], 1.0 / BLK), writes=[onesc])
    P.dma("sp", gb[:, :], g_d.partition_broadcast(128), gb, True)
    P.dma("pool", negm[:, :], cst["negm"], negm, True)
    for a_ in aug:
        P.op("pool", lambda e, a_=a_: e.memset(a_[:, :, :], 0.0), writes=[a_])
    for t in range(nt):
        P.op("pool", lambda e, t=t: e.memset(VP[t][:, :, :], 1.0), writes=[VP[t]])

    def load(t, qpass):
        i = t % 2
        P.dma("sp", xt[i][:, :], x_in[t * 128:(t + 1) * 128, :], xt[i], True)
        if mla:
            if qpass:
                P.dma("sp", rqt[i][:, :, :, :], cst["rq"][t * 128:(t + 1) * 128, :].rearrange("p (a h f) -> p a h f", a=2, h=NH),
                      rqt[i], True)
            else:
                P.dma("sp", rkt[i][:, :, :], cst["rk"][t * 128:(t + 1) * 128, :].rearrange("p (a f) -> p a f", a=2), rkt[i], True)
        else:
            if qpass:
                P.dma("sp", qct[i][:, :, :], cst["qc"][t * 128:(t + 1) * 128, :].rearrange("p (h f) -> p h f", h=NH), qct[i], True)
            else:
                P.dma("sp", kct[i][:, :, :], cst["kc"][t * 128:(t + 1) * 128, :].rearrange("p (h f) -> p h f", h=NH), kct[i], True)

    def proj(w, col0, ncols, bank, nk=8, lhs=None):
        lhs = hT if lhs is None else lhs
        for k in range(nk):
            P.op("pe", lambda e, k=k: e.matmul(bank[:, :ncols], lhsT=lhs[:, k, :], rhs=w[:, k, col0:col0 + ncols],
                                               start=(k == 0), stop=(k == nk - 1)),
                 reads=[lhs, w], writes=[bank])

    def latent_norm(ncols, gbt, nchunk, bank):
        rms_rstd(P, C, latf, ncols, st2[0], st2[1], st2[2], junk)
        P.op("dve", lambda e: e.scalar_tensor_tensor(out=latn[:, :ncols], in0=latf[:, :ncols], scalar=st2[2][:, :],
                                                     in1=gbt[:, :ncols], op0=ALU.mult, op1=ALU.mult),
             reads=[latf, st2[2], gbt], writes=[latn])
        bv = bank.h[:, :].bitcast(BF16).rearrange("p (a b) -> p a b", b=128)
        for j in range(nchunk):
            P.op("pe", lambda e, j=j: e.transpose(bv[:, j, :], latn[:, j * 128:(j + 1) * 128], C.ident[:, :]),
                 reads=[latn, C.ident], writes=[bank])
        P.op("act", lambda e: e.copy(out=latT[:, 0:nchunk, :], in_=bv[:, 0:nchunk, :]), reads=[bank], writes=[latT])

    def rope(x1, x2, cs, sn, o1, o2, srct, tabt, dstt, small):
        t = [tt_[:, 0, :] if small else tt_[:, :, :] for tt_ in tmp]
        P.op("pool", lambda e: e.tensor_tensor(out=t[0], in0=x1, in1=cs, op=ALU.mult), reads=[srct, tabt], writes=[tmp[0]])
        P.op("pool", lambda e: e.tensor_tensor(out=t[1], in0=x2, in1=sn, op=ALU.mult), reads=[srct, tabt], writes=[tmp[1]])
        P.op("dve", lambda e: e.tensor_tensor(out=o1, in0=t[0], in1=t[1], op=ALU.subtract), reads=[tmp[0], tmp[1]], writes=[dstt])
        P.op("pool", lambda e: e.tensor_tensor(out=t[2], in0=x2, in1=cs, op=ALU.mult), reads=[srct, tabt], writes=[tmp[2]])
        P.op("pool", lambda e: e.tensor_tensor(out=t[3], in0=x1, in1=sn, op=ALU.mult), reads=[srct, tabt], writes=[tmp[3]])
        P.op("dve", lambda e: e.tensor_tensor(out=o2, in0=t[2], in1=t[3], op=ALU.add), reads=[tmp[2], tmp[3]], writes=[dstt])

    def pe_fence(bank, col, reads, covers=()):
        P.op("pe", lambda e: e.matmul(bank[:, col:col + 2], lhsT=C.ident[:, :], rhs=C.ident[:, 0:2], start=False, stop=True,
                                      skip_group_check=True),
             reads=[C.ident] + list(reads), writes=[bank] + list(covers))

    def transpose_aug(a_, dst_ap, dst_tile):
        bank = B[1]
        bv = bank.h[:, :].bitcast(BF16).rearrange("p (a b) -> p a b", b=128)
        for h in range(NH):
            P.op("pe", lambda e, h=h: e.transpose(bv[:, h, :], a_[:, h, :], C.ident[:, :]), reads=[a_, C.ident], writes=[bank])
        P.op("act", lambda e: e.copy(out=dst_ap, in_=bv[:, 0:8, :]), reads=[bank], writes=[dst_tile])

    load(0, False)
    for t in range(nt):
        i = t % 2
        x = xt[i]
        a_ = aug[i]
        norm_T(P, C, x, gb, xn, st, junk, B[0], hT)
        if t + 1 < nt:
            load(t + 1, False)
        if mla:
            b = B[2]
            proj(win, 384, 288, b)
            P.op("act", lambda e, b=b: e.copy(out=latf[:, 0:288], in_=b[:, 0:288]), reads=[b], writes=[latf])
            rope(latf[:, 256:272], latf[:, 272:288], rkt[i][:, 0, :], rkt[i][:, 1, :], krr[:, 0:16], krr[:, 16:32],
                 latf, rkt[i], krr, True)
            latent_norm(256, kvgb, 2, B[0])
            for hg in range(2):
                b = B[2 + hg]
                proj(wkv, hg * 512, 512, b, nk=2, lhs=latT)
                bv = b[:, :].rearrange("p (h f) -> p h f", h=4)
                P.op("act", lambda e, bv=bv, hg=hg, a_=a_: e.copy(out=a_[:, hg * 4:hg * 4 + 4, 0:64], in_=bv[:, :, 0:64]),
                     reads=[b], writes=[a_])
                P.op("dve", lambda e, bv=bv, hg=hg, t=t: e.tensor_copy(out=VP[t][:, hg * 4:hg * 4 + 4, 0:64], in_=bv[:, :, 64:128]),
                     reads=[b], writes=[VP[t]])
            P.op("dve", lambda e, a_=a_: e.tensor_copy(out=a_[:, :, 64:96], in_=krr[:, :].unsqueeze(1).to_broadcast([128, NH, 32])),
                 reads=[krr], writes=[a_])
            P.op("pool", lambda e, a_=a_: e.memset(a_[:, :, 96:97], 1.0), writes=[a_])
        else:
            b = B[2]
            proj(win, 512, 512, b)
            P.op("act", lambda e, b=b, a_=a_: e.copy(out=a_[:, :, 0:64], in_=b[:, :].rearrange("p (h f) -> p h f", h=NH)),
                 reads=[b], writes=[a_])
            P.op("dve", lambda e, b=b: e.tensor_copy(out=mf[:, :], in_=b[:, :]), reads=[b], writes=[mf])
            b = B[3]
            proj(win, 1024, 512, b)
            P.op("act", lambda e, b=b, t=t: e.copy(out=VP[t][:, :, 0:64], in_=b[:, :].rearrange("p (h f) -> p h f", h=NH)),
                 reads=[b], writes=[VP[t]])
            P.op("pool", lambda e, a_=a_, i=i: e.tensor_copy(out=a_[:, :, 64:96], in_=kct[i][:, :, :]), reads=[kct[i]], writes=[a_])
            kb = B[7]
            for p_ in range(4):
                P.op("pe", lambda e, p_=p_, t=t: e.matmul(kb[:, 2 * p_:2 * p_ + 2], lhsT=mf[:, p_ * 128:(p_ + 1) * 128], rhs=onesc[:, :],
                                                          start=(p_ == 0), stop=(p_ == 3),
                                                          skip_group_check=True),
                     reads=[mf, onesc], writes=[kb])
            pe_fence(kb, 256, [mf, onesc])
            j = t // 2
            kv_ = kb[:, 0:8].rearrange("p (a b) -> p a b", b=2)
            if t % 2 == 0:
                P.op("dve", lambda e, j=j, kv_=kv_: e.tensor_copy(out=KM[0:64, :, j:j + 1], in_=kv_[0:64, :, 0:1]),
                     reads=[kb], writes=[KM])
                P.op("dve", lambda e, j=j, kv_=kv_: e.tensor_copy(out=KM[64:128, :, 16 + j:17 + j], in_=kv_[64:128, :, 0:1]),
                     reads=[kb], writes=[KM])
            else:
                P.op("dve", lambda e, j=j, kv_=kv_: e.tensor_tensor(out=KM[0:64, :, j:j + 1], in0=KM[0:64, :, j:j + 1],
                                                                    in1=kv_[0:64, :, 0:1], op=ALU.add),
                     reads=[kb, KM], writes=[KM])
                P.op("dve", lambda e, j=j, kv_=kv_: e.tensor_tensor(out=KM[64:128, :, 16 + j:17 + j], in0=KM[64:128, :, 16 + j:17 + j],
                                                                    in1=kv_[64:128, :, 0:1], op=ALU.add),
                     reads=[kb, KM], writes=[KM])
        transpose_aug(a_, KT[t][:, :, :], KT[t])

    load(0, True)
    for qb in range(nqb):
        for tt in range(4):
            t = qb * 4 + tt
            i = t % 2
            x = xt[i]
            a_ = aug[i]
            norm_T(P, C, x, gb, xn, st, junk, B[0], hT)
            if t + 1 < nt:
                load(t + 1, True)
            if mla:
                b = B[2]
                proj(win, 0, 384, b)
                P.op("act", lambda e, b=b: e.copy(out=latf[:, 0:384], in_=b[:, 0:384]), reads=[b], writes=[latf])
                latent_norm(384, qgb, 3, B[0])
                for hg in range(2):
                    b = B[2 + hg]
                    proj(wq, hg * 384, 384, b, nk=3, lhs=latT)
                    P.op("act", lambda e, b=b, hg=hg: e.copy(out=qf[:, hg * 4:hg * 4 + 4, :],
                                                             in_=b[:, 0:384].rearrange("p (h f) -> p h f", h=4)),
                         reads=[b], writes=[qf])
                P.op("dve", lambda e, a_=a_: e.tensor_scalar(out=a_[:, :, 0:64], in0=qf[:, :, 0:64], scalar1=MLA_SCALE, scalar2=None,
                                                             op0=ALU.mult), reads=[qf], writes=[a_])
                rope(qf[:, :, 64:80], qf[:, :, 80:96], rqt[i][:, 0, :, :], rqt[i][:, 1, :, :], a_[:, :, 64:80], a_[:, :, 80:96],
                     qf, rqt[i], a_, False)
                P.op("pool", lambda e: e.tensor_tensor(out=sq[:, :, :], in0=qf[:, :, :], in1=qf[:, :, :], op=ALU.mult),
                     reads=[qf], writes=[sq])
                P.op("dve", lambda e: e.tensor_reduce(out=ssq[:, :], in_=sq[:, :, :], axis=AX.X, op=ALU.add), reads=[sq], writes=[ssq])
                P.op("dve", lambda e, a_=a_: e.tensor_scalar(out=a_[:, :, 96:97], in0=ssq[:, :].unsqueeze(2), scalar1=-MLA_SCALE / 2,
                                                             scalar2=None, op0=ALU.mult), reads=[ssq], writes=[a_])
            else:
                own = t // 2
                b = B[2]
                proj(win, 0, 512, b)
                P.op("act", lambda e, b=b: e.copy(out=mf[:, :], in_=b[:, :]), reads=[b], writes=[mf])
                mv3 = mf[:, :].rearrange("p (h f) -> p h f", h=NH)
                P.op("dve", lambda e, a_=a_: e.tensor_scalar(out=a_[:, :, 0:64], in0=mv3, scalar1=MOBA_SCALE, scalar2=None,
                                                             op0=ALU.mult), reads=[mf], writes=[a_])
                P.op("pool", lambda e: e.tensor_tensor(out=sq[:, :, :], in0=mv3, in1=mv3, op=ALU.mult), reads=[mf], writes=[sq])
                P.op("dve", lambda e: e.tensor_reduce(out=ssq[:, :], in_=sq[:, :, :], axis=AX.X, op=ALU.add), reads=[sq], writes=[ssq])
                P.op("dve", lambda e, a_=a_: e.tensor_scalar(out=a_[:, :, 84:85], in0=ssq[:, :].unsqueeze(2), scalar1=-MOBA_SCALE / 2,
                                                             scalar2=None, op0=ALU.mult), reads=[ssq], writes=[a_])
                P.op("pool", lambda e, a_=a_, i=i: e.tensor_copy(out=a_[:, :, 80:84], in_=qct[i][:, :, :]), reads=[qct[i]], writes=[a_])
                if own == 0 or n_sel == 0:
                    P.op("pool", lambda e: e.memset(selm[:, :, :], -1.0), writes=[selm])
                else:
                    bt = B[3]
                    btv = bt[:, :].rearrange("p (a b) -> p a b", b=128)
                    for p_ in range(4):
                        P.op("pe", lambda e, p_=p_: e.transpose(btv[:, p_, :], mf[:, p_ * 128:(p_ + 1) * 128], C.identf[:, :]),
                             reads=[mf, C.identf], writes=[bt])
                    pe_fence(B[2], 256, [mf, C.identf], covers=[bt])
                    P.op("act", lambda e: e.copy(out=mqT[:, :, :], in_=btv[:, 0:4, :]), reads=[bt], writes=[mqT])
                    bg = B[2]
                    for p_ in range(4):
                        P.op("pe", lambda e, p_=p_: e.matmul(bg[:, p_ * 32:(p_ + 1) * 32], lhsT=mqT[:, p_, :], rhs=KM[:, p_, :],
                                                             start=True, stop=True, skip_group_check=True),
                             reads=[mqT, KM], writes=[bg])
                    pe_fence(bg, 256, [mqT, KM])
                    P.op("dve", lambda e: e.tensor_copy(out=gs[:, :, :], in_=bg[:, 0:128].rearrange("p (h f) -> p h f", h=NH)),
                         reads=[bg], writes=[gs])
                    if own < 16:
                        P.op("dve", lambda e, own=own: e.memset(gs[:, :, own:16], -1e30), writes=[gs])
                    if own > n_sel:
                        for h in range(NH):
                            P.op("dve", lambda e, h=h: e.max(out=m8[:, h, :], in_=gs[:, h, :]), reads=[gs], writes=[m8])
                        for h in range(NH):
                            P.op("dve", lambda e, h=h: e.tensor_scalar(out=selm[:, h, :], in0=gs[:, h, :],
                                                                       scalar1=m8[:, h, n_sel - 1:n_sel], scalar2=1.0,
                                                                       op0=ALU.is_ge, op1=ALU.subtract),
                                 reads=[gs, m8], writes=[selm])
                    else:
                        P.op("dve", lambda e: e.tensor_scalar(out=selm[:, :, :], in0=gs[:, :, :], scalar1=-1e29, scalar2=1.0,
                                                              op0=ALU.is_ge, op1=ALU.subtract), reads=[gs], writes=[selm])
                P.op("dve", lambda e, own=own: e.memset(selm[:, :, own:own + 1], 0.0), writes=[selm])
                P.op("dve", lambda e, a_=a_: e.tensor_copy(out=a_[:, :, 64:80], in_=selm[:, :, :]), reads=[selm], writes=[a_])
            transpose_aug(a_, QT[:, :, tt * 128:(tt + 1) * 128], QT)
        nkt = 4 * (qb + 1)
        its = [(h, kt) for h in range(NH) for kt in range(nkt)]
        LOOK = 2
        scb = [B[3], B[4], B[5]]

        def emit_qk(i):
            h, kt = its[i]
            lo = max(0, kt - 4 * qb)
            sc = scb[i % 3]
            pt = PT[i % 4]
            diag = kt >= 4 * qb
            P.op("pe", lambda e: e.matmul(sc[:, lo * 128:512], lhsT=KT[kt][:, h, :], rhs=QT[:, h, lo * 128:512],
                                          start=True, stop=not diag),
                 reads=[KT[kt], QT], writes=[sc])
            if diag:
                P.op("pe", lambda e: e.matmul(sc[:, lo * 128:(lo + 1) * 128], lhsT=C.ident[:, :], rhs=negm[:, :],
                                              start=False, stop=True),
                     reads=[C.ident, negm], writes=[sc])
            P.op("act", lambda e: e.activation(out=pt[:, lo * 128:512], in_=sc[:, lo * 128:512], func=AF.Exp),
                 reads=[sc], writes=[pt])

        def emit_pv(i):
            h, kt = its[i]
            lo = max(0, kt - 4 * qb)
            pt = PT[i % 4]
            acc = B[6 + h % 2]
            accv = acc[:, :].rearrange("p (a b) -> p a b", b=128)
            for qt in range(lo, 4):
                P.op("pe", lambda e, qt=qt: e.matmul(
                    accv[:, qt, 0:65], lhsT=pt[:, qt * 128:(qt + 1) * 128], rhs=VP[kt][:, h, :],
                    start=(kt == 0 and qt == 0), stop=(kt == 4 * qb + qt), skip_group_check=True),
                    reads=[pt, VP[kt]], writes=[acc])
            if kt == nkt - 1:
                r_ = rc[h % 2]
                P.op("dve", lambda e: e.reciprocal(r_[:, :].unsqueeze(2), accv[:, :, 64:65]), reads=[acc], writes=[r_])
                for qt in range(4):
                    P.op("dve", lambda e, qt=qt: e.tensor_scalar(
                        out=o4[qt][:, h * 64:(h + 1) * 64], in0=accv[:, qt, 0:64], scalar1=r_[:, qt:qt + 1], scalar2=None,
                        op0=ALU.mult), reads=[acc, r_], writes=[o4[qt]])

        for i in range(len(its) + LOOK):
            if i < len(its):
                emit_qk(i)
            if i >= LOOK:
                emit_pv(i - LOOK)
        for tt in range(4):
            t = qb * 4 + tt
            P.dma("sp", out_d[t * 128:(t + 1) * 128, :], o4[tt][:, :], o4[tt], False)
    return o4


def about_stage(P, C, S, x_in, a_in, b_in, x_out, wout_d, pfx="ao"):
    nt = S // 128
    B = P.banks
    wo = P.sb(f"{pfx}wo", [128, 8, D], BF16)
    xt = [P.sb(f"{pfx}xt{i}", [128, D], F32) for i in range(3)]
    ab = [P.sb(f"{pfx}ab{i}", [128, 1024], F32) for i in range(2)]
    abn = P.sb(f"{pfx}abn", [128, 1024], BF16)
    abT = P.sb(f"{pfx}abT", [128, 8, 128], BF16)
    for k in range(8):
        P.dma("pool", wo[:, k, :], wout_d[k * 128:(k + 1) * 128, :], wo, True)

    def load(t):
        P.dma("sp", xt[t % 3][:, :], x_in[t * 128:(t + 1) * 128, :], xt[t % 3], True)
        P.dma("sp", ab[t % 2][:, 0:512], a_in[t * 128:(t + 1) * 128, :], ab[t % 2], True)
        P.dma("sp", ab[t % 2][:, 512:1024], b_in[t * 128:(t + 1) * 128, :], ab[t % 2], True)

    load(0)
    for t in range(nt):
        x = xt[t % 3]
        a_ = ab[t % 2]
        P.op("pool", lambda e, a_=a_: e.tensor_copy(out=abn[:, :], in_=a_[:, :]), reads=[a_], writes=[abn])
        if t + 1 < nt:
            load(t + 1)
        bank = B[t % 2]
        bv = bank.h[:, :].bitcast(BF16).rearrange("p (a b) -> p a b", b=128)
        for k in range(8):
            P.op("pe", lambda e, bv=bv, k=k: e.transpose(bv[:, k, :], abn[:, k * 128:(k + 1) * 128], C.ident[:, :]),
                 reads=[abn, C.ident], writes=[bank])
        P.op("act", lambda e, bv=bv: e.copy(out=abT[:, :, :], in_=bv[:, 0:8, :]), reads=[bank], writes=[abT])
        for dh in range(2):
            b = B[2 + (2 * t + dh) % 4]
            for k in range(8):
                P.op("pe", lambda e, b=b, k=k, dh=dh: e.matmul(b[:, :], lhsT=abT[:, k, :], rhs=wo[:, k, dh * 512:(dh + 1) * 512],
                                                               start=(k == 0), stop=(k == 7)),
                     reads=[abT, wo], writes=[b])
            P.op("dve", lambda e, b=b, dh=dh, x=x: e.tensor_tensor(out=x[:, dh * 512:(dh + 1) * 512],
                                                                   in0=x[:, dh * 512:(dh + 1) * 512], in1=b[:, :], op=ALU.add),
                 reads=[x, b], writes=[x])
        P.dma("sp", x_out[t * 128:(t + 1) * 128, :], x[:, :], x, False)
    return xt


def attn_stage2(P, C, S, kind, x_in, out_d, win_d, g_d, cst, wq_d=None, qg_d=None, wkv_d=None, kvg_d=None, pfx="at"):
    mla = kind == "mla"
    nt = S // 128
    nqb = S // 512
    nblk = S // BLK
    n_sel = min(3, nblk - 1)
    B = P.banks
    TB0, TB1, TB2 = B[0], B[1], B[2]
    pfx = pfx + ("a" if mla else "b")

    if mla:
        win = P.sb(f"{pfx}win", [128, 8, 672], BF16)
        wq = P.sb(f"{pfx}wq", [128, 3, 768], BF16)
        wkv = P.sb(f"{pfx}wkv", [128, 2, 1024], BF16)
        qgb = P.sb(f"{pfx}qgb", [128, 384], F32)
        kvgb = P.sb(f"{pfx}kvgb", [128, 256], F32)
        rqt = [P.sb(f"{pfx}rq{i}", [128, 2, NH, 16], F32) for i in range(2)]
        rkt = [P.sb(f"{pfx}rk{i}", [128, 2, 16], F32) for i in range(2)]
        latfK = P.sb(f"{pfx}latfK", [128, 288], F32)
        latfQ = P.sb(f"{pfx}latfQ", [128, 384], F32)
        latnK = P.sb(f"{pfx}latnK", [128, 256], BF16)
        latnQ = P.sb(f"{pfx}latnQ", [128, 384], BF16)
        latTK = P.sb(f"{pfx}latTK", [128, 2, 128], BF16)
        latTQ = P.sb(f"{pfx}latTQ", [128, 3, 128], BF16)
        qf = P.sb(f"{pfx}qf", [128, NH, 96], F32)
        krr = P.sb(f"{pfx}krr", [128, 32], F32)
        sq = P.sb(f"{pfx}sq", [128, NH, 96], F32)
        st2K = [P.sb(f"{pfx}st2K{j}", [128, 1], F32) for j in range(3)]
        st2Q = [P.sb(f"{pfx}st2Q{j}", [128, 1], F32) for j in range(3)]
        junk2 = P.sb(f"{pfx}junk2", [128, 384], BF16)
    else:
        win = P.sb(f"{pfx}win", [128, 8, 1536], BF16)
        kct = [P.sb(f"{pfx}kc{i}", [128, NH, 32], F32) for i in range(2)]
        qct = [P.sb(f"{pfx}qc{i}", [128, NH, 4], F32) for i in range(2)]
        mfK = P.sb(f"{pfx}mfK", [128, 512], F32)
        mfQ = P.sb(f"{pfx}mfQ", [128, 512], F32)
        mqT = P.sb(f"{pfx}mqT", [128, 4, 128], F32)
        KM = P.sb(f"{pfx}KM", [128, 4, 32], F32)
        onesc = P.sb(f"{pfx}ones", [128, 2], F32)
        gs = P.sb(f"{pfx}gs", [128, NH, 16], F32)
        m8 = P.sb(f"{pfx}m8", [128, NH, 8], F32)
        selm = P.sb(f"{pfx}selm", [128, NH, 16], F32)
        sq = P.sb(f"{pfx}sq", [128, NH, 64], F32)
    gb = P.sb(f"{pfx}gb", [128, D], F32)
    negm = P.sb(f"{pfx}negm", [128, 128], BF16)
    xt = [P.sb(f"{pfx}xt{i}", [128, D], F32) for i in range(2)]
    xn = P.sb(f"{pfx}xn", [128, D], BF16)
    junk = P.sb(f"{pfx}junk", [128, D], BF16)
    st = [P.sb(f"{pfx}st{j}", [128, 1], F32) for j in range(3)]
    hT = P.sb(f"{pfx}hT", [128, 8, 128], BF16)
    tmp = [P.sb(f"{pfx}tmp{i}", [128, NH, 16], F32) for i in range(4)]
    ssq = P.sb(f"{pfx}ssq", [128, NH], F32)
    augK = [P.sb(f"{pfx}augK{i}", [128, NH, 128], BF16) for i in range(2)]
    augQ = [P.sb(f"{pfx}augQ{i}", [128, NH, 128], BF16) for i in range(2)]
    KT = [P.sb(f"{pfx}KT{t}", [128, NH, 128], BF16) for t in range(nt)]
    VP = [P.sb(f"{pfx}VP{t}", [128, NH, 65], BF16) for t in range(nt)]
    QT = [P.sb(f"{pfx}QT{i}", [128, NH, 512], BF16) for i in range(2)]
    PT = [P.sb(f"{pfx}PT{i}", [128, 512], BF16) for i in range(4)]
    o4 = [P.sb(f"{pfx}o{i}", [128, 512], F32) for i in range(4)]
    rc = [P.sb(f"{pfx}rc{i}", [128, 4], F32) for i in range(2)]

    if mla:
        for k in range(8):
            P.dma("pool", win[:, k, :], win_d[k * 128:(k + 1) * 128, 0:672], win, True)
        for j in range(3):
            P.dma("pool", wq[:, j, :], wq_d[j * 128:(j + 1) * 128, :], wq, True)
        for j in range(2):
            P.dma("pool", wkv[:, j, :], wkv_d[j * 128:(j + 1) * 128, :], wkv, True)
        P.dma("sp", qgb[:, :], qg_d.partition_broadcast(128), qgb, True)
        P.dma("sp", kvgb[:, :], kvg_d.partition_broadcast(128), kvgb, True)
    else:
        for k in range(8):
            P.dma("pool", win[:, k, :], win_d[k * 128:(k + 1) * 128, 672:2208], win, True)
        P.op("pool", lambda e: e.memset(KM[:, :, :], 0.0), writes=[KM])
        P.op("pool", lambda e: e.memset(onesc[:, :], 1.0 / BLK), writes=[onesc])
    P.dma("sp", gb[:, :], g_d.partition_broadcast(128), gb, True)
    P.dma("pool", negm[:, :], cst["negm"], negm, True)
    for a_ in augK + augQ:
        P.op("pool", lambda e, a_=a_: e.memset(a_[:, :, :], 0.0), writes=[a_])
    for t in range(nt):
        P.op("pool", lambda e, t=t: e.memset(VP[t][:, :, :], 1.0), writes=[VP[t]])

    def load(t):
        i = t % 2
        P.dma("sp", xt[i][:, :], x_in[t * 128:(t + 1) * 128, :], xt[i], True)
        if mla:
            P.dma("sp", rqt[i][:, :, :, :], cst["rq"][t * 128:(t + 1) * 128, :].rearrange("p (a h f) -> p a h f", a=2, h=NH),
                  rqt[i], True)
            P.dma("sp", rkt[i][:, :, :], cst["rk"][t * 128:(t + 1) * 128, :].rearrange("p (a f) -> p a f", a=2), rkt[i], True)
        else:
            P.dma("sp", qct[i][:, :, :], cst["qc"][t * 128:(t + 1) * 128, :].rearrange("p (h f) -> p h f", h=NH), qct[i], True)
            P.dma("sp", kct[i][:, :, :], cst["kc"][t * 128:(t + 1) * 128, :].rearrange("p (h f) -> p h f", h=NH), kct[i], True)

    def proj(w, col0, ncols, bank, nk=8, lhs=None):
        lhs = hT if lhs is None else lhs
        for k in range(nk):
            P.op("pe", lambda e, k=k: e.matmul(bank[:, :ncols], lhsT=lhs[:, k, :], rhs=w[:, k, col0:col0 + ncols],
                                               start=(k == 0), stop=(k == nk - 1)),
                 reads=[lhs, w], writes=[bank])

    def latent_norm(latf, latn, latT, st2, ncols, gbt, nchunk, bank):
        rms_rstd(P, C, latf, ncols, st2[0], st2[1], st2[2], junk2)
        P.op("dve", lambda e: e.scalar_tensor_tensor(out=latn[:, :ncols], in0=latf[:, :ncols], scalar=st2[2][:, :],
                                                     in1=gbt[:, :ncols], op0=ALU.mult, op1=ALU.mult),
             reads=[latf, st2[2], gbt], writes=[latn])
        yield
        bv = bank.h[:, :].bitcast(BF16).rearrange("p (a b) -> p a b", b=128)
        for j in range(nchunk):
            P.op("pe", lambda e, j=j: e.transpose(bv[:, j, :], latn[:, j * 128:(j + 1) * 128], C.ident[:, :]),
                 reads=[latn, C.ident], writes=[bank])
        yield
        P.op("act", lambda e: e.copy(out=latT[:, 0:nchunk, :], in_=bv[:, 0:nchunk, :]), reads=[bank], writes=[latT])

    def rope(x1, x2, cs, sn, o1, o2, srct, tabt, dstt, small):
        t = [tt_[:, 0, :] if small else tt_[:, :, :] for tt_ in tmp]
        P.op("pool", lambda e: e.tensor_tensor(out=t[0], in0=x1, in1=cs, op=ALU.mult), reads=[srct, tabt], writes=[tmp[0]])
        P.op("pool", lambda e: e.tensor_tensor(out=t[1], in0=x2, in1=sn, op=ALU.mult), reads=[srct, tabt], writes=[tmp[1]])
        P.op("pool", lambda e: e.tensor_tensor(out=t[2], in0=x2, in1=cs, op=ALU.mult), reads=[srct, tabt], writes=[tmp[2]])
        P.op("pool", lambda e: e.tensor_tensor(out=t[3], in0=x1, in1=sn, op=ALU.mult), reads=[srct, tabt], writes=[tmp[3]])
        yield
        P.op("dve", lambda e: e.tensor_tensor(out=o1, in0=t[0], in1=t[1], op=ALU.subtract), reads=[tmp[0], tmp[1]], writes=[dstt])
        P.op("dve", lambda e: e.tensor_tensor(out=o2, in0=t[2], in1=t[3], op=ALU.add), reads=[tmp[2], tmp[3]], writes=[dstt])

    def pe_fence(bank, col, reads, covers=()):
        P.op("pe", lambda e: e.matmul(bank[:, col:col + 2], lhsT=C.ident[:, :], rhs=C.ident[:, 0:2], start=False, stop=True,
                                      skip_group_check=True),
             reads=[C.ident] + list(reads), writes=[bank] + list(covers))

    def transpose_aug(a_, dst_ap, dst_tile):
        bank = TB0
        bv = bank.h[:, :].bitcast(BF16).rearrange("p (a b) -> p a b", b=128)
        for h in range(NH):
            P.op("pe", lambda e, h=h: e.transpose(bv[:, h, :], a_[:, h, :], C.ident[:, :]), reads=[a_, C.ident], writes=[bank])
        yield
        P.op("act", lambda e: e.copy(out=dst_ap, in_=bv[:, 0:8, :]), reads=[bank], writes=[dst_tile])

    def norm_T_gen(x):
        rms_rstd(P, C, x, D, st[0], st[1], st[2], junk)
        P.op("dve", lambda e: e.scalar_tensor_tensor(out=xn[:, :], in0=x[:, :], scalar=st[2][:, :], in1=gb[:, :],
                                                     op0=ALU.mult, op1=ALU.mult),
             reads=[x, st[2], gb], writes=[xn])
        yield
        yield
        bv = TB0.h[:, :].bitcast(BF16).rearrange("p (a b) -> p a b", b=128)
        for k in range(8):
            P.op("pe", lambda e, k=k: e.transpose(bv[:, k, :], xn[:, k * 128:(k + 1) * 128], C.ident[:, :]),
                 reads=[xn, C.ident], writes=[TB0])
        yield
        P.op("act", lambda e: e.copy(out=hT[:, :, :], in_=bv[:, 0:8, :]), reads=[TB0], writes=[hT])
        yield

    def tile_gen(t, tt, QTb):
        i = t % 2
        x = xt[i]
        aK = augK[i]
        aQ = augQ[i]
        yield from norm_T_gen(x)
        if t + 1 < nt:
            load(t + 1)
        if mla:
            proj(win, 384, 288, TB1)
            proj(win, 0, 384, TB2)
            yield
            P.op("act", lambda e: e.copy(out=latfK[:, 0:288], in_=TB1[:, 0:288]), reads=[TB1], writes=[latfK])
            P.op("act", lambda e: e.copy(out=latfQ[:, 0:384], in_=TB2[:, 0:384]), reads=[TB2], writes=[latfQ])
            yield
            yield from rope(latfK[:, 256:272], latfK[:, 272:288], rkt[i][:, 0, :], rkt[i][:, 1, :], krr[:, 0:16], krr[:, 16:32],
                            latfK, rkt[i], krr, True)
            yield
            yield from latent_norm(latfK, latnK, latTK, st2K, 256, kvgb, 2, TB0)
            yield
            for hg in range(2):
                b = TB1 if hg == 0 else TB2
                proj(wkv, hg * 512, 512, b, nk=2, lhs=latTK)
            yield
            for hg in range(2):
                b = TB1 if hg == 0 else TB2
                bv = b[:, :].rearrange("p (h f) -> p h f", h=4)
                P.op("act", lambda e, bv=bv, hg=hg: e.copy(out=aK[:, hg * 4:hg * 4 + 4, 0:64], in_=bv[:, :, 0:64]),
                     reads=[b], writes=[aK])
                P.op("dve", lambda e, bv=bv, hg=hg: e.tensor_copy(out=VP[t][:, hg * 4:hg * 4 + 4, 0:64], in_=bv[:, :, 64:128]),
                     reads=[b], writes=[VP[t]])
            P.op("dve", lambda e: e.tensor_copy(out=aK[:, :, 64:96], in_=krr[:, :].unsqueeze(1).to_broadcast([128, NH, 32])),
                 reads=[krr], writes=[aK])
            P.op("pool", lambda e: e.memset(aK[:, :, 96:97], 1.0), writes=[aK])
            yield
            yield from transpose_aug(aK, KT[t][:, :, :], KT[t])
            yield
            yield from latent_norm(latfQ, latnQ, latTQ, st2Q, 384, qgb, 3, TB0)
            yield
            for hg in range(2):
                b = TB1 if hg == 0 else TB2
                proj(wq, hg * 384, 384, b, nk=3, lhs=latTQ)
            yield
            for hg in range(2):
                b = TB1 if hg == 0 else TB2
                P.op("act", lambda e, b=b, hg=hg: e.copy(out=qf[:, hg * 4:hg * 4 + 4, :],
                                                         in_=b[:, 0:384].rearrange("p (h f) -> p h f", h=4)),
                     reads=[b], writes=[qf])
            yield
            P.op("dve", lambda e: e.tensor_scalar(out=aQ[:, :, 0:64], in0=qf[:, :, 0:64], scalar1=MLA_SCALE, scalar2=None,
                                                  op0=ALU.mult), reads=[qf], writes=[aQ])
            P.op("pool", lambda e: e.tensor_tensor(out=sq[:, :, :], in0=qf[:, :, :], in1=qf[:, :, :], op=ALU.mult),
                 reads=[qf], writes=[sq])
            yield
            yield from rope(qf[:, :, 64:80], qf[:, :, 80:96], rqt[i][:, 0, :, :], rqt[i][:, 1, :, :], aQ[:, :, 64:80], aQ[:, :, 80:96],
                            qf, rqt[i], aQ, False)
            P.op("dve", lambda e: e.tensor_reduce(out=ssq[:, :], in_=sq[:, :, :], axis=AX.X, op=ALU.add), reads=[sq], writes=[ssq])
            P.op("dve", lambda e: e.tensor_scalar(out=aQ[:, :, 96:97], in0=ssq[:, :].unsqueeze(2), scalar1=-MLA_SCALE / 2,
                                                  scalar2=None, op0=ALU.mult), reads=[ssq], writes=[aQ])
            yield
        else:
            own = t // 2
            proj(win, 512, 512, TB1)
            proj(win, 1024, 512, TB2)
            yield
            P.op("act", lambda e: e.copy(out=aK[:, :, 0:64], in_=TB1[:, :].rearrange("p (h f) -> p h f", h=NH)),
                 reads=[TB1], writes=[aK])
            P.op("dve", lambda e: e.tensor_copy(out=mfK[:, :], in_=TB1[:, :]), reads=[TB1], writes=[mfK])
            P.op("act", lambda e: e.copy(out=VP[t][:, :, 0:64], in_=TB2[:, :].rearrange("p (h f) -> p h f", h=NH)),
                 reads=[TB2], writes=[VP[t]])
            P.op("pool", lambda e: e.tensor_copy(out=aK[:, :, 64:96], in_=kct[i][:, :, :]), reads=[kct[i]], writes=[aK])
            yield
            kb = TB1
            for p_ in range(4):
                P.op("pe", lambda e, p_=p_: e.matmul(kb[:, 2 * p_:2 * p_ + 2], lhsT=mfK[:, p_ * 128:(p_ + 1) * 128], rhs=onesc[:, :],
                                                     start=(p_ == 0), stop=(p_ == 3), skip_group_check=True),
                     reads=[mfK, onesc], writes=[kb])
            pe_fence(kb, 256, [mfK, onesc])
            proj(win, 0, 512, TB2)
            yield
            j = t // 2
            kv_ = kb[:, 0:8].rearrange("p (a b) -> p a b", b=2)
            if t % 2 == 0:
                P.op("dve", lambda e: e.tensor_copy(out=KM[0:64, :, j:j + 1], in_=kv_[0:64, :, 0:1]), reads=[kb], writes=[KM])
                P.op("dve", lambda e: e.tensor_copy(out=KM[64:128, :, 16 + j:17 + j], in_=kv_[64:128, :, 0:1]),
                     reads=[kb], writes=[KM])
            else:
                P.op("dve", lambda e: e.tensor_tensor(out=KM[0:64, :, j:j + 1], in0=KM[0:64, :, j:j + 1], in1=kv_[0:64, :, 0:1],
                                                      op=ALU.add), reads=[kb, KM], writes=[KM])
                P.op("dve", lambda e: e.tensor_tensor(out=KM[64:128, :, 16 + j:17 + j], in0=KM[64:128, :, 16 + j:17 + j],
                                                      in1=kv_[64:128, :, 0:1], op=ALU.add), reads=[kb, KM], writes=[KM])
            P.op("act", lambda e: e.copy(out=mfQ[:, :], in_=TB2[:, :]), reads=[TB2], writes=[mfQ])
            yield
            yield from transpose_aug(aK, KT[t][:, :, :], KT[t])
            yield
            mv3 = mfQ[:, :].rearrange("p (h f) -> p h f", h=NH)
            P.op("dve", lambda e: e.tensor_scalar(out=aQ[:, :, 0:64], in0=mv3, scalar1=MOBA_SCALE, scalar2=None, op0=ALU.mult),
                 reads=[mfQ], writes=[aQ])
            P.op("pool", lambda e: e.tensor_tensor(out=sq[:, :, :], in0=mv3, in1=mv3, op=ALU.mult), reads=[mfQ], writes=[sq])
            P.op("pool", lambda e: e.tensor_copy(out=aQ[:, :, 80:84], in_=qct[i][:, :, :]), reads=[qct[i]], writes=[aQ])
            yield
            P.op("dve", lambda e: e.tensor_reduce(out=ssq[:, :], in_=sq[:, :, :], axis=AX.X, op=ALU.add), reads=[sq], writes=[ssq])
            P.op("dve", lambda e: e.tensor_scalar(out=aQ[:, :, 84:85], in0=ssq[:, :].unsqueeze(2), scalar1=-MOBA_SCALE / 2,
                                                  scalar2=None, op0=ALU.mult), reads=[ssq], writes=[aQ])
            if own == 0 or n_sel == 0:
                P.op("pool", lambda e: e.memset(selm[:, :, :], -1.0), writes=[selm])
            else:
                bt = TB1
                btv = bt[:, :].rearrange("p (a b) -> p a b", b=128)
                for p_ in range(4):
                    P.op("pe", lambda e, p_=p_: e.transpose(btv[:, p_, :], mfQ[:, p_ * 128:(p_ + 1) * 128], C.identf[:, :]),
                         reads=[mfQ, C.identf], writes=[bt])
                pe_fence(TB2, 256, [mfQ, C.identf], covers=[bt])
                yield
                P.op("act", lambda e: e.copy(out=mqT[:, :, :], in_=btv[:, 0:4, :]), reads=[bt], writes=[mqT])
                yield
                bg = TB2
                for p_ in range(4):
                    P.op("pe", lambda e, p_=p_: e.matmul(bg[:, p_ * 32:(p_ + 1) * 32], lhsT=mqT[:, p_, :], rhs=KM[:, p_, :],
                                                         start=True, stop=True, skip_group_check=True),
                         reads=[mqT, KM], writes=[bg])
                pe_fence(bg, 256, [mqT, KM])
                yield
                P.op("dve", lambda e: e.tensor_copy(out=gs[:, :, :], in_=bg[:, 0:128].rearrange("p (h f) -> p h f", h=NH)),
                     reads=[bg], writes=[gs])
                if own < 16:
                    P.op("dve", lambda e: e.memset(gs[:, :, own:16], -1e30), writes=[gs])
                if own > n_sel:
                    for h in range(NH):
                        P.op("dve", lambda e, h=h: e.max(out=m8[:, h, :], in_=gs[:, h, :]), reads=[gs], writes=[m8])
                    yield
                    for h in range(NH):
                        P.op("dve", lambda e, h=h: e.tensor_scalar(out=selm[:, h, :], in0=gs[:, h, :],
                                                                   scalar1=m8[:, h, n_sel - 1:n_sel], scalar2=1.0,
                                                                   op0=ALU.is_ge, op1=ALU.subtract),
                             reads=[gs, m8], writes=[selm])
                else:
                    P.op("dve", lambda e: e.tensor_scalar(out=selm[:, :, :], in0=gs[:, :, :], scalar1=-1e29, scalar2=1.0,
                                                          op0=ALU.is_ge, op1=ALU.subtract), reads=[gs], writes=[selm])
            yield
            P.op("dve", lambda e: e.memset(selm[:, :, own:own + 1], 0.0), writes=[selm])
            P.op("dve", lambda e: e.tensor_copy(out=aQ[:, :, 64:80], in_=selm[:, :, :]), reads=[selm], writes=[aQ])
            yield
        yield from transpose_aug(aQ, QTb[:, :, tt * 128:(tt + 1) * 128], QTb)
        yield

    def block_gen(qb):
        for tt in range(4):
            yield from tile_gen(qb * 4 + tt, tt, QT[qb % 2])

    load(0)
    for _ in block_gen(0):
        pass
    for qb in range(nqb):
        nxt = block_gen(qb + 1) if qb + 1 < nqb else None
        QTb = QT[qb % 2]
        nkt = 4 * (qb + 1)
        its = [(h, kt) for h in range(NH) for kt in range(nkt)]
        LOOK = 2
        scb = [B[3], B[4], B[5]]
        NSTEP = 110
        per = max(1, -(-NSTEP // len(its)))
        every = max(1, len(its) // NSTEP)

        def emit_qk(i):
            h, kt = its[i]
            lo = max(0, kt - 4 * qb)
            sc = scb[i % 3]
            pt = PT[i % 4]
            diag = kt >= 4 * qb
            q_ap = QTb[:, h, lo * 128:512]
            k_ap = KT[kt][:, h, :]
            P.op("pe", lambda e: e.matmul(sc[:, lo * 128:512], lhsT=k_ap, rhs=q_ap, start=True, stop=not diag),
                 reads=[KT[kt], QTb], writes=[sc])
            if diag:
                P.op("pe", lambda e: e.matmul(sc[:, lo * 128:(lo + 1) * 128], lhsT=C.ident[:, :], rhs=negm[:, :],
                                              start=False, stop=True),
                     reads=[C.ident, negm], writes=[sc])
            P.op("act", lambda e: e.activation(out=pt[:, lo * 128:512], in_=sc[:, lo * 128:512], func=AF.Exp),
                 reads=[sc], writes=[pt])

        def emit_pv(i):
            h, kt = its[i]
            lo = max(0, kt - 4 * qb)
            pt = PT[i % 4]
            acc = B[6 + h % 2]
            accv = acc[:, :].rearrange("p (a b) -> p a b", b=128)
            v_ap = VP[kt][:, h, :]
            for qt in range(lo, 4):
                stp = (kt == 4 * qb + qt)
                P.op("pe", lambda e, qt=qt, stp=stp: e.matmul(
                    accv[:, qt, 0:65], lhsT=pt[:, qt * 128:(qt + 1) * 128], rhs=v_ap,
                    start=(kt == 0 and qt == 0), stop=stp, skip_group_check=True),
                    reads=[pt, VP[kt]], writes=[acc])
            if kt == nkt - 1:
                r_ = rc[h % 2]
                P.op("dve", lambda e: e.reciprocal(r_[:, :].unsqueeze(2), accv[:, :, 64:65]), reads=[acc], writes=[r_])
                for qt in range(4):
                    P.op("dve", lambda e, qt=qt: e.tensor_scalar(
                        out=o4[qt][:, h * 64:(h + 1) * 64], in0=accv[:, qt, 0:64], scalar1=r_[:, qt:qt + 1], scalar2=None,
                        op0=ALU.mult), reads=[acc, r_], writes=[o4[qt]])

        for i in range(len(its) + LOOK):
            if i < len(its):
                emit_qk(i)
            if i >= LOOK:
                emit_pv(i - LOOK)
            if nxt is not None and i % every == 0:
                for _ in range(per):
                    next(nxt, None)
        if nxt is not None:
            for _ in nxt:
                pass
        for tt in range(4):
            t = qb * 4 + tt
            P.dma("sp", out_d[t * 128:(t + 1) * 128, :], o4[tt][:, :], o4[tt], False)
    return o4


def retA_stage2(P, C, S, x_in, on_out, win_d, g_d, cst, cdec, pfx="ra"):
    nch = S // CH
    B = P.banks
    w = P.sb(f"{pfx}w", [128, 8, 4096], BF16)
    gb = P.sb(f"{pfx}gb", [128, D], F32)
    dtt = P.sb(f"{pfx}dt", [128, RH, CH], F32)
    kdec = P.sb(f"{pfx}kdec", [128, RH], F32)
    xt = [P.sb(f"{pfx}xt{i}", [128, D], F32) for i in range(2)]
    tq = [P.sb(f"{pfx}tq{i}", [128, 2, RH, 128], F32) for i in range(2)]
    tk = [P.sb(f"{pfx}tk{i}", [128, 2, 128], F32) for i in range(2)]
    xn = P.sb(f"{pfx}xn", [128, D], BF16)
    junk = P.sb(f"{pfx}junk", [128, D], BF16)
    st = [P.sb(f"{pfx}st{j}", [128, 1], F32) for j in range(3)]
    hT = P.sb(f"{pfx}hT", [128, 8, 128], BF16)
    qs = P.sb(f"{pfx}qs", [128, RH, 2, 128], F32)
    ks = P.sb(f"{pfx}ks", [128, RH, 2, 128], F32)
    tmp = [P.sb(f"{pfx}tmp{i}", [128, RH, 128], F32) for i in range(4)]
    qd2 = [P.sb(f"{pfx}qd{i}", [128, RH, 2, 128], BF16) for i in range(2)]
    kr2 = [P.sb(f"{pfx}kr{i}", [128, RH, 2, 128], BF16) for i in range(2)]
    v2 = [[P.sb(f"{pfx}v{i}_{h}", [128, RDV], BF16) for h in range(RH)] for i in range(2)]
    vd2 = [[P.sb(f"{pfx}vd{i}_{h}", [128, RDV], BF16) for h in range(RH)] for i in range(2)]
    qdT2 = [P.sb(f"{pfx}qdT{i}", [128, 8, 128], BF16) for i in range(2)]
    kT2 = [P.sb(f"{pfx}kT{i}", [128, 8, 128], BF16) for i in range(2)]
    Sf = [P.sb(f"{pfx}S{h}", [128, 2, RDV], F32) for h in range(RH)]
    Sb = [P.sb(f"{pfx}Sb{h}", [128, 2, RDV], BF16) for h in range(RH)]
    PT = [P.sb(f"{pfx}PT{i}", [128, 128], BF16) for i in range(2)]
    gst = [[P.sb(f"{pfx}gs{i}_{j}", [128, 8], F32) for j in range(5)] for i in range(2)]
    on = [P.sb(f"{pfx}on{i}", [128, RH * RDV], F32) for i in range(2)]

    for k in range(8):
        for c0 in range(0, 4096, 2048):
            P.dma("pool", w[:, k, c0:c0 + 2048], win_d[k * 128:(k + 1) * 128, c0:c0 + 2048], w, True)
    P.dma("sp", gb[:, :], g_d.partition_broadcast(128), gb, True)
    P.dma("sp", dtt[:, :, :], cst["dt"].rearrange("p (h q) -> p h q", h=RH), dtt, True)
    P.dma("sp", kdec[:, :], cst["kdec"], kdec, True)

    def load(c):
        i = c % 2
        P.dma("sp", xt[i][:, :], x_in[c * CH:(c + 1) * CH, :], xt[i], True)
        P.dma("sp", tq[i][:, :, :, :], cst["tq"][c * CH:(c + 1) * CH, :].rearrange("p (a h f) -> p a h f", a=2, h=RH),
              tq[i], True)
        P.dma("sp", tk[i][:, :, :], cst["tk"][c * CH:(c + 1) * CH, :].rearrange("p (a f) -> p a f", a=2), tk[i], True)

    def proj(col0, ncols, bank):
        for k in range(8):
            P.op("pe", lambda e, k=k: e.matmul(bank[:, :ncols], lhsT=hT[:, k, :], rhs=w[:, k, col0:col0 + ncols],
                                               start=(k == 0), stop=(k == 7)),
                 reads=[hT, w], writes=[bank])

    def rotate(src, dst, cs, sn, tab):
        x1, x2 = src[:, :, 0, :], src[:, :, 1, :]
        P.op("pool", lambda e: e.tensor_tensor(out=tmp[0][:, :, :], in0=x1, in1=cs, op=ALU.mult), reads=[src, tab], writes=[tmp[0]])
        P.op("pool", lambda e: e.tensor_tensor(out=tmp[1][:, :, :], in0=x2, in1=sn, op=ALU.mult), reads=[src, tab], writes=[tmp[1]])
        P.op("pool", lambda e: e.tensor_tensor(out=tmp[2][:, :, :], in0=x2, in1=cs, op=ALU.mult), reads=[src, tab], writes=[tmp[2]])
        P.op("pool", lambda e: e.tensor_tensor(out=tmp[3][:, :, :], in0=x1, in1=sn, op=ALU.mult), reads=[src, tab], writes=[tmp[3]])
        P.op("dve", lambda e: e.tensor_tensor(out=dst[:, :, 0, :], in0=tmp[0][:, :, :], in1=tmp[1][:, :, :], op=ALU.subtract),
             reads=[tmp[0], tmp[1]], writes=[dst])
        P.op("dve", lambda e: e.tensor_tensor(out=dst[:, :, 1, :], in0=tmp[2][:, :, :], in1=tmp[3][:, :, :], op=ALU.add),
             reads=[tmp[2], tmp[3]], writes=[dst])

    def A_gen(c):
        i = c % 2
        x = xt[i]
        qd, kr, v, vd, qdT, kT = qd2[i], kr2[i], v2[i], vd2[i], qdT2[i], kT2[i]
        norm_T(P, C, x, gb, xn, st, junk, B[0], hT)
        if c + 1 < nch:
            load(c + 1)
        yield
        for g in range(2):
            b = B[2 + g]
            proj(g * 512, 512, b)
            P.op("act", lambda e, b=b, g=g: e.copy(out=qs[:, 2 * g:2 * g + 2, :, :],
                                                   in_=b[:, :].rearrange("p (h a f) -> p h a f", h=2, a=2)),
                 reads=[b], writes=[qs])
            yield
        rotate(qs, qd, tq[i][:, 0, :, :], tq[i][:, 1, :, :], tq[i])
        for g in range(2):
            b = B[2 + g]
            proj(1024 + g * 512, 512, b)
            P.op("act", lambda e, b=b, g=g: e.copy(out=ks[:, 2 * g:2 * g + 2, :, :],
                                                   in_=b[:, :].rearrange("p (h a f) -> p h a f", h=2, a=2)),
                 reads=[b], writes=[ks])
            yield
        rotate(ks, kr, tk[i][:, 0, :].unsqueeze(1).to_broadcast([128, RH, 128]),
               tk[i][:, 1, :].unsqueeze(1).to_broadcast([128, RH, 128]), tk[i])
        for h in range(RH):
            b = B[2 + h % 2]
            proj(2048 + h * 512, 512, b)
            P.op("act", lambda e, b=b, h=h: e.copy(out=v[h][:, :], in_=b[:, :]), reads=[b], writes=[v[h]])
            P.op("dve", lambda e, b=b, h=h: e.tensor_scalar(out=vd[h][:, :], in0=b[:, :], scalar1=kdec[:, h:h + 1],
                                                            scalar2=None, op0=ALU.mult),
                 reads=[b, kdec], writes=[vd[h]])
            yield
        for (src, dstT, bank, eng) in ((qd, qdT, B[0], "dve"), (kr, kT, B[1], "act")):
            bv = bank.h[:, :].bitcast(BF16).rearrange("p (a b) -> p a b", b=128)
            for h in range(RH):
                for a in range(2):
                    P.op("pe", lambda e, src=src, bv=bv, h=h, a=a: e.transpose(bv[:, 2 * h + a, :], src[:, h, a, :],
                                                                              C.ident[:, :]),
                         reads=[src, C.ident], writes=[bank])
            if eng == "dve":
                P.op("dve", lambda e, dstT=dstT, bv=bv: e.tensor_copy(out=dstT[:, :, :], in_=bv[:, 0:8, :]),
                     reads=[bank], writes=[dstT])
            else:
                P.op("act", lambda e, dstT=dstT, bv=bv: e.copy(out=dstT[:, :, :], in_=bv[:, 0:8, :]),
                     reads=[bank], writes=[dstT])
            yield

    def R_gen(c):
        i = c % 2
        qd, kr, v, vd, qdT, kT = qd2[i], kr2[i], v2[i], vd2[i], qdT2[i], kT2[i]
        o_t = on[c % 2]
        first = (c == 0)
        lastc = (c + 1 >= nch)
        for h in range(RH):
            sc = B[4]
            for a in range(2):
                P.op("pe", lambda e, h=h, a=a: e.matmul(sc[:, 0:128], lhsT=kT[:, 2 * h + a, :], rhs=qdT[:, 2 * h + a, :],
                                                        start=(a == 0), stop=(a == 1)),
                     reads=[kT, qdT], writes=[sc])
            sus = []
            for a in range(2):
                su = B[6 + a]
                sus.append(su)
                P.op("pe", lambda e, su=su, h=h, a=a: e.matmul(su[:, :], lhsT=kr[:, h, a, :], rhs=vd[h][:, :],
                                                               start=True, stop=True),
                     reads=[kr, vd[h]], writes=[su])
            yield
            pt = PT[h % 2]
            P.op("dve", lambda e, pt=pt, h=h: e.tensor_tensor(out=pt[:, :], in0=sc[:, 0:128], in1=dtt[:, h, :], op=ALU.mult),
                 reads=[sc, dtt], writes=[pt])
            yield
            ob = B[5]
            P.op("pe", lambda e, pt=pt, h=h: e.matmul(ob[:, :], lhsT=pt[:, :], rhs=v[h][:, :], start=True, stop=first),
                 reads=[pt, v[h]], writes=[ob])
            if not first:
                for a in range(2):
                    P.op("pe", lambda e, h=h, a=a: e.matmul(ob[:, :], lhsT=qdT[:, 2 * h + a, :], rhs=Sb[h][:, a, :],
                                                            start=False, stop=(a == 1)),
                         reads=[qdT, Sb[h]], writes=[ob])
            yield
            for a in range(2):
                su = sus[a]
                if first:
                    P.op("dve", lambda e, su=su, h=h, a=a: e.tensor_copy(out=Sf[h][:, a, :], in_=su[:, :]),
                         reads=[su], writes=[Sf[h]])
                else:
                    P.op("dve", lambda e, su=su, h=h, a=a: e.scalar_tensor_tensor(
                        out=Sf[h][:, a, :], in0=Sf[h][:, a, :], scalar=cdec[h], in1=su[:, :], op0=ALU.mult, op1=ALU.add),
                        reads=[su, Sf[h]], writes=[Sf[h]])
            if not lastc:
                P.op("pool", lambda e, h=h: e.tensor_copy(out=Sb[h][:, :, :], in_=Sf[h][:, :, :]),
                     reads=[Sf[h]], writes=[Sb[h]])
            g5 = gst[h % 2]
            P.op("dve", lambda e, g5=g5: e.bn_stats(out=g5[0][:, 0:6], in_=ob[:, :]), reads=[ob], writes=[g5[0]])
            P.op("dve", lambda e, g5=g5: e.bn_aggr(out=g5[1][:, 0:2], in_=g5[0][:, 0:6]), reads=[g5[0]], writes=[g5[1]])
            yield
            P.op("act", lambda e, g5=g5: e.activation(out=g5[2][:, 0:1], in_=g5[1][:, 1:2], func=AF.Sqrt,
                                                      bias=C.eps[:, :], scale=1.0),
                 reads=[g5[1], C.eps], writes=[g5[2]])
            P.op("dve", lambda e, g5=g5: e.reciprocal(g5[3][:, 0:1], g5[2][:, 0:1]), reads=[g5[2]], writes=[g5[3]])
            P.op("dve", lambda e, g5=g5: e.tensor_scalar(out=g5[4][:, 0:1], in0=g5[1][:, 0:1], scalar1=g5[3][:, 0:1],
                                                         scalar2=-1.0, op0=ALU.mult, op1=ALU.mult),
                 reads=[g5[1], g5[3]], writes=[g5[4]])
            yield
            P.op("act", lambda e, g5=g5, h=h: e.activation(
                out=o_t[:, h * RDV:(h + 1) * RDV], in_=ob[:, :], func=AF.Identity, bias=g5[4][:, 0:1], scale=g5[3][:, 0:1]),
                reads=[ob, g5[3], g5[4]], writes=[o_t])
            yield
        P.dma("sp", on_out[c * CH:(c + 1) * CH, :], o_t[:, :], o_t, False)

    load(0)
    for _ in A_gen(0):
        pass
    for c in range(nch):
        ag = A_gen(c + 1) if c + 1 < nch else None
        rg = R_gen(c)
        done_a = ag is None
        done_r = False
        while not (done_a and done_r):
            if not done_a:
                try:
                    next(ag)
                except StopIteration:
                    done_a = True
            for _ in range(2):
                if not done_r:
                    try:
                        next(rg)
                    except StopIteration:
                        done_r = True
    return on


from concourse.bass_utils import run_bass_kernel_spmd

SEQ_FULL = 4096
NCORES = 8
MODE = "fused"


def _consts(S):
    c1, cdec = ret_consts(S)
    c2 = ab_consts(S)
    c = {}
    c.update(c1)
    c.update(c2)
    c["identf"] = np.eye(128, dtype=np.float32)
    return c, cdec


class _Builder:
    def __init__(self):
        self.nc = bass.Bass("TRN2", target_bir_lowering=False)
        self.P = Prog(self.nc)
        self.P.make_banks()
        self.C = Common(self.P)
        self.ins = {}

    def din(self, name, shape):
        if name not in self.ins:
            self.ins[name] = self.nc.dram_tensor(name, list(shape), F32, kind="ExternalInput").ap()
        return self.ins[name]

    def dout(self, name, shape):
        return self.nc.dram_tensor(name, list(shape), F32, kind="ExternalOutput").ap()

    def scratch(self, name, shape):
        return self.nc.dram_tensor(name, list(shape), F32).ap()

    def cst(self, cnp):
        b = self

        class _Lazy(dict):
            def __missing__(self, k):
                v = b.din("c_" + k, cnp[k].shape)
                self[k] = v
                return v
        return _Lazy()

    def start(self, cst):
        self.C.setup(cst["identf"])
        self.P.dma_tiles_keep = list(self.P.dma_tiles)

    def finish(self):
        self.P.barrier()
        self.P.finalize()
        return self.nc


_CONST_CACHE = {}


def _get_consts(S):
    if S not in _CONST_CACHE:
        _CONST_CACHE[S] = _consts(S)
    return _CONST_CACHE[S]


def build_stage_prog(kind, S):
    cnp, cdec = _get_consts(S)
    b = _Builder()
    cst = b.cst(cnp)
    b.start(cst)
    P, C = b.P, b.C
    P.stage_begin()
    x = b.din("x", (S, D))
    if kind in ("mla", "moba"):
        o = b.dout("o", (S, 512))
        if kind == "mla":
            attn_stage2(P, C, S, kind, x, o, b.din("win", (D, 2208)), b.din("g", (D,)), cst,
                       b.din("wq", (384, 768)), b.din("qg", (384,)), b.din("wkv", (256, 1024)), b.din("kvg", (256,)))
        else:
            attn_stage2(P, C, S, kind, x, o, b.din("win", (D, 2208)), b.din("g", (D,)), cst)
    elif kind == "about":
        y = b.dout("y", (S, D))
        about_stage(P, C, S, x, b.din("a", (S, 512)), b.din("b", (S, 512)), y, b.din("wout", (1024, D)))
    elif kind == "retA":
        o = b.dout("on", (S, 2048))
        retA_stage2(P, C, S, x, o, b.din("win", (D, 6144)), b.din("g", (D,)), cst, cdec)
    elif kind == "retB":
        y = b.dout("y", (S, D))
        retB_stage(P, C, S, x, b.din("onin", (S, 2048)), y, b.din("win", (D, 6144)), b.din("wout", (2048, D)),
                   b.din("g", (D,)), b.din("rn", (128, 16)))
    elif kind in ("mlp", "mlpf"):
        y = b.dout("y", (S, D))
        mlp_stage(P, C, S, x, y, b.din("wup", (D, FF)), b.din("wdn", (FF, D)), b.din("g", (D,)),
                  b.din("gf", (D,)) if kind == "mlpf" else None)
    P.stage_end()
    nc = b.finish()
    return nc, list(b.ins.keys())


def build_fused_prog(S, depth=4):
    cnp, cdec = _get_consts(S)
    b = _Builder()
    cst = b.cst(cnp)
    b.start(cst)
    P, C = b.P, b.C
    x = b.din("x", (S, D))
    y = b.dout("y", (S, D))
    n_even = (depth + 1) // 2
    n_odd = depth // 2
    norm_mix = b.din("norm_mix", (depth, D))
    norm_mlp = b.din("norm_mlp", (depth, D))
    wup = b.din("w_mlp_up", (depth, D, FF))
    wdn = b.din("w_mlp_down", (depth, FF, D))
    win_ab = b.din("w_in_ab", (n_even, D, 2208))
    qg = b.din("mla_q_norm", (n_even, 384))
    wq = b.din("mla_w_q_up", (n_even, 384, 768))
    kvg = b.din("mla_kv_norm", (n_even, 256))
    wkv = b.din("mla_w_kv_up", (n_even, 256, 1024))
    wout_ab = b.din("w_out_ab", (n_even, 1024, D))
    if n_odd:
        win_c = b.din("w_in_c", (n_odd, D, 6144))
        rn = b.din("ret_norm_l", (n_odd, 128, 16))
        wout_c = b.din("w_out_c", (n_odd, 2048, D))
    gf = b.din("norm_final", (D,))
    xs = [b.scratch("xs0", (S, D)), b.scratch("xs1", (S, D))]
    a_s = b.scratch("a_s", (S, 512))
    b_s = b.scratch("b_s", (S, 512))
    on_s = b.scratch("on_s", (S, 2048))
    cur = x
    for l in range(depth):
        j = l // 2
        mid = xs[0]
        if l % 2 == 0:
            P.stage_begin()
            attn_stage2(P, C, S, "mla", cur, a_s, win_ab[j], norm_mix[l], cst, wq[j], qg[j], wkv[j], kvg[j], pfx=f"L{l}")
            P.stage_end()
            P.stage_begin()
            attn_stage2(P, C, S, "moba", cur, b_s, win_ab[j], norm_mix[l], cst, pfx=f"L{l}")
            P.stage_end()
            P.stage_begin()
            about_stage(P, C, S, cur, a_s, b_s, mid, wout_ab[j], pfx=f"L{l}ao")
            P.stage_end()
        else:
            P.stage_begin()
            retA_stage2(P, C, S, cur, on_s, win_c[j], norm_mix[l], cst, cdec, pfx=f"L{l}ra")
            P.stage_end()
            P.stage_begin()
            retB_stage(P, C, S, cur, on_s, mid, win_c[j], wout_c[j], norm_mix[l], rn[j], pfx=f"L{l}rb")
            P.stage_end()
        last = l == depth - 1
        dst = y if last else xs[1]
        P.stage_begin()
        mlp_stage(P, C, S, mid, dst, wup[l], wdn[l], norm_mlp[l], gf if last else None, pfx=f"L{l}m")
        P.stage_end()
        cur = dst
    nc = b.finish()
    return nc, list(b.ins.keys())


_PROGS = {}


def _prog(kind, S, depth=4):
    key = (kind, S, depth)
    if key not in _PROGS:
        _PROGS[key] = build_stage_prog(kind, S) if kind != "fused" else build_fused_prog(S, depth)
    return _PROGS[key]


def _launch(kind, S, per_core_inputs, shared, depth=4):
    nc, names = _prog(kind, S, depth)
    cnp, _ = _get_consts(S)
    in_maps = []
    for pc in per_core_inputs:
        m = {}
        for n in names:
            if n in pc:
                m[n] = pc[n]
            elif n in shared:
                m[n] = shared[n]
            elif n.startswith("c_"):
                m[n] = cnp[n[2:]]
            else:
                raise KeyError(n)
        in_maps.append(m)
    res = run_bass_kernel_spmd(nc, in_maps, core_ids=list(range(len(in_maps))))
    return res.results


def _f32(a):
    return np.ascontiguousarray(np.asarray(a, dtype=np.float32))


def forward(inputs, S, ncores, mode):
    x = _f32(inputs["x"])
    depth = inputs["norm_mix"].shape[0]
    W = {k: _f32(v) for k, v in inputs.items() if k != "x"}
    rnl = np.ascontiguousarray(W["ret_norm"].reshape(-1, 16, 128).transpose(0, 2, 1)) if "ret_norm" in W else None
    if mode == "fused":
        shared = dict(W)
        shared.pop("ret_norm", None)
        if rnl is not None:
            shared["ret_norm_l"] = rnl
        out = _launch("fused", S, [{"x": x[c]} for c in range(ncores)], shared, depth)
        return np.stack([o["y"] for o in out], axis=0)
    cur = [x[c] for c in range(ncores)]
    for l in range(depth):
        j = l // 2
        g = W["norm_mix"][l]
        if l % 2 == 0:
            sh = dict(win=W["w_in_ab"][j], g=g, wq=W["mla_w_q_up"][j], qg=W["mla_q_norm"][j],
                      wkv=W["mla_w_kv_up"][j], kvg=W["mla_kv_norm"][j])
            a = _launch("mla", S, [{"x": c} for c in cur], sh)
            bb = _launch("moba", S, [{"x": c} for c in cur], sh)
            r = _launch("about", S, [{"x": c, "a": a[i]["o"], "b": bb[i]["o"]} for i, c in enumerate(cur)],
                        dict(wout=W["w_out_ab"][j]))
        else:
            sh = dict(win=W["w_in_c"][j], g=g, wout=W["w_out_c"][j], rn=rnl[j])
            on = _launch("retA", S, [{"x": c} for c in cur], sh)
            r = _launch("retB", S, [{"x": c, "onin": on[i]["on"]} for i, c in enumerate(cur)], sh)
        cur = [o["y"] for o in r]
        last = l == depth - 1
        sh = dict(wup=W["w_mlp_up"][l], wdn=W["w_mlp_down"][l], g=W["norm_mlp"][l], gf=W["norm_final"])
        r = _launch("mlpf" if last else "mlp", S, [{"x": c} for c in cur], sh)
        cur = [o["y"] for o in r]
    return np.stack(cur, axis=0)


def kernel(**inputs):
    out = forward(inputs, SEQ_FULL, NCORES, MODE)
    return out.astype(np.float32)
```
